# Optimizing a Trainium2 kernel written in Bass

```python
import math
import jax
import jax.numpy as jnp
from jax import lax
import numpy as np

D_MODEL = 1024
BATCH = 16
SEQ = 2048
DEPTH = 2

CTX_LEN = 256
GRID_W = 64
HEAD_DIM = 64
MIX_WIDTH = D_MODEL
S5_WIDTH = MIX_WIDTH // 4
S5_GROUP = 16
S5_GROUPS = S5_WIDTH // S5_GROUP
S5_STATE = 64
ATT_WIDTH = MIX_WIDTH // 2
ATT_HEADS = ATT_WIDTH // HEAD_DIM
ATT_KV_HEADS = ATT_HEADS // 4
ATT_REP = ATT_HEADS // ATT_KV_HEADS
KV_WIDTH = ATT_KV_HEADS * HEAD_DIM
WINDOW = 128
ATT_BLOCK = 128
RET_WIDTH = MIX_WIDTH - S5_WIDTH - ATT_WIDTH
RET_HEADS = RET_WIDTH // HEAD_DIM
RET_CHUNK = 128
IN_SIZES = (S5_WIDTH, ATT_WIDTH, KV_WIDTH, KV_WIDTH, RET_WIDTH, RET_WIDTH, RET_WIDTH, RET_WIDTH)
IN_WIDTH = sum(IN_SIZES)
D_FF = -(-8 * D_MODEL // (3 * 128)) * 128
N_EXPERTS = 8
TOP_K = 2
D_FF_EXPERT = 7 * D_MODEL // 2
N_DENSE_LAYERS = (DEPTH + 1) // 2
N_MOE_LAYERS = DEPTH // 2
ALPHA = (2 * DEPTH) ** 0.25
BETA = (8 * DEPTH) ** -0.25
LN_EPS = 1e-5
ROPE_BASE = 10000.0
NEG_INF = -1e30

kernel_name = 'hybrid_s5_swa_retention_moe_dit'


def layer_norm(x):
    xf = x.astype(jnp.float32)
    mu = jnp.mean(xf, axis=-1, keepdims=True)
    var = jnp.mean(jnp.square(xf - mu), axis=-1, keepdims=True)
    return ((xf - mu) * lax.rsqrt(var + LN_EPS)).astype(x.dtype)


def modulate(x, shift, scale):
    return layer_norm(x) * (1.0 + scale) + shift


def post_norm(x, update, gain, bias):
    return layer_norm(ALPHA * x + update) * gain + bias


def rope(x, pos):
    d = x.shape[-1]
    inv_freq = ROPE_BASE ** (-jnp.arange(0, d, 2, dtype=jnp.float32) / d)
    ang = pos[:, None] * inv_freq[None, :]
    cos = jnp.cos(ang)[None, :, None, :]
    sin = jnp.sin(ang)[None, :, None, :]
    x1, x2 = jnp.split(x.astype(jnp.float32), 2, axis=-1)
    return jnp.concatenate([x1 * cos - x2 * sin, x1 * sin + x2 * cos], axis=-1).astype(x.dtype)


def axial_rope(x, rows, cols):
    xr, xc = jnp.split(x, 2, axis=-1)
    return jnp.concatenate([rope(xr, rows), rope(xc, cols)], axis=-1)


def diag_linear_scan(lam_bar, bu):
    a = jnp.broadcast_to(lam_bar, bu.shape)

    def combine(e1, e2):
        a1, b1 = e1
        a2, b2 = e2
        return a1 * a2, a2 * b1 + b2

    return lax.associative_scan(combine, (a, bu), axis=1)[1]


def s5_direction(ux, uz, lam_re, lam_im, log_step, b_re, b_im, c_re, c_im, reverse, need_ctx):
    f32 = jnp.float32
    lam = lax.complex(lam_re.astype(f32), lam_im.astype(f32))
    lam_dt = lam * jnp.exp(log_step.astype(f32))[:, None]
    lam_bar = jnp.exp(lam_dt)
    b_bar = lax.complex(b_re.astype(f32), b_im.astype(f32)) * ((lam_bar - 1.0) / lam)[:, :, None]
    c_mat = lax.complex(c_re.astype(f32), c_im.astype(f32))
    if reverse:
        ux, uz = jnp.flip(ux, axis=1), jnp.flip(uz, axis=1)

    def drive(u):
        return lax.complex(jnp.einsum('btgc,gnc->btgn', u, b_bar.real),
                           jnp.einsum('btgc,gnc->btgn', u, b_bar.imag))

    def read(h):
        return jnp.einsum('btgn,gcn->btgc', h, c_mat).real

    h_z = diag_linear_scan(lam_bar, drive(uz))
    steps = jnp.arange(1, ux.shape[1] + 1, dtype=f32)[:, None, None]
    h_x = diag_linear_scan(lam_bar, drive(ux)) + jnp.exp(steps * lam_dt) * h_z[:, -1:]
    y_x = read(h_x)
    y_z = read(h_z) if need_ctx else None
    if reverse:
        y_x = jnp.flip(y_x, axis=1)
        y_z = jnp.flip(y_z, axis=1) if need_ctx else None
    return y_x, y_z


def s5_mixer(u, L, lam_re, lam_im, log_step, b_re, b_im, c_re, c_im, d_skip, w_glu, b_glu, need_ctx):
    B, n_tok, _ = u.shape
    ug = u.astype(jnp.float32).reshape(B, n_tok, S5_GROUPS, S5_GROUP)
    uz, ux = ug[:, :L], ug[:, L:]
    yf_x, yf_z = s5_direction(ux, uz, lam_re[0], lam_im[0], log_step[0], b_re[0], b_im[0],
                              c_re[0], c_im[0], False, need_ctx)
    yb_x, yb_z = s5_direction(ux, uz, lam_re[1], lam_im[1], log_step[1], b_re[1], b_im[1],
                              c_re[1], c_im[1], True, need_ctx)
    y = yf_x + yb_x
    u_sel = ux
    if need_ctx:
        y = jnp.concatenate([yf_z + yb_z, y], axis=1)
        u_sel = ug
    y = y + d_skip.astype(jnp.float32).reshape(S5_GROUPS, S5_GROUP) * u_sel
    g = jax.nn.gelu(y.reshape(B, -1, S5_WIDTH)).astype(u.dtype)
    return g * jax.nn.sigmoid(g @ w_glu + b_glu)


def window_attention_latent(q, k, v, kc, vc, sink):
    B, T, _, _ = q.shape
    nb = T // ATT_BLOCK
    nk = 3 * ATT_BLOCK
    n_ctx = kc.shape[1]
    scale = HEAD_DIM ** -0.5
    qb = q.reshape(B, nb, ATT_BLOCK, ATT_KV_HEADS, ATT_REP, HEAD_DIM)
    pad = ((0, 0), (ATT_BLOCK, ATT_BLOCK), (0, 0), (0, 0))

    def band(t):
        tp = jnp.pad(t, pad).reshape(B, nb + 2, ATT_BLOCK, ATT_KV_HEADS, HEAD_DIM)
        return jnp.concatenate([tp[:, :-2], tp[:, 1:-1], tp[:, 2:]], axis=2)

    kw, vw = band(k), band(v)
    blk = jnp.arange(nb)[:, None, None] * ATT_BLOCK
    q_pos = blk + jnp.arange(ATT_BLOCK)[None, :, None]
    k_pos = blk - ATT_BLOCK + jnp.arange(nk)[None, None, :]
    valid = (jnp.abs(k_pos - q_pos) <= WINDOW) & (k_pos >= 0) & (k_pos < T)
    s_loc = jnp.einsum('bnqgrd,bnkgd->bgrnqk', qb, kw).astype(jnp.float32) * scale
    s_loc = jnp.where(valid, s_loc, NEG_INF)
    s_ctx = jnp.einsum('bnqgrd,bcgd->bgrnqc', qb, kc).astype(jnp.float32) * scale
    s_sink = jnp.broadcast_to(sink.astype(jnp.float32).reshape(1, ATT_KV_HEADS, ATT_REP, 1, 1, 1),
                              s_loc.shape[:-1] + (1,))
    p = jax.nn.softmax(jnp.concatenate([s_loc, s_ctx, s_sink], axis=-1), axis=-1).astype(v.dtype)
    o = (jnp.einsum('bgrnqk,bnkgd->bnqgrd', p[..., :nk], vw)
         + jnp.einsum('bgrnqc,bcgd->bnqgrd', p[..., nk:nk + n_ctx], vc))
    return o.reshape(B, T, ATT_WIDTH)


def context_attention(q, k, v, sink):
    B, L, _, _ = q.shape
    qg = q.reshape(B, L, ATT_KV_HEADS, ATT_REP, HEAD_DIM)
    s = jnp.einsum('blgrd,bcgd->bgrlc', qg, k).astype(jnp.float32) * HEAD_DIM ** -0.5
    s_sink = jnp.broadcast_to(sink.astype(jnp.float32).reshape(1, ATT_KV_HEADS, ATT_REP, 1, 1),
                              s.shape[:-1] + (1,))
    p = jax.nn.softmax(jnp.concatenate([s, s_sink], axis=-1), axis=-1).astype(v.dtype)
    o = jnp.einsum('bgrlc,bcgd->blgrd', p[..., :k.shape[1]], v)
    return o.reshape(B, L, ATT_WIDTH)


def window_attention_mixer(q, k, v, L, rows, cols, sink, need_ctx):
    B, n_tok, _ = q.shape
    q = q.reshape(B, n_tok, ATT_HEADS, HEAD_DIM)
    k = k.reshape(B, n_tok, ATT_KV_HEADS, HEAD_DIM)
    v = v.reshape(B, n_tok, ATT_KV_HEADS, HEAD_DIM)
    kz, vz = k[:, :L], v[:, :L]
    qx = axial_rope(q[:, L:], rows, cols)
    kx = axial_rope(k[:, L:], rows, cols)
    ox = window_attention_latent(qx, kx, v[:, L:], kz, vz, sink)
    if need_ctx:
        return jnp.concatenate([context_attention(q[:, :L], kz, vz, sink), ox], axis=1)
    return ox


def retention_chunkwise(q, k, v, log_gamma, s0, include_diag):
    B, T, H, _ = q.shape
    n = T // RET_CHUNK
    qc, kc, vc = (t.reshape(B, n, RET_CHUNK, H, -1) for t in (q, k, v))
    idx = jnp.arange(RET_CHUNK, dtype=jnp.float32)
    diff = idx[:, None] - idx[None, :]
    mask = (diff >= 0) if include_diag else (diff > 0)
    decay = jnp.where(mask, jnp.exp(log_gamma[:, None, None] * jnp.maximum(diff, 0.0)), 0.0)
    scores = jnp.einsum('bnihd,bnjhd->bnhij', qc, kc) * decay
    o_intra = jnp.einsum('bnhij,bnjhe->bnihe', scores, vc)
    k_w = jnp.exp(log_gamma[None, :] * (RET_CHUNK - 1 - idx)[:, None])
    contrib = jnp.einsum('bnjhd,jh,bnjhe->bnhde', kc, k_w, vc)
    chunk_decay = jnp.exp(log_gamma * RET_CHUNK)[None, :, None, None]

    def step(s, cb):
        return chunk_decay * s + cb, s

    s_final, s_before = lax.scan(step, s0, jnp.moveaxis(contrib, 1, 0))
    q_w = jnp.exp(log_gamma[None, :] * (idx + 1.0)[:, None])
    o_cross = jnp.einsum('bnihd,ih,nbhde->bnihe', qc, q_w, s_before)
    return (o_intra + o_cross).reshape(B, T, H, -1), s_final


def retention_final_state(k, v, log_gamma):
    T = k.shape[1]
    w = jnp.exp(log_gamma[None, :] * (T - 1 - jnp.arange(T, dtype=jnp.float32))[:, None])
    return jnp.einsum('bthd,th,bthe->bhde', k, w, v)


def retention_mixer(q, k, v, g, L, pos, log_gamma, need_ctx):
    B, n_tok, _ = q.shape
    f32 = jnp.float32

    def heads(t):
        return t.astype(f32).reshape(B, n_tok, RET_HEADS, HEAD_DIM)

    q, k, v = heads(q), heads(k) * HEAD_DIM ** -0.5, heads(v)
    qz, qx = q[:, :L], rope(q[:, L:], pos)
    kz, kx = k[:, :L], rope(k[:, L:], pos)
    vz, vx = v[:, :L], v[:, L:]
    lg_f, lg_b = log_gamma[0].astype(f32), log_gamma[1].astype(f32)

    def flip(t):
        return jnp.flip(t, axis=1)

    if need_ctx:
        zeros = jnp.zeros((B, RET_HEADS, HEAD_DIM, HEAD_DIM), f32)
        oz_f, s_f = retention_chunkwise(qz, kz, vz, lg_f, zeros, True)
        oz_b, s_b = retention_chunkwise(flip(qz), flip(kz), flip(vz), lg_b, zeros, False)
    else:
        s_f = retention_final_state(kz, vz, lg_f)
        s_b = retention_final_state(flip(kz), flip(vz), lg_b)
    ox_f, _ = retention_chunkwise(qx, kx, vx, lg_f, s_f, True)
    ox_b, _ = retention_chunkwise(flip(qx), flip(kx), flip(vx), lg_b, s_b, False)
    o = ox_f + flip(ox_b)
    g_sel = g[:, L:]
    if need_ctx:
        o = jnp.concatenate([oz_f + flip(oz_b), o], axis=1)
        g_sel = g
    o = layer_norm(o).reshape(B, -1, RET_WIDTH).astype(g.dtype)
    return o * jax.nn.silu(g_sel)


def hybrid_mixer(hx, hz, rows, cols, pos, w_in, lam_re, lam_im, log_step, b_re, b_im, c_re, c_im,
                 d_skip, w_glu, b_glu, sink, log_gamma, w_out, need_ctx):
    L = hz.shape[1]
    p = jnp.concatenate([hz, hx], axis=1) @ w_in
    splits = [int(i) for i in np.cumsum(IN_SIZES)[:-1]]
    u, qa, ka, va, qr, kr, vr, gr = jnp.split(p, splits, axis=-1)
    y_s5 = s5_mixer(u, L, lam_re, lam_im, log_step, b_re, b_im, c_re, c_im, d_skip, w_glu, b_glu, need_ctx)
    y_att = window_attention_mixer(qa, ka, va, L, rows, cols, sink, need_ctx)
    y_ret = retention_mixer(qr, kr, vr, gr, L, pos, log_gamma, need_ctx)
    out = jnp.concatenate([y_s5, y_att, y_ret], axis=-1) @ w_out
    if need_ctx:
        return out[:, L:], out[:, :L]
    return out, None


def swiglu(h, w1, w3, w2):
    return (jax.nn.silu(h @ w1) * (h @ w3)) @ w2


def moe_swiglu(h, router, w1, w3, w2):
    logits = (h @ router).astype(jnp.float32)
    top_val, top_idx = lax.top_k(logits, TOP_K)
    top_w = jax.nn.softmax(top_val, axis=-1)
    gates = jnp.sum(jax.nn.one_hot(top_idx, N_EXPERTS, dtype=jnp.float32) * top_w[..., None], axis=-2)
    gates = gates.astype(h.dtype)
    out = gates[..., 0:1] * swiglu(h, w1[0], w3[0], w2[0])
    for e in range(1, N_EXPERTS):
        out = out + gates[..., e:e + 1] * swiglu(h, w1[e], w3[e], w2[e])
    return out


def channel_mixer(h, l, ffn_w1, ffn_w3, ffn_w2, moe_router, moe_w1, moe_w3, moe_w2):
    i = l // 2
    if l % 2 == 0:
        return swiglu(h, ffn_w1[i], ffn_w3[i], ffn_w2[i])
    return moe_swiglu(h, moe_router[i], moe_w1[i], moe_w3[i], moe_w2[i])


def setup_inputs(seed: int = 0) -> dict:
    key = jax.random.key(seed)
    ks = jax.random.split(key, 32)
    f32 = jnp.float32

    def nrm(k, shape, scale):
        return jax.random.normal(k, shape, f32) * scale

    s5_shape = (DEPTH, 2, S5_GROUPS, S5_STATE)
    n_idx = jnp.arange(S5_STATE, dtype=f32)
    ret_base = jnp.log(1.0 - 2.0 ** (-5.0 - jnp.arange(RET_HEADS, dtype=f32)))
    return {
        'x': nrm(ks[0], (BATCH, SEQ, D_MODEL), 1.0),
        'c': nrm(ks[1], (BATCH, D_MODEL), 1.0),
        'ctx': nrm(ks[2], (BATCH, CTX_LEN, D_MODEL), 1.0),
        'c_ctx': nrm(ks[3], (D_MODEL,), 1.0),
        'w_mod': nrm(ks[4], (DEPTH, D_MODEL, 6 * D_MODEL), 0.5 * D_MODEL ** -0.5),
        'b_mod': nrm(ks[5], (DEPTH, 6 * D_MODEL), 0.02),
        'w_in': nrm(ks[6], (DEPTH, D_MODEL, IN_WIDTH), D_MODEL ** -0.5),
        's5_lam_re': -0.5 + nrm(ks[7], s5_shape, 0.01),
        's5_lam_im': math.pi * n_idx + nrm(ks[8], s5_shape, 0.01),
        's5_log_step': jax.random.uniform(ks[9], (DEPTH, 2, S5_GROUPS), f32, math.log(0.001), math.log(0.1)),
        's5_b_re': nrm(ks[10], (DEPTH, 2, S5_GROUPS, S5_STATE, S5_GROUP), (2 * S5_GROUP) ** -0.5),
        's5_b_im': nrm(ks[11], (DEPTH, 2, S5_GROUPS, S5_STATE, S5_GROUP), (2 * S5_GROUP) ** -0.5),
        's5_c_re': nrm(ks[12], (DEPTH, 2, S5_GROUPS, S5_GROUP, S5_STATE), (2 * S5_STATE) ** -0.5),
        's5_c_im': nrm(ks[13], (DEPTH, 2, S5_GROUPS, S5_GROUP, S5_STATE), (2 * S5_STATE) ** -0.5),
        's5_d': nrm(ks[14], (DEPTH, S5_WIDTH), 1.0),
        's5_w_glu': nrm(ks[15], (DEPTH, S5_WIDTH, S5_WIDTH), S5_WIDTH ** -0.5),
        's5_b_glu': nrm(ks[16], (DEPTH, S5_WIDTH), 0.02),
        'attn_sink': nrm(ks[17], (DEPTH, ATT_HEADS), 0.5),
        'ret_log_gamma': ret_base * (1.0 + nrm(ks[18], (DEPTH, 2, RET_HEADS), 0.05)),
        'w_out': nrm(ks[19], (DEPTH, MIX_WIDTH, D_MODEL), BETA * MIX_WIDTH ** -0.5),
        'ln1_g': 1.0 + nrm(ks[20], (DEPTH, D_MODEL), 0.02),
        'ln1_b': nrm(ks[21], (DEPTH, D_MODEL), 0.02),
        'ln2_g': 1.0 + nrm(ks[22], (DEPTH, D_MODEL), 0.02),
        'ln2_b': nrm(ks[23], (DEPTH, D_MODEL), 0.02),
        'ffn_w1': nrm(ks[24], (N_DENSE_LAYERS, D_MODEL, D_FF), D_MODEL ** -0.5),
        'ffn_w3': nrm(ks[25], (N_DENSE_LAYERS, D_MODEL, D_FF), D_MODEL ** -0.5),
        'ffn_w2': nrm(ks[26], (N_DENSE_LAYERS, D_FF, D_MODEL), BETA * D_FF ** -0.5),
        'moe_router': nrm(ks[27], (N_MOE_LAYERS, D_MODEL, N_EXPERTS), D_MODEL ** -0.5),
        'moe_w1': nrm(ks[28], (N_MOE_LAYERS, N_EXPERTS, D_MODEL, D_FF_EXPERT), D_MODEL ** -0.5),
        'moe_w3': nrm(ks[29], (N_MOE_LAYERS, N_EXPERTS, D_MODEL, D_FF_EXPERT), D_MODEL ** -0.5),
        'moe_w2': nrm(ks[30], (N_MOE_LAYERS, N_EXPERTS, D_FF_EXPERT, D_MODEL), BETA * D_FF_EXPERT ** -0.5),
    }


def reference(x, c, ctx, c_ctx, w_mod, b_mod, w_in, s5_lam_re, s5_lam_im, s5_log_step, s5_b_re, s5_b_im,
              s5_c_re, s5_c_im, s5_d, s5_w_glu, s5_b_glu, attn_sink, ret_log_gamma, w_out,
              ln1_g, ln1_b, ln2_g, ln2_b, ffn_w1, ffn_w3, ffn_w2, moe_router, moe_w1, moe_w3, moe_w2):
    B, T, D = x.shape
    L = ctx.shape[1]
    ROWS = T // GRID_W
    t = jnp.arange(ROWS * GRID_W)
    rows = (t // GRID_W).astype(jnp.float32)
    cols = (t % GRID_W).astype(jnp.float32)
    pos = t.astype(jnp.float32)
    z = ctx
    for l in range(DEPTH):
        need_ctx = l < DEPTH - 1
        mod_x = (jax.nn.silu(c) @ w_mod[l] + b_mod[l]).reshape(B, 6, 1, D)
        mod_z = (jax.nn.silu(c_ctx) @ w_mod[l] + b_mod[l]).reshape(6, 1, 1, D)
        hx = modulate(x, mod_x[:, 0], mod_x[:, 1])
        hz = modulate(z, mod_z[0], mod_z[1])
        mix_x, mix_z = hybrid_mixer(hx, hz, rows, cols, pos, w_in[l], s5_lam_re[l], s5_lam_im[l],
                                    s5_log_step[l], s5_b_re[l], s5_b_im[l], s5_c_re[l], s5_c_im[l],
                                    s5_d[l], s5_w_glu[l], s5_b_glu[l], attn_sink[l], ret_log_gamma[l],
                                    w_out[l], need_ctx)
        x = post_norm(x, mod_x[:, 2] * mix_x, ln1_g[l], ln1_b[l])
        fx = modulate(x, mod_x[:, 3], mod_x[:, 4])
        if need_ctx:
            z = post_norm(z, mod_z[2] * mix_z, ln1_g[l], ln1_b[l])
            fz = modulate(z, mod_z[3], mod_z[4])
            f = channel_mixer(jnp.concatenate([fz, fx], axis=1), l, ffn_w1, ffn_w3, ffn_w2,
                              moe_router, moe_w1, moe_w3, moe_w2)
            z = post_norm(z, mod_z[5] * f[:, :L], ln2_g[l], ln2_b[l])
            fx_out = f[:, L:]
        else:
            fx_out = channel_mixer(fx, l, ffn_w1, ffn_w3, ffn_w2, moe_router, moe_w1, moe_w3, moe_w2)
        x = post_norm(x, mod_x[:, 5] * fx_out, ln2_g[l], ln2_b[l])
    return x
```

```python
import contextlib
import math
import numpy as np
import concourse.bass as bass
import concourse.mybir as mybir
from concourse.bass_utils import run_bass_kernel_spmd

F32 = mybir.dt.float32
BF16 = mybir.dt.bfloat16
AF = mybir.ActivationFunctionType
ALU = mybir.AluOpType
AX = mybir.AxisListType

D = 1024
NB = 2
LCTX = 256
TSEQ = 2048
TPB = LCTX + TSEQ
NTOK = NB * TPB
TILES_PB = TPB // 128
NTILES = NTOK // 128
DEPTH = 2
DFF = 2816
NEXP = 8
DFFE = 3584
ALPHA = (2 * DEPTH) ** 0.25
LN_EPS = 1e-5
NCORES = 8
WINC = 3456
TWO_PI = 2.0 * math.pi
S5NJ = 39

ENGS = ("pe", "act", "dve", "pool", "sp")
NDMA = {"sp": 24, "pool": 16, "act": 8}


class Buf:
    __slots__ = ("ap", "lastw", "readers", "name")

    def __init__(self, ap, name=""):
        self.ap = ap
        self.lastw = None
        self.readers = []
        self.name = name

    def __getitem__(self, k):
        return self.ap[k]


class _Rec:
    def __init__(self):
        self.calls = []

    def __getattr__(self, name):
        def f(*a, **k):
            self.calls.append((name, a, k))
            return None
        return f


class KB:
    def __init__(self, nc):
        self.nc = nc
        self.es = contextlib.ExitStack()
        self.base_es = self.es
        self.ops = {e: [] for e in ENGS}
        self.cnt = {e: 0 for e in ENGS}
        self.sem = {}
        for e in ("pe", "act", "dve", "pool"):
            self.sem[e] = self.es.enter_context(nc.semaphore("s_" + e))
        self.dsem = {}
        for q, n in NDMA.items():
            self.dsem[q] = [[self.es.enter_context(nc.semaphore(f"d_{q}{i}")), 0] for i in range(n)]
        self.drr = {q: 0 for q in NDMA}
        self.waited = {e: {} for e in ENGS}
        self.dram_bufs = {}
        self.uid = 0

    def sb(self, name, shape, dtype):
        self.uid += 1
        t = self.es.enter_context(self.nc.sbuf_tensor(f"{name}_{self.uid}", list(shape), dtype))
        return Buf(t, name)

    def ps(self, name, shape, dtype=F32):
        self.uid += 1
        t = self.es.enter_context(self.nc.psum_tensor(f"{name}_{self.uid}", list(shape), dtype))
        return Buf(t, name)

    def dbuf(self, key):
        b = self.dram_bufs.get(key)
        if b is None:
            b = Buf(None, str(key))
            self.dram_bufs[key] = b
        return b

    @contextlib.contextmanager
    def phase(self):
        old = self.es
        with contextlib.ExitStack() as es:
            self.es = es
            yield
            self.es = old
        self.barrier()

    def _deps(self, eng, reads, writes):
        toks = []
        for b in reads:
            if b.lastw is not None:
                toks.append(b.lastw)
        for b in writes:
            if b.lastw is not None:
                toks.append(b.lastw)
            toks.extend(b.readers)
        need = {}
        for (kind, s, v, src) in toks:
            if kind == "eng" and src == eng and eng == "pe":
                continue
            k = id(s)
            if k not in need or need[k][1] < v:
                need[k] = (s, v)
        out = []
        w = self.waited[eng]
        for k, (s, v) in need.items():
            if w.get(k, 0) >= v:
                continue
            w[k] = v
            out.append((s, v))
        return out

    def _commit(self, tok, reads, writes):
        for b in reads:
            b.readers.append(tok)
            if len(b.readers) > 96:
                b.readers = b.readers[-64:]
        for b in writes:
            b.lastw = tok
            b.readers = []

    def op(self, eng, fn, reads=(), writes=()):
        waits = self._deps(eng, reads, writes)
        self.cnt[eng] += 1
        tok = ("eng", self.sem[eng], self.cnt[eng], eng)
        rec = _Rec()
        fn(rec)
        assert rec.calls
        self.ops[eng].append((waits, rec.calls, (self.sem[eng], 1)))
        self._commit(tok, reads, writes)
        return tok

    def dma(self, q, out, in_, reads=(), writes=(), **kw):
        pool = self.dsem[q]
        i = self.drr[q]
        self.drr[q] = (i + 1) % len(pool)
        ent = pool[i]
        waits = self._deps(q, reads, writes)
        w = self.waited[q]
        if ent[1] > 0 and w.get(id(ent[0]), 0) < ent[1]:
            w[id(ent[0])] = ent[1]
            waits.append((ent[0], ent[1]))
        ent[1] += 16
        tok = ("dma", ent[0], ent[1], q)
        kw2 = dict(kw)
        kw2["out"] = out
        kw2["in_"] = in_
        self.ops[q].append((waits, [("dma_start", (), kw2)], (ent[0], 16)))
        self._commit(tok, reads, writes)
        return tok

    def barrier(self):
        targets = []
        for e in ("pe", "act", "dve", "pool"):
            if self.cnt[e] > 0:
                targets.append((self.sem[e], self.cnt[e]))
        for q in NDMA:
            for ent in self.dsem[q]:
                if ent[1] > 0:
                    targets.append((ent[0], ent[1]))
        for e in ENGS:
            w = self.waited[e]
            waits = []
            for (s, v) in targets:
                if w.get(id(s), 0) < v:
                    w[id(s)] = v
                    waits.append((s, v))
            if waits:
                self.ops[e].append((waits, None, None))

    def emit(self):
        nc = self.nc
        with nc.Block() as block:
            def run(e, h):
                for (waits, fn, inc) in self.ops[e]:
                    for (s, v) in waits:
                        h.wait_ge(s, v)
                    if fn is not None:
                        for (m_, a_, k_) in fn:
                            ins = getattr(h, m_)(*a_, **k_)
                        ins.then_inc(inc[0], inc[1])

            @block.tensor
            def _(h):
                run("pe", h)

            @block.scalar
            def _(h):
                run("act", h)

            @block.vector
            def _(h):
                run("dve", h)

            @block.gpsimd
            def _(h):
                run("pool", h)

            @block.sync
            def _(h):
                run("sp", h)
        self.base_es.close()


def _att_partner_perm():
    d = np.arange(64)
    return np.where((d % 32) < 16, d + 16, d - 16)


def _ret_partner_perm():
    d = np.arange(64)
    return np.where(d < 32, d + 32, d - 32)


def win_columns():
    pa = _att_partner_perm()
    pr = _ret_partner_perm()
    cols = []
    cols += list(range(0, 256))
    for j in range(4):
        cols += [256 + j * 64 + d for d in range(64)] + [256 + (4 + j) * 64 + d for d in range(64)]
    for j in range(4):
        cols += [256 + j * 64 + pa[d] for d in range(64)] + [256 + (4 + j) * 64 + pa[d] for d in range(64)]
    cols += [768 + g * 64 + d for g in range(2) for d in range(64)]
    cols += [768 + g * 64 + pa[d] for g in range(2) for d in range(64)]
    cols += [1024 + h * 64 + d for h in range(4) for d in range(64)]
    cols += [1024 + h * 64 + pr[d] for h in range(4) for d in range(64)]
    cols += [1280 + h * 64 + d for h in range(4) for d in range(64)]
    cols += [1280 + h * 64 + pr[d] for h in range(4) for d in range(64)]
    cols += list(range(896, 1024))
    cols += list(range(1536, 1792))
    cols += list(range(1792, 2048))
    cols += list(range(1280, 1536))
    assert len(cols) == WINC
    return np.asarray(cols)


def rope_tables():
    t = np.arange(TSEQ)
    rows = (t // 64).astype(np.float32)
    colsp = (t % 64).astype(np.float32)
    pos = t.astype(np.float32)
    f16 = (10000.0 ** (-np.arange(0, 32, 2, dtype=np.float32) / 32)).astype(np.float32)
    f32_ = (10000.0 ** (-np.arange(0, 64, 2, dtype=np.float32) / 64)).astype(np.float32)
    d = np.arange(64)
    ang = np.where((d < 32)[:, None], rows[None, :] * f16[(d % 16)][:, None], colsp[None, :] * f16[(d % 16)][:, None]).astype(np.float32)
    sgn = np.where((d % 32) < 16, -1.0, 1.0).astype(np.float32)[:, None]
    att_cos = np.ones((64, TPB), np.float32)
    att_sin = np.zeros((64, TPB), np.float32)
    att_cos[:, LCTX:] = np.cos(ang)
    att_sin[:, LCTX:] = np.sin(ang) * sgn
    angr = (pos[None, :] * f32_[(d % 32)][:, None]).astype(np.float32)
    sgr = np.where(d < 32, -1.0, 1.0).astype(np.float32)[:, None]
    ret_cos = np.ones((64, TPB), np.float32)
    ret_sin = np.zeros((64, TPB), np.float32)
    ret_cos[:, LCTX:] = np.cos(angr)
    ret_sin[:, LCTX:] = np.sin(angr) * sgr
    tab = np.stack([np.tile(att_cos, (2, 1)), np.tile(att_sin, (2, 1)),
                    np.tile(ret_cos, (2, 1)), np.tile(ret_sin, (2, 1))], axis=1)
    tm = np.zeros((TPB, 2, 32), np.float32)
    tm[:, 0, :] = 1.0
    a2 = pos[:, None] * f32_[None, :]
    tm[LCTX:, 0, :] = np.cos(a2)
    tm[LCTX:, 1, :] = np.sin(a2)
    tm = tm.reshape(TILES_PB, 128, 2, 1, 32).transpose(1, 0, 2, 3, 4)
    tm = np.broadcast_to(tm, (128, TILES_PB, 2, 4, 32))
    return np.ascontiguousarray(tab), np.ascontiguousarray(tm)


def tile_info(tt):
    b = tt // TILES_PB
    r = tt % TILES_PB
    is_ctx = r < 2
    midx = 2 if is_ctx else b
    return b, r, is_ctx, midx


TOKEN_GROUPS = []
for _b in range(NB):
    for _s in (0, 4, 8, 12, 16):
        TOKEN_GROUPS.append(list(range(_b * TILES_PB + _s, _b * TILES_PB + min(_s + 4, TILES_PB))))


def build(cfg=None):
    cfg = cfg or {}
    dump = set(cfg.get("dump", ()))
    layers = cfg.get("layers", list(range(DEPTH)))
    stop_after = cfg.get("stop_after", None)
    nc = bass.Bass("TRN2", target_bir_lowering=False)

    def din(name, shape, dt=F32):
        return nc.dram_tensor(name, list(shape), dt, kind="ExternalInput").ap()

    def dscr(name, shape, dt):
        kind = "ExternalOutput" if name in dump else "Internal"
        return nc.dram_tensor(name, list(shape), dt, kind=kind).ap()

    XZ = din("xz", [NTOK, D])
    CT = din("cT", [128, 8, 3])
    WMOD = din("w_mod", [DEPTH, D, 6 * D])
    BMOD = din("b_mod", [DEPTH, 6 * D])
    WIN = din("w_in_p", [DEPTH, D, WINC])
    ROPE = din("rope_tab", [128, 4, TPB])
    ROPETM = din("rope_tm", [128, TILES_PB, 2, 4, 32])
    IDENT = din("ident", [128, 128])
    AMASK = din("att_mask", [128, 2, 4, 128])
    SINK = din("attn_sink", [DEPTH, 8])
    RD12 = din("ret_d12", [128, 2, 128])
    RJC = din("ret_jc", [128, 2])
    RIROW = din("ret_irow", [128, 2, 128])
    LGB = din("ret_lgb", [DEPTH, 128, 8])
    LGP = din("ret_lgp", [DEPTH, 128, 2, 2])
    S5P = din("s5_par", [DEPTH, 128, 3, 16])
    S5B = din("s5_b", [DEPTH, 128, 2, 16, 16])
    S5C = din("s5_c", [DEPTH, 128, 2, 16, 16])
    S5JT = din("s5_jt", [128, S5NJ, 16])
    S5D = din("s5_dcol", [DEPTH, 128, 2])
    WGLU = din("s5_w_glu", [DEPTH, 256, 256])
    BGLU = din("s5_bglu", [DEPTH, 128, 2])
    WOUT = din("w_out", [DEPTH, D, D])
    LNG = din("ln_gb", [DEPTH, 4, D])
    FW1 = din("ffn_w1", [1, D, DFF])
    FW3 = din("ffn_w3", [1, D, DFF])
    FW2 = din("ffn_w2", [1, DFF, D])
    ROUT = din("moe_router", [1, D, NEXP])
    if cfg.get("small_moe"):
        MW1 = MW3 = MW2 = None
    else:
        MW1 = din("moe_w1", [1, NEXP, D, DFFE])
        MW3 = din("moe_w3", [1, NEXP, D, DFFE])
        MW2 = din("moe_w2", [1, NEXP, DFFE, D])
    OUT = nc.dram_tensor("out", [NB * TSEQ, D], F32, kind="ExternalOutput").ap()

    XS = dscr("XS", [NTOK, D], F32)
    MODR = dscr("MODR", [DEPTH, 3, 6 * D], F32)
    PF = dscr("PF", [11, 128, NTOK], BF16)
    PT = dscr("PT", [NTOK, 896], BF16)
    YA = dscr("YA", [8, 64, NTOK], BF16)
    YR = dscr("YR", [2, 128, NTOK], BF16)
    YS = dscr("YS", [2, 128, NTOK], BF16)

    kb = KB(nc)

    ident = kb.sb("ident", [128, 128], BF16)
    kb.dma("pool", ident[:], IDENT[:, :], writes=[ident])
    identf = kb.sb("identf", [128, 128], F32)
    kb.dma("sp", identf[:], IDENT[:, :], writes=[identf])
    epsc = kb.sb("epsc", [128, 1], F32)
    kb.op("dve", lambda e: e.memset(epsc[:], LN_EPS), writes=[epsc])

    with kb.phase():
        ct = kb.sb("ct", [128, 8, 3], F32)
        kb.dma("sp", ct[:], CT[:, :, :], writes=[ct])
        silT = kb.sb("silT", [128, 8, 3], BF16)
        kb.op("act", lambda e: e.activation(out=silT[:], in_=ct[:], func=AF.Silu), reads=[ct], writes=[silT])
        wm = [kb.sb(f"wm{i}", [128, 8, 512], BF16) for i in range(2)]
        bm = kb.sb("bm", [3, 6 * D], F32)
        modrow = kb.sb("modrow", [3, 6 * D], F32)
        pm = [kb.ps(f"pm{i}", [3, 512]) for i in range(2)]
        for l in range(DEPTH):
            kb.dma("sp", bm[:].unsqueeze(1), BMOD[l:l + 1, :].partition_broadcast(3), writes=[bm])
            for cb in range(12):
                w = wm[cb % 2]
                p = pm[cb % 2]
                kb.dma("pool", w[:], WMOD[l].rearrange("(k p) n -> p k n", p=128)[:, :, cb * 512:(cb + 1) * 512], writes=[w])

                def mm(e, w=w, p=p):
                    for k in range(8):
                        ins = e.matmul(p[:], silT[:, k, :], w[:, k, :], start=(k == 0), stop=(k == 7))
                    return ins
                kb.op("pe", mm, reads=[w, silT], writes=[p])
                kb.op("dve", lambda e, p=p, cb=cb: e.tensor_tensor(out=modrow[:, cb * 512:(cb + 1) * 512], in0=p[:], in1=bm[:, cb * 512:(cb + 1) * 512], op=ALU.add),
                      reads=[p, bm], writes=[modrow])
            kb.dma("sp", MODR[l], modrow[:], reads=[modrow], writes=[kb.dbuf(("MODR", l))])

    def load_modT(l, modT):
        for i in range(3):
            kb.dma("sp", modT[:, :, i], MODR[l, i].rearrange("(c p) -> p c", p=128), reads=[kb.dbuf(("MODR", l))], writes=[modT],
                   allow_slow_non_contiguous=True)
        for c0 in (8, 32):
            kb.op("dve", lambda e, c0=c0: e.tensor_scalar(out=modT[:, c0:c0 + 8, :], in0=modT[:, c0:c0 + 8, :], scalar1=1.0, scalar2=None, op0=ALU.add),
                  reads=[modT], writes=[modT])

    def ln_tile(xt_ap, xt_buf, xn_ap, xn_buf, st, mv, rs):
        for h in range(2):
            kb.op("dve", lambda e, h=h: e.bn_stats(out=st[:, h, :], in_=xt_ap[:, h * 512:(h + 1) * 512]), reads=[xt_buf], writes=[st])
        kb.op("dve", lambda e: e.bn_aggr(out=mv[:], in_=st[:].rearrange("p a b -> p (a b)")), reads=[st], writes=[mv])
        kb.op("act", lambda e: e.activation(out=rs[:, 0:1], in_=mv[:, 1:2], func=AF.Sqrt, bias=epsc[:], scale=1.0), reads=[mv, epsc], writes=[rs])
        kb.op("dve", lambda e: e.reciprocal(out=rs[:, 1:2], in_=rs[:, 0:1]), reads=[rs], writes=[rs])
        kb.op("dve", lambda e: e.tensor_scalar(out=xn_ap, in0=xt_ap, scalar1=mv[:, 0:1], scalar2=rs[:, 1:2], op0=ALU.subtract, op1=ALU.mult),
              reads=[xt_buf, mv, rs], writes=[xn_buf])

    def phase2(l, SRC, src_key):
        with kb.phase():
            modT = kb.sb("modT", [128, 48, 3], F32)
            load_modT(l, modT)
            win = kb.sb("win", [128, 8, WINC], BF16)
            for k in range(8):
                kb.dma("pool", win[:, k, :], WIN[l, k * 128:(k + 1) * 128, :], writes=[win])
            rope = kb.sb("rope", [128, 4, TPB], F32)
            kb.dma("sp", rope[:], ROPE[:, :, :], writes=[rope])
            ropetm = kb.sb("ropetm", [128, TILES_PB, 2, 4, 32], F32)
            kb.dma("sp", ropetm[:], ROPETM[:, :, :, :, :], writes=[ropetm])
            xt = [kb.sb(f"xt{i}", [128, 4, D], F32) for i in range(2)]
            xn = [kb.sb(f"xn{i}", [128, D], BF16) for i in range(2)]
            st = kb.sb("st", [128, 2, 6], F32)
            mv = kb.sb("mv", [128, 2], F32)
            rs = kb.sb("rs", [128, 2], F32)
            tmpm = [kb.sb(f"tmpm{i}", [128, 8, 128], F32) for i in range(2)]
            hT = [kb.sb(f"hT{i}", [128, 8, 512], BF16) for i in range(2)]
            ptr = [kb.ps(f"ptr{i}", [128, 8, 128], BF16) for i in range(2)]
            pf = [kb.ps(f"pf{i}", [128, 512]) for i in range(4)]
            ptk = [kb.ps(f"ptk{i}", [128, 512]) for i in range(2)]
            t1 = [kb.sb(f"t1_{i}", [128, 512], F32) for i in range(2)]
            t2 = [kb.sb(f"t2_{i}", [128, 512], F32) for i in range(2)]
            stf = [kb.sb(f"stf{i}", [128, 512], BF16) for i in range(4)]
            stt = [kb.sb(f"stt{i}", [128, 896], BF16) for i in range(2)]
            kt = [kb.sb(f"kt{i}", [128, 4, 4, 32], F32) for i in range(2)]
            kcp = [kb.sb(f"kcp{i}", [128, 256], F32) for i in range(2)]
            SRCv = SRC.rearrange("(t p) d -> p t d", p=128)
            nfe = 0
            lim = cfg.get("p2_lim", 99)
            groups2 = TOKEN_GROUPS[:cfg.get("p2_groups", 99)]

            def load_x2(gi_):
                grp_ = groups2[gi_]
                kb.dma("sp", xt[gi_ % 2][:, 0:len(grp_), :], SRCv[:, grp_[0]:grp_[0] + len(grp_), :], reads=[kb.dbuf((src_key, g)) for g in grp_], writes=[xt[gi_ % 2]])
            load_x2(0)
            for gi, grp in enumerate(groups2):
                ng = len(grp)
                ncol = ng * 128
                x = xt[gi % 2]
                h = hT[gi % 2]
                if gi + 1 < len(groups2):
                    load_x2(gi + 1)
                b0, r0, _, _ = tile_info(grp[0])
                if lim < 2:
                    continue
                for i, tt in enumerate(grp):
                    b, r, is_ctx, midx = tile_info(tt)
                    xnb = xn[tt % 2]
                    ln_tile(x[:, i, :], x, xnb[:], xnb, st, mv, rs)
                    if lim < 3:
                        continue
                    p = ptr[tt % 2]

                    def tr(e, xnb=xnb, p=p):
                        for k in range(8):
                            ins = e.transpose(p[:, k, :], xnb[:, k * 128:(k + 1) * 128], ident[:])
                        return ins
                    kb.op("pe", tr, reads=[xnb, ident], writes=[p])
                    tm_ = tmpm[tt % 2]
                    kb.op("dve", lambda e, p=p, tm_=tm_, midx=midx: e.tensor_tensor(out=tm_[:], in0=p[:], in1=modT[:, 8:16, midx].unsqueeze(2).to_broadcast([128, 8, 128]), op=ALU.mult),
                          reads=[p, modT], writes=[tm_])
                    kb.op("pool", lambda e, tm_=tm_, h=h, i=i, midx=midx: e.tensor_tensor(out=h[:, :, i * 128:(i + 1) * 128], in0=tm_[:], in1=modT[:, 0:8, midx].unsqueeze(2).to_broadcast([128, 8, 128]), op=ALU.add),
                          reads=[tm_, modT], writes=[h])
                if lim < 4:
                    continue
                c0 = r0 * 128
                col0 = grp[0] * 128

                def proj(e, p, ci, h=h, ncol=ncol):
                    for k in range(8):
                        ins = e.matmul(p[:, 0:ncol], win[:, k, ci * 128:(ci + 1) * 128], h[:, k, 0:ncol], start=(k == 0), stop=(k == 7))
                    return ins
                for ci in (0, 1):
                    p = pf[nfe % 4]
                    s = stf[nfe % 4]
                    nfe += 1
                    kb.op("pe", lambda e, p=p, ci=ci, f=proj: f(e, p, ci), reads=[win, h], writes=[p])
                    kb.op("act", lambda e, p=p, s=s, ncol=ncol: e.activation(out=s[:, 0:ncol], in_=p[:, 0:ncol], func=AF.Copy), reads=[p], writes=[s])
                    kb.dma("sp", PF[ci, :, col0:col0 + ncol], s[:, 0:ncol], reads=[s], writes=[kb.dbuf(("PF", ci, gi))])
                pairs = [(2 + j, 6 + j, 2 + j, 0) for j in range(4)] + [(10, 11, 6, 0)] + \
                        [(12 + j, 14 + j, 7 + j, 2) for j in range(2)] + [(16 + j, 18 + j, 9 + j, 2) for j in range(2)]
                for (ca, cr, oi, tb) in pairs:
                    pa = pf[nfe % 4]
                    pr_ = pf[(nfe + 1) % 4]
                    s = stf[nfe % 4]
                    a1 = t1[(nfe // 2) % 2]
                    a2 = t2[(nfe // 2) % 2]
                    nfe += 2
                    kb.op("pe", lambda e, p=pa, ci=ca, f=proj: f(e, p, ci), reads=[win, h], writes=[pa])
                    kb.op("pe", lambda e, p=pr_, ci=cr, f=proj: f(e, p, ci), reads=[win, h], writes=[pr_])
                    kb.op("dve", lambda e, pa=pa, a1=a1, tb=tb, ncol=ncol, c0=c0: e.tensor_tensor(out=a1[:, 0:ncol], in0=pa[:, 0:ncol], in1=rope[:, tb, c0:c0 + ncol], op=ALU.mult),
                          reads=[pa, rope], writes=[a1])
                    kb.op("dve", lambda e, pr_=pr_, a2=a2, tb=tb, ncol=ncol, c0=c0: e.tensor_tensor(out=a2[:, 0:ncol], in0=pr_[:, 0:ncol], in1=rope[:, tb + 1, c0:c0 + ncol], op=ALU.mult),
                          reads=[pr_, rope], writes=[a2])
                    kb.op("pool", lambda e, a1=a1, a2=a2, s=s, ncol=ncol: e.tensor_tensor(out=s[:, 0:ncol], in0=a1[:, 0:ncol], in1=a2[:, 0:ncol], op=ALU.add),
                          reads=[a1, a2], writes=[s])
                    kb.dma("sp", PF[oi, :, col0:col0 + ncol], s[:, 0:ncol], reads=[s], writes=[kb.dbuf(("PF", oi, gi))])
                if lim < 5:
                    continue
                for i, tt in enumerate(grp):
                    b, r, is_ctx, midx = tile_info(tt)
                    p1 = ptk[0]
                    p2 = ptk[1]
                    s = stt[tt % 2]
                    k_ = kt[tt % 2]

                    def tproj(e, p, c_lo, n, i=i, h=h):
                        for k in range(8):
                            ins = e.matmul(p[:, 0:n], h[:, k, i * 128:(i + 1) * 128], win[:, k, c_lo:c_lo + n], start=(k == 0), stop=(k == 7))
                        return ins
                    kb.op("pe", lambda e, p1=p1, f=tproj: f(e, p1, 2560, 512), reads=[win, h], writes=[p1])
                    kb.op("pe", lambda e, p2=p2, f=tproj: f(e, p2, 3072, 384), reads=[win, h], writes=[p2])
                    kb.op("act", lambda e, p1=p1, s=s: e.activation(out=s[:, 0:512], in_=p1[:], func=AF.Copy), reads=[p1], writes=[s])
                    kb.op("act", lambda e, p2=p2, s=s: e.activation(out=s[:, 512:640], in_=p2[:, 0:128], func=AF.Copy), reads=[p2], writes=[s])
                    tml = cfg.get("tm_lim", 99)
                    if tml < 2:
                        continue
                    kc_ = kcp[tt % 2]
                    kb.op("act", lambda e, p2=p2, kc_=kc_: e.activation(out=kc_[:], in_=p2[:, 128:384], func=AF.Copy), reads=[p2], writes=[kc_])
                    kv = kc_[:].rearrange("p (h a f) -> p h a f", h=4, a=2)
                    cosb = ropetm[:, r, 0, :, :]
                    if cfg.get('dbgA'):
                        cosb = rope[:, 0, 0:128].rearrange('p (h f) -> p h f', h=4)
                    sinb = ropetm[:, r, 1, :, :]
                    for j, (src_a, tb_) in enumerate(((0, cosb), (1, sinb), (0, sinb), (1, cosb))[:cfg.get('tmj', 4)]):
                        kb.op("dve", lambda e, j=j, src_a=src_a, tb_=tb_, k_=k_, kv=kv: e.tensor_tensor(out=k_[:, :, j, :], in0=kv[:, :, src_a, :], in1=tb_, op=ALU.mult),
                              reads=[kc_, ropetm], writes=[k_])
                    if tml < 3:
                        continue
                    so = s[:, 640:896].rearrange("p (h a f) -> p h a f", h=4, a=2)
                    kb.op("pool", lambda e, k_=k_, so=so: e.tensor_tensor(out=so[:, :, 0, :], in0=k_[:, :, 0, :], in1=k_[:, :, 1, :], op=ALU.subtract), reads=[k_], writes=[s])
                    kb.op("pool", lambda e, k_=k_, so=so: e.tensor_tensor(out=so[:, :, 1, :], in0=k_[:, :, 2, :], in1=k_[:, :, 3, :], op=ALU.add), reads=[k_], writes=[s])
                    if tml < 4:
                        continue
                    kb.dma("sp", PT[tt * 128:(tt + 1) * 128, :], s[:], reads=[s], writes=[kb.dbuf(("PT", tt))])


    def pf_reads(ci):
        return [kb.dbuf(("PF", ci, gi)) for gi in range(len(TOKEN_GROUPS))]

    def pt_reads(tiles):
        return [kb.dbuf(("PT", tt)) for tt in tiles]

    def phase3(l, need_ctx):
        with kb.phase():
            amask = kb.sb("amask", [128, 2, 4, 128], BF16)
            kb.dma("pool", amask[:], AMASK[:, :, :, :], writes=[amask])
            ones64 = kb.sb("ones64", [128, 64], BF16)
            kb.op("dve", lambda e: e.memset(ones64[:], 1.0), writes=[ones64])
            sk = kb.sb("sk", [1, 8], F32)
            kb.dma("sp", sk[:], SINK[l:l + 1, :], writes=[sk])
            esk = kb.sb("esk", [1, 8], F32)
            kb.op("act", lambda e: e.activation(out=esk[:], in_=sk[:], func=AF.Exp), reads=[sk], writes=[esk])
            esrow = kb.sb("esrow", [1, 8, 128], BF16)
            kb.op("dve", lambda e: e.tensor_copy(out=esrow[:], in_=esk[:].unsqueeze(2).to_broadcast([1, 8, 128])), reads=[esk], writes=[esrow])
            QT = kb.sb("QT", [128, 4, NTOK], BF16)
            KT = kb.sb("KT", [128, NTOK], BF16)
            V = kb.sb("V", [128, NTILES, 128], BF16)
            for j in range(4):
                kb.dma("sp", QT[:, j, :], PF[2 + j, :, :], reads=pf_reads(2 + j), writes=[QT])
            kb.dma("sp", KT[:], PF[6, :, :], reads=pf_reads(6), writes=[KT])
            kb.dma("sp", V[:], PT.rearrange("(t p) c -> p t c", p=128)[:, :, 0:128], reads=pt_reads(range(NTILES)), writes=[V])
            pss = [kb.ps(f"pss{i}", [128, 512]) for i in range(3)]
            po = [kb.ps(f"po{i}", [64, 512]) for i in range(2)]
            pd = [kb.ps(f"pd{i}", [64, 512]) for i in range(2)]
            pT = [kb.sb(f"pT{i}", [128, 512], BF16) for i in range(4)]
            pTm = [kb.sb(f"pTm{i}", [128, 512], BF16) for i in range(3)]
            rden = [kb.sb(f"rden{i}", [64, 512], F32) for i in range(2)]
            ot = [kb.sb(f"ot{i}", [64, 512], BF16) for i in range(2)]
            items = []
            nu = 0
            for b in range(NB):
                for qt in range(TILES_PB):
                    if qt < 2 and not need_ctx:
                        continue
                    keys = [(0, None), (1, None)]
                    if qt >= 2:
                        if qt - 1 >= 2:
                            keys.append((qt - 1, 0))
                        keys.append((qt, None))
                        if qt + 1 < TILES_PB:
                            keys.append((qt + 1, 1))
                    for g in range(2):
                        for idx, (kt_, mk) in enumerate(keys):
                            items.append((b, qt, g, nu, idx, len(keys), kt_, mk))
                        nu += 1
            LA = 2
            nm = 0
            sres = {}
            for it in range(len(items) + LA):
                if it < len(items):
                    (b, qt, g, u, idx, nkeys, kt_, mk) = items[it]
                    gs = slice(g * 64, (g + 1) * 64)
                    qc0 = b * TPB + qt * 128
                    kc0 = b * TPB + kt_ * 128
                    p = pss[it % 3]
                    kb.op("pe", lambda e, p=p, kc0=kc0, gs=gs, qc0=qc0: e.matmul(p[:], KT[gs, kc0:kc0 + 128], QT[gs, :, qc0:qc0 + 128], start=True, stop=True),
                          reads=[KT, QT], writes=[p])
                    t_ = pT[it % 4]
                    kb.op("act", lambda e, p=p, t_=t_: e.activation(out=t_[:], in_=p[:], func=AF.Exp, scale=0.125), reads=[p], writes=[t_])
                    if mk is not None:
                        tm_ = pTm[nm % 3]
                        nm += 1
                        kb.op("pool", lambda e, t_=t_, tm_=tm_, mk=mk: e.tensor_tensor(out=tm_[:], in0=t_[:], in1=amask[:, mk, :, :].rearrange("p a b -> p (a b)"), op=ALU.mult),
                              reads=[t_, amask], writes=[tm_])
                        t_ = tm_
                    sres[it] = t_
                j = it - LA
                if j < 0:
                    continue
                (b, qt, g, u, idx, nkeys, kt_, mk) = items[j]
                gs = slice(g * 64, (g + 1) * 64)
                qc0 = b * TPB + qt * 128
                t_ = sres.pop(j)
                o_ps = po[u % 2]
                d_ps = pd[u % 2]
                kb.op("pe", lambda e, t_=t_, kt_=kt_, idx=idx, b=b, gs=gs, o_ps=o_ps, nkeys=nkeys: e.matmul(o_ps[:], V[:, b * TILES_PB + kt_, gs], t_[:], start=(idx == 0), stop=(idx == nkeys - 1)),
                      reads=[V, t_], writes=[o_ps])
                kb.op("pe", lambda e, t_=t_, idx=idx, d_ps=d_ps: e.matmul(d_ps[:], ones64[:], t_[:], start=(idx == 0), stop=False),
                      reads=[ones64, t_], writes=[d_ps])
                if idx == nkeys - 1:
                    kb.op("pe", lambda e, d_ps=d_ps, g=g: e.matmul(d_ps[:], ones64[0:1, :], esrow[0:1, g * 4:(g + 1) * 4, :], start=False, stop=True),
                          reads=[ones64, esrow], writes=[d_ps])
                    rd = rden[u % 2]
                    o_ = ot[u % 2]
                    kb.op("dve", lambda e, rd=rd, d_ps=d_ps: e.reciprocal(out=rd[:], in_=d_ps[:]), reads=[d_ps], writes=[rd])
                    kb.op("dve", lambda e, rd=rd, o_=o_, o_ps=o_ps: e.tensor_tensor(out=o_[:], in0=o_ps[:], in1=rd[:], op=ALU.mult), reads=[o_ps, rd], writes=[o_])
                    kb.dma("sp", YA[g * 4:(g + 1) * 4, :, qc0:qc0 + 128].rearrange("h d t -> d h t"), o_[:].rearrange("d (h t) -> d h t", h=4),
                           reads=[o_], writes=[kb.dbuf(("YA", b, qt, g))])

    def phase4(l, need_ctx):
        LN8 = math.log(0.125)
        with kb.phase():
            d12 = kb.sb("d12", [128, 2, 128], F32)
            kb.dma("sp", d12[:], RD12[:, :, :], writes=[d12])
            jc = kb.sb("jc", [128, 2], F32)
            kb.dma("sp", jc[:], RJC[:, :], writes=[jc])
            irow = kb.sb("irow", [128, 2, 128], F32)
            kb.dma("sp", irow[:], RIROW[:, :, :], writes=[irow])
            lgb = kb.sb("lgb", [128, 8], F32)
            kb.dma("sp", lgb[:], LGB[l], writes=[lgb])
            lgp = kb.sb("lgp", [128, 2, 2], F32)
            kb.dma("sp", lgp[:], LGP[l], writes=[lgp])
            ln8 = kb.sb("ln8", [128, 1], F32)
            kb.op("dve", lambda e: e.memset(ln8[:], LN8), writes=[ln8])
            marg = kb.sb("marg", [128, 4, 128], F32)
            M = kb.sb("M", [128, 4, 128], F32)
            for hp in range(4):
                h = (hp % 2) * 2 + hp // 2
                kb.op("dve", lambda e, h=h, hp=hp: e.tensor_scalar(out=marg[:, hp, :], in0=d12[:, 0, :], scalar1=lgb[:, h:h + 1], scalar2=None, op0=ALU.mult), reads=[d12, lgb], writes=[marg])
                kb.op("dve", lambda e, h=h, hp=hp: e.scalar_tensor_tensor(out=marg[:, hp, :], in0=d12[:, 1, :], scalar=lgb[:, 4 + h:5 + h], in1=marg[:, hp, :], op0=ALU.mult, op1=ALU.add),
                      reads=[d12, lgb, marg], writes=[marg])
            kb.op("act", lambda e: e.activation(out=M[:], in_=marg[:], func=AF.Exp, bias=ln8[:], scale=1.0), reads=[marg, ln8], writes=[M])
            warg = kb.sb("warg", [128, 2, 4], F32)
            wk = kb.sb("wk", [128, 2, 4], F32)
            for dr in range(2):
                kb.op("dve", lambda e, dr=dr: e.tensor_scalar(out=warg[:, dr, :], in0=lgb[:, dr * 4:(dr + 1) * 4], scalar1=jc[:, dr:dr + 1], scalar2=None, op0=ALU.mult), reads=[lgb, jc], writes=[warg])
            kb.op("act", lambda e: e.activation(out=wk[:], in_=warg[:], func=AF.Exp, bias=ln8[:], scale=1.0), reads=[warg, ln8], writes=[wk])
            qw = kb.sb("qw", [128, 2, 2, 128], BF16)
            for c in range(2):
                for dr in range(2):
                    kb.op("act", lambda e, c=c, dr=dr: e.activation(out=qw[:, c, dr, :], in_=irow[:, dr, :], func=AF.Exp, scale=lgp[:, c, dr:dr + 1]), reads=[irow, lgp], writes=[qw])
            dec = kb.sb("dec", [128, 2, 2], F32)
            kb.op("act", lambda e: e.activation(out=dec[:], in_=lgp[:].rearrange("p c d -> p d c"), func=AF.Exp, scale=128.0), reads=[lgp], writes=[dec])

            QTr = kb.sb("QTr", [128, 2, TPB], BF16)
            KTr = kb.sb("KTr", [128, 2, TPB], BF16)
            Vr = kb.sb("Vr", [128, TILES_PB, 256], BF16)
            Kr = kb.sb("Kr", [128, TILES_PB, 256], BF16)
            Gr = kb.sb("Gr", [128, TILES_PB, 256], BF16)
            CT = kb.sb("CTs", [128, TILES_PB, 2, 2, 64], F32)
            SA = kb.sb("SA", [128, TILES_PB, 2, 2, 64], BF16)
            srun = [kb.sb(f"srun{i}", [128, 2, 64], F32) for i in range(2)]
            stmp = kb.sb("stmp", [128, 2, 64], F32)
            kw = [kb.sb(f"kw{i}", [128, 2, 256], BF16) for i in range(2)]
            pc = [kb.ps(f"pc{i}", [128, 512]) for i in range(1)]
            psr = [kb.ps(f"psr{i}", [128, 512]) for i in range(4)]
            por = [kb.ps(f"por{i}", [128, 256]) for i in range(2)]
            ptr2 = [kb.ps(f"ptr2{i}", [128, 2, 128], BF16) for i in range(1)]
            PTm = [kb.sb(f"PTm{i}", [128, 4, 128], BF16) for i in range(2)]
            qs = [kb.sb(f"qs{i}", [128, 2, 2, 128], BF16) for i in range(2)]
            o32 = [kb.sb(f"o32{i}", [128, 256], F32) for i in range(2)]
            sq = kb.sb("sq", [128, 256], F32)
            sg = kb.sb("sg", [128, 256], F32)
            stt_ = kb.sb("stats", [128, 6, 4], F32)
            yt = [kb.sb(f"yt{i}", [128, 256], BF16) for i in range(2)]
            yT = [kb.sb(f"yT{i}", [128, 2, 128], BF16) for i in range(2)]
            PTv = PT.rearrange("(t p) c -> p t c", p=128)
            rl = cfg.get("r_lim", 99)
            for b in range(NB):
                if rl < 2:
                    break
                t0 = b * TILES_PB
                c0 = b * TPB
                for c in range(2):
                    kb.dma("sp", QTr[:, c, :], PF[7 + c, :, c0:c0 + TPB], reads=pf_reads(7 + c), writes=[QTr])
                    kb.dma("sp", KTr[:, c, :], PF[9 + c, :, c0:c0 + TPB], reads=pf_reads(9 + c), writes=[KTr])
                kb.dma("sp", Vr[:], PTv[:, t0:t0 + TILES_PB, 128:384], reads=pt_reads(range(t0, t0 + TILES_PB)), writes=[Vr])
                kb.dma("sp", Gr[:], PTv[:, t0:t0 + TILES_PB, 384:640], reads=pt_reads(range(t0, t0 + TILES_PB)), writes=[Gr])
                kb.dma("sp", Kr[:], PTv[:, t0:t0 + TILES_PB, 640:896], reads=pt_reads(range(t0, t0 + TILES_PB)), writes=[Kr])
                for t in range(TILES_PB):
                    k_ = kw[t % 2]
                    for dr in range(2):
                        eng = "dve" if dr == 0 else "pool"
                        kb.op(eng, lambda e, k_=k_, dr=dr, t=t: e.tensor_tensor(out=k_[:, dr, :].rearrange("p (h d) -> p h d", h=4), in0=Kr[:, t, :].rearrange("p (h d) -> p h d", h=4),
                                                                                  in1=wk[:, dr, :].unsqueeze(2).to_broadcast([128, 4, 64]), op=ALU.mult),
                              reads=[Kr, wk], writes=[k_])
                    p = pc[0]

                    def cm(e, p=p, k_=k_, t=t):
                        for dr in range(2):
                            for c in range(2):
                                ins = e.matmul(p[:, (dr * 2 + c) * 128:(dr * 2 + c + 1) * 128], k_[:, dr, c * 128:(c + 1) * 128], Vr[:, t, c * 128:(c + 1) * 128], start=True, stop=True)
                        return ins
                    kb.op("pe", cm, reads=[k_, Vr], writes=[p])
                    pv_ = p[:].rearrange("p (a c) -> p a c", c=128)
                    kb.op("act", lambda e, pv_=pv_, t=t: e.activation(out=CT[0:64, t, :, :, :].rearrange("p a b c -> p (a b) c"), in_=pv_[0:64, :, 0:64], func=AF.Copy), reads=[p], writes=[CT])
                    kb.op("act", lambda e, pv_=pv_, t=t: e.activation(out=CT[64:128, t, :, :, :].rearrange("p a b c -> p (a b) c"), in_=pv_[64:128, :, 64:128], func=AF.Copy), reads=[p], writes=[CT])
                for dr in range(2):
                    if rl < 3:
                        break
                    order = list(range(TILES_PB)) if dr == 0 else [1, 0] + list(range(TILES_PB - 1, 1, -1))
                    cur = srun[0]
                    nxt = srun[1]
                    kb.op("dve", lambda e, cur=cur: e.memset(cur[:], 0.0), writes=[cur])
                    for t in order:
                        kb.op("pool", lambda e, cur=cur, t=t, dr=dr: e.tensor_copy(out=SA[:, t, dr, :, :], in_=cur[:]), reads=[cur], writes=[SA])
                        kb.op("dve", lambda e, cur=cur, dr=dr: e.tensor_tensor(out=stmp[:], in0=cur[:], in1=dec[:, dr, :].unsqueeze(2).to_broadcast([128, 2, 64]), op=ALU.mult),
                              reads=[cur, dec], writes=[stmp])
                        kb.op("dve", lambda e, nxt=nxt, t=t, dr=dr: e.tensor_tensor(out=nxt[:], in0=stmp[:], in1=CT[:, t, dr, :, :], op=ALU.add), reads=[stmp, CT], writes=[nxt])
                        cur, nxt = nxt, cur
                for t in range(TILES_PB):
                    if t < 2 and not need_ctx:
                        continue
                    if rl < 4:
                        continue
                    tc0 = t * 128
                    pA = psr[(t % 2) * 2]
                    pB = psr[(t % 2) * 2 + 1]

                    def sc(e, pA=pA, pB=pB, tc0=tc0):
                        for hl, p in ((0, pA), (1, pB)):
                            hs = slice(hl * 64, hl * 64 + 64)
                            for c in range(2):
                                ins = e.matmul(p[:, c * 128:(c + 1) * 128], KTr[hs, c, tc0:tc0 + 128], QTr[hs, c, tc0:tc0 + 128], start=True, stop=True)
                        return ins
                    kb.op("pe", sc, reads=[KTr, QTr], writes=[pA, pB])
                    if cfg.get("r4", 9) < 2:
                        continue
                    pm_ = PTm[t % 2]
                    for hl, p in ((0, pA), (1, pB)):
                        kb.op("dve", lambda e, p=p, pm_=pm_, hl=hl: e.tensor_tensor(out=pm_[:, hl * 2:hl * 2 + 2, :].rearrange("p a b -> p (a b)"), in0=p[:, 0:256], in1=M[:, hl * 2:hl * 2 + 2, :].rearrange("p a b -> p (a b)"), op=ALU.mult), reads=[p, M], writes=[pm_])
                    q_ = qs[t % 2]
                    if cfg.get("r4", 9) < 3:
                        continue
                    for dr in range(2):
                        for c in range(2):
                            kb.op("pool" if c == 0 else "dve", lambda e, q_=q_, dr=dr, c=c, tc0=tc0: e.tensor_tensor(out=q_[:, c, dr, :], in0=QTr[:, c, tc0:tc0 + 128], in1=qw[:, c, dr, :], op=ALU.mult), reads=[QTr, qw], writes=[q_])
                    if rl < 5:
                        continue
                    o_ps = por[t % 2]

                    def om(e, o_ps=o_ps, pm_=pm_, q_=q_, t=t):
                        for h in range(4):
                            hl = h % 2
                            c = h // 2
                            hs = slice(hl * 64, hl * 64 + 64)
                            e.matmul(o_ps[:, h * 64:(h + 1) * 64], pm_[:, hl * 2 + c, :], Vr[:, t, h * 64:(h + 1) * 64], start=True, stop=False)
                            e.matmul(o_ps[:, h * 64:(h + 1) * 64], q_[hs, c, 0, :], SA[hs, t, 0, c, :], start=False, stop=False)
                            ins = e.matmul(o_ps[:, h * 64:(h + 1) * 64], q_[hs, c, 1, :], SA[hs, t, 1, c, :], start=False, stop=True)
                        return ins
                    kb.op("pe", om, reads=[pm_, q_, Vr, SA], writes=[o_ps])
                    o_ = o32[t % 2]
                    kb.op("act", lambda e, o_=o_, o_ps=o_ps: e.activation(out=o_[:], in_=o_ps[:], func=AF.Copy), reads=[o_ps], writes=[o_])
                    ov = o_[:].rearrange("p (h d) -> p h d", h=4)
                    if rl < 6:
                        continue
                    kb.op("dve", lambda e, ov=ov: e.tensor_reduce(out=stt_[:, 0, :], in_=ov, axis=AX.X, op=ALU.add), reads=[o_], writes=[stt_])
                    kb.op("act", lambda e, o_=o_: e.activation(out=sq[:], in_=o_[:], func=AF.Square), reads=[o_], writes=[sq])
                    kb.op("dve", lambda e: e.tensor_reduce(out=stt_[:, 1, :], in_=sq[:].rearrange("p (h d) -> p h d", h=4), axis=AX.X, op=ALU.add), reads=[sq], writes=[stt_])
                    kb.op("dve", lambda e: e.tensor_scalar(out=stt_[:, 2:4, :], in0=stt_[:, 0:2, :], scalar1=1.0 / 64, scalar2=None, op0=ALU.mult), reads=[stt_], writes=[stt_])
                    kb.op("dve", lambda e: e.tensor_tensor(out=stt_[:, 4, :], in0=stt_[:, 2, :], in1=stt_[:, 2, :], op=ALU.mult), reads=[stt_], writes=[stt_])
                    kb.op("dve", lambda e: e.tensor_tensor(out=stt_[:, 5, :], in0=stt_[:, 3, :], in1=stt_[:, 4, :], op=ALU.subtract), reads=[stt_], writes=[stt_])
                    kb.op("act", lambda e: e.activation(out=stt_[:, 4, :], in_=stt_[:, 5, :], func=AF.Sqrt, bias=epsc[:], scale=1.0), reads=[stt_, epsc], writes=[stt_])
                    kb.op("dve", lambda e: e.reciprocal(out=stt_[:, 5, :], in_=stt_[:, 4, :]), reads=[stt_], writes=[stt_])
                    kb.op("dve", lambda e, ov=ov: e.tensor_tensor(out=ov, in0=ov, in1=stt_[:, 2, :].unsqueeze(2).to_broadcast([128, 4, 64]), op=ALU.subtract), reads=[o_, stt_], writes=[o_])
                    kb.op("dve", lambda e, ov=ov: e.tensor_tensor(out=ov, in0=ov, in1=stt_[:, 5, :].unsqueeze(2).to_broadcast([128, 4, 64]), op=ALU.mult), reads=[o_, stt_], writes=[o_])
                    kb.op("act", lambda e, t=t: e.activation(out=sg[:], in_=Gr[:, t, :], func=AF.Silu), reads=[Gr], writes=[sg])
                    y_ = yt[t % 2]
                    kb.op("dve", lambda e, y_=y_, o_=o_: e.tensor_tensor(out=y_[:], in0=o_[:], in1=sg[:], op=ALU.mult), reads=[o_, sg], writes=[y_])
                    if rl < 7:
                        continue
                    pt_ = ptr2[0]

                    def tr2(e, pt_=pt_, y_=y_):
                        for c in range(2):
                            ins = e.transpose(pt_[:, c, :], y_[:, c * 128:(c + 1) * 128], ident[:])
                        return ins
                    kb.op("pe", tr2, reads=[y_, ident], writes=[pt_])
                    yT_ = yT[t % 2]
                    kb.op("act", lambda e, pt_=pt_, yT_=yT_: e.activation(out=yT_[:], in_=pt_[:], func=AF.Copy), reads=[pt_], writes=[yT_])
                    kb.dma("sp", YR[:, :, c0 + tc0:c0 + tc0 + 128].rearrange("c p t -> p c t"), yT_[:], reads=[yT_], writes=[kb.dbuf(("YR", b, t))])

    def phase5(l):
        PI = math.pi
        NK = TPB // 8
        with kb.phase():
            alt = [0]

            def ve():
                alt[0] ^= 1
                return "dve" if alt[0] else "pool"
            sd = kb.sb("sd", [128, 2], F32)
            kb.dma("sp", sd[:], S5D[l], writes=[sd])
            bgl = kb.sb("bgl", [128, 2], F32)
            kb.dma("sp", bgl[:], BGLU[l], writes=[bgl])
            wgl = kb.sb("wgl", [128, 2, 256], BF16)
            kb.dma("pool", wgl[:], WGLU[l].rearrange("(k p) n -> p k n", p=128), writes=[wgl])
            Z = kb.sb("Z", [128, 2, S5NJ, 16], F32)
            bb = kb.sb("bb", [128, 2, 16, 16], F32)
            BL = kb.sb("BL", [128, 16, 8, 2, 16], BF16)
            CL = kb.sb("CL", [128, 16, 9, 2, 16], BF16)
            with kb.phase():
                par = kb.sb("par", [128, 3, 16], F32)
                kb.dma("sp", par[:], S5P[l], writes=[par])
                bri = kb.sb("bri", [128, 2, 16, 16], F32)
                kb.dma("sp", bri[:], S5B[l], writes=[bri])
                cri = kb.sb("cri", [128, 2, 16, 16], F32)
                kb.dma("sp", cri[:], S5C[l], writes=[cri])
                jt = kb.sb("jt", [128, S5NJ, 16], F32)
                kb.dma("sp", jt[:], S5JT[:, :, :], writes=[jt])
                negpi = kb.sb("negpi", [128, 1], F32)
                kb.op("dve", lambda e: e.memset(negpi[:], -PI), writes=[negpi])
                lre = par[:, 0, :]
                lim = par[:, 1, :]
                dt = kb.sb("dt", [128, 16], F32)
                kb.op("act", lambda e: e.activation(out=dt[:], in_=par[:, 2, :], func=AF.Exp), reads=[par], writes=[dt])
                ab = kb.sb("ab", [128, 2, 16], F32)
                for i in range(2):
                    kb.op("dve", lambda e, i=i: e.tensor_tensor(out=ab[:, i, :], in0=par[:, i, :], in1=dt[:], op=ALU.mult), reads=[par, dt], writes=[ab])
                am = kb.sb("am", [128, S5NJ, 16], F32)
                bp = kb.sb("bp", [128, S5NJ, 16], F32)
                kb.op("dve", lambda e: e.tensor_tensor(out=am[:], in0=ab[:, 0, :].unsqueeze(1).to_broadcast([128, S5NJ, 16]), in1=jt[:], op=ALU.mult), reads=[ab, jt], writes=[am])
                kb.op("dve", lambda e: e.tensor_tensor(out=bp[:], in0=ab[:, 1, :].unsqueeze(1).to_broadcast([128, S5NJ, 16]), in1=jt[:], op=ALU.mult), reads=[ab, jt], writes=[bp])
                mag = kb.sb("mag", [128, S5NJ, 16], F32)
                kb.op("act", lambda e: e.activation(out=mag[:].rearrange("p a b -> p (a b)"), in_=am[:].rearrange("p a b -> p (a b)"), func=AF.Exp), reads=[am], writes=[mag])
                rsn = kb.sb("rsn", [128, 2, S5NJ, 16], F32)
                MAGIC = 12582912.0
                xs_ = kb.sb("xs_", [128, 2, S5NJ, 16], F32)
                kk_ = kb.sb("kk_", [128, 2, S5NJ, 16], F32)
                kb.op("dve", lambda e: e.tensor_scalar(out=xs_[:, 0, :, :], in0=bp[:], scalar1=0.5 * PI, scalar2=None, op0=ALU.add), reads=[bp], writes=[xs_])
                kb.op("dve", lambda e: e.tensor_copy(out=xs_[:, 1, :, :], in_=bp[:]), reads=[bp], writes=[xs_])
                fl = lambda a: a[:].rearrange("p a b c -> p (a b c)")
                kb.op("dve", lambda e: e.tensor_scalar(out=fl(kk_), in0=fl(xs_), scalar1=1.0 / TWO_PI, scalar2=MAGIC, op0=ALU.mult, op1=ALU.add), reads=[xs_], writes=[kk_])
                kb.op("dve", lambda e: e.tensor_scalar(out=fl(kk_), in0=fl(kk_), scalar1=-MAGIC, scalar2=None, op0=ALU.add), reads=[kk_], writes=[kk_])
                kb.op("dve", lambda e: e.scalar_tensor_tensor(out=fl(rsn), in0=fl(kk_), scalar=-TWO_PI, in1=fl(xs_), op0=ALU.mult, op1=ALU.add), reads=[kk_, xs_], writes=[rsn])
                csn = kb.sb("csn", [128, 2, S5NJ, 16], F32)
                kb.op("act", lambda e: e.activation(out=csn[:].rearrange("p a b c -> p (a b c)"), in_=rsn[:].rearrange("p a b c -> p (a b c)"), func=AF.Sin), reads=[rsn], writes=[csn])
                for i in range(2):
                    kb.op("dve", lambda e, i=i: e.tensor_tensor(out=Z[:, i, :, :], in0=csn[:, i, :, :], in1=mag[:], op=ALU.mult), reads=[csn, mag], writes=[Z])
                tw = kb.sb("tw", [128, 8, 16], F32)
                W = kb.sb("W", [128, 2, 16], F32)

                def tt(o, a, b, op):
                    kb.op("dve", lambda e: e.tensor_tensor(out=o, in0=a, in1=b, op=op), reads=[par, Z, tw, W], writes=[tw, W])
                tt(tw[:, 0, :], lre, lre, ALU.mult)
                tt(tw[:, 1, :], lim, lim, ALU.mult)
                tt(tw[:, 0, :], tw[:, 0, :], tw[:, 1, :], ALU.add)
                kb.op("dve", lambda e: e.reciprocal(out=tw[:, 1, :], in_=tw[:, 0, :]), reads=[tw], writes=[tw])
                kb.op("dve", lambda e: e.tensor_scalar(out=tw[:, 2, :], in0=Z[:, 0, 1, :], scalar1=-1.0, scalar2=None, op0=ALU.add), reads=[Z], writes=[tw])
                tt(tw[:, 3, :], tw[:, 2, :], lre, ALU.mult)
                tt(tw[:, 4, :], Z[:, 1, 1, :], lim, ALU.mult)
                tt(tw[:, 3, :], tw[:, 3, :], tw[:, 4, :], ALU.add)
                tt(W[:, 0, :], tw[:, 3, :], tw[:, 1, :], ALU.mult)
                tt(tw[:, 5, :], Z[:, 1, 1, :], lre, ALU.mult)
                tt(tw[:, 6, :], tw[:, 2, :], lim, ALU.mult)
                tt(tw[:, 5, :], tw[:, 5, :], tw[:, 6, :], ALU.subtract)
                tt(W[:, 1, :], tw[:, 5, :], tw[:, 1, :], ALU.mult)
                t4 = [kb.sb(f"t4_{i}", [128, 16, 16], F32) for i in range(4)]

                def cmul(out_r, out_i, xr, xi, fr, fi, rd, wr, neg_im=False):
                    frb = fr.unsqueeze(2).to_broadcast([128, 16, 16])
                    fib = fi.unsqueeze(2).to_broadcast([128, 16, 16])
                    e1, e2 = ve(), ve()
                    kb.op(e1, lambda e: e.tensor_tensor(out=t4[0][:], in0=xr, in1=frb, op=ALU.mult), reads=rd, writes=[t4[0]])
                    kb.op(e2, lambda e: e.tensor_tensor(out=t4[1][:], in0=xi, in1=fib, op=ALU.mult), reads=rd, writes=[t4[1]])
                    kb.op(e1, lambda e: e.tensor_tensor(out=t4[2][:], in0=xr, in1=fib, op=ALU.mult), reads=rd, writes=[t4[2]])
                    kb.op(e2, lambda e: e.tensor_tensor(out=t4[3][:], in0=xi, in1=frb, op=ALU.mult), reads=rd, writes=[t4[3]])
                    kb.op(e1, lambda e: e.tensor_tensor(out=out_r, in0=t4[0][:], in1=t4[1][:], op=ALU.subtract), reads=[t4[0], t4[1]], writes=wr)
                    if neg_im:
                        kb.op("dve", lambda e: e.scalar_tensor_tensor(out=out_i, in0=t4[2][:], scalar=-1.0, in1=t4[3][:], op0=ALU.mult, op1=ALU.subtract), reads=[t4[2], t4[3]], writes=wr)
                    else:
                        kb.op(e2, lambda e: e.tensor_tensor(out=out_i, in0=t4[2][:], in1=t4[3][:], op=ALU.add), reads=[t4[2], t4[3]], writes=wr)
                cmul(bb[:, 0, :, :], bb[:, 1, :, :], bri[:, 0, :, :], bri[:, 1, :, :], W[:, 0, :], W[:, 1, :], [bri, W], [bb])
                for sidx in range(8):
                    cmul(BL[:, :, sidx, 0, :], BL[:, :, sidx, 1, :], bb[:, 0, :, :], bb[:, 1, :, :], Z[:, 0, 7 - sidx, :], Z[:, 1, 7 - sidx, :], [bb, Z], [BL])
                for j in range(9):
                    cmul(CL[:, :, j, 0, :], CL[:, :, j, 1, :], cri[:, 0, :, :], cri[:, 1, :, :], Z[:, 0, j, :], Z[:, 1, j, :], [cri, Z], [CL], neg_im=True)

            g_fm = kb.sb("g_fm", [128, 2, NTOK], BF16)
            u_fm = kb.sb("u_fm", [128, NTOK], BF16)
            Cp = kb.sb("Cp", [128, 8, 9, 2, 128], BF16)
            Bpad = kb.sb("Bpad", [128, 8, 2, 128], BF16)
            Xd = kb.sb("Xd", [128, 8, 2, 128], BF16)
            Ld = [kb.sb(f"Ld{i}", [128, 8, 2, 128], BF16) for i in range(2)]
            BD = kb.sb("BD", [128, 2, 8, 128], BF16)
            dgl = kb.sb("dgl", [128, 128], BF16)
            Db = [kb.sb(f"Db{d}", [128, 16, NK], F32) for d in range(2)]
            Ssh = kb.sb("Ssh", [128, 2, 16, NK], BF16)
            AcT = [kb.sb(f"AcT{d}", [128, 16, 2, 4, 2], F32) for d in range(2)]
            BcT = [kb.sb(f"BcT{d}", [128, 16, 2, 4, 2], F32) for d in range(2)]
            AcR = kb.sb("AcR", [128, 15, 2, 4, 2], F32)
            BcR = kb.sb("BcR", [128, 15, 2, 4, 2], F32)
            T1 = [kb.sb(f"T1{d}", [128, 16, 18], F32) for d in range(2)]
            T2 = [kb.sb(f"T2{d}", [128, 16, 18], F32) for d in range(2)]
            ptp = [kb.ps(f"ptp{i}", [128, 512]) for i in range(2)]
            pbd = kb.ps("pbd", [128, 512])
            pdv = [kb.ps(f"pdv{i}", [128, 512]) for i in range(2)]
            pyr = [kb.ps(f"pyr{i}", [128, 512]) for i in range(2)]
            pgl = kb.ps("pgl", [128, 512])
            gx2 = [kb.sb(f"gx2{i}", [128, NK], F32) for i in range(2)]
            gt = [kb.sb(f"gt{i}", [128, NK], F32) for i in range(2)]
            gsg = [kb.sb(f"gsg{i}", [128, NK], F32) for i in range(2)]
            uv = u_fm[:].rearrange("p (b k s) -> p b k s", b=NB, s=8)
            nev = 0
            for h in range(2):
                kb.dma("sp", u_fm[:], PF[h, :, :], reads=pf_reads(h), writes=[u_fm])
                kb.op("pool", lambda e: e.memset(Cp[:].rearrange("p a b c d -> p (a b c d)"), 0.0), writes=[Cp])
                kb.op("pool", lambda e: e.memset(Bpad[:].rearrange("p a b c -> p (a b c)"), 0.0), writes=[Bpad])
                kb.op("dve", lambda e, h=h: e.tensor_scalar(out=dgl[:], in0=identf[:], scalar1=sd[:, h:h + 1], scalar2=None, op0=ALU.mult), reads=[identf, sd], writes=[dgl])
                for d in range(2):
                    for j4 in range(4):
                        q = d * 8 + 4 * h + j4
                        ql = d * 4 + j4
                        for g2 in range(2):
                            hs = slice(g2 * 64, g2 * 64 + 64)
                            c0 = 32 * j4 + 16 * g2
                            kb.op(ve(), lambda e, hs=hs, ql=ql, q=q, c0=c0: e.tensor_copy(out=Cp[hs, ql, :, :, c0:c0 + 16].rearrange("p j r c -> p (j r) c"), in_=CL[hs, q, :, :, :].rearrange("p j r c -> p (j r) c")), reads=[CL], writes=[Cp])
                            kb.op(ve(), lambda e, hs=hs, ql=ql, q=q, c0=c0: e.tensor_copy(out=Bpad[hs, ql, :, c0:c0 + 16], in_=bb[hs, :, q, :]), reads=[bb], writes=[Bpad])
                for d in range(2):
                    for l0 in (0, 4):
                        def bdm(e, d=d, l0=l0):
                            for lg in range(l0, l0 + 4):
                                n = 0
                                for j4 in range(4):
                                    for ri in range(2):
                                        ins = e.matmul(pbd[:, (lg - l0) * 128:(lg - l0 + 1) * 128], Bpad[:, d * 4 + j4, ri, :], Cp[:, d * 4 + j4, lg, ri, :], start=(n == 0), stop=(n == 7))
                                        n += 1
                            return ins
                        kb.op("pe", bdm, reads=[Bpad, Cp], writes=[pbd])
                        kb.op("act", lambda e, d=d, l0=l0: e.activation(out=BD[:, d, l0:l0 + 4, :].rearrange("p a b -> p (a b)"), in_=pbd[:], func=AF.Copy), reads=[pbd], writes=[BD])
                for d in range(2):
                    for j4 in range(4):
                        q = d * 8 + 4 * h + j4
                        ql = d * 4 + j4
                        kb.op("pool", lambda e: e.memset(Xd[:].rearrange("p a b c -> p (a b c)"), 0.0), writes=[Xd])
                        for g2 in range(2):
                            hs = slice(g2 * 64, g2 * 64 + 64)
                            c0 = 32 * j4 + 16 * g2
                            kb.op("pool", lambda e, hs=hs, q=q, c0=c0: e.tensor_copy(out=Xd[hs, :, :, c0:c0 + 16].rearrange("p s r c -> p (s r) c"), in_=BL[hs, q, :, :, :].rearrange("p s r c -> p (s r) c")), reads=[BL], writes=[Xd])
                        L_ = Ld[ql % 2]
                        for s0 in range(0, 8, 2):
                            pt_ = ptp[(s0 // 2) % 2]

                            def trm(e, pt_=pt_, s0=s0):
                                for i in range(4):
                                    ins = e.matmul(pt_[:, i * 128:(i + 1) * 128], Xd[:, s0 + i // 2, i % 2, :], ident[:], start=True, stop=True)
                                return ins
                            kb.op("pe", trm, reads=[Xd, ident], writes=[pt_])
                            kb.op("act", lambda e, pt_=pt_, L_=L_, s0=s0: e.activation(out=L_[:, s0:s0 + 2, :, :].rearrange("p a b c -> p (a b c)"), in_=pt_[:], func=AF.Copy), reads=[pt_], writes=[L_])
                        for ri in range(2):
                            for b in range(NB):
                                pd_ = pdv[nev % 2]
                                nev += 1

                                def drv(e, pd_=pd_, L_=L_, ri=ri, b=b, d=d):
                                    for sp_ in range(8):
                                        st_ = sp_ if d == 0 else 7 - sp_
                                        ins = e.matmul(pd_[:, 0:NK], L_[:, sp_, ri, :], uv[:, b, :, st_], start=(sp_ == 0), stop=(sp_ == 7))
                                    return ins
                                kb.op("pe", drv, reads=[L_, u_fm], writes=[pd_])
                                col = ri * 8 + j4 * 2 + b
                                kb.op("act", lambda e, pd_=pd_, d=d, col=col: e.activation(out=Db[d][:, col, :], in_=pd_[:, 0:NK], func=AF.Copy), reads=[pd_], writes=[Db[d]])
                def cplx_acc(eng, d, out_ap, v_lo, v_hi, v_all, a_ap, b_lo, b_hi, shape):
                    t1 = T1[d][:, :, 0:shape[1]] if len(shape) == 2 else T1[d][:, :, 0]
                    t2 = T2[d][:, :, 0:shape[1]] if len(shape) == 2 else T2[d][:, :, 0]
                    kb.op(eng, lambda e: e.tensor_tensor(out=t1, in0=v_all, in1=a_ap, op=ALU.mult), reads=[Db[d], AcT[d], AcR], writes=[T1[d]])
                    kb.op(eng, lambda e: e.tensor_tensor(out=t2[:, 0:8], in0=v_hi, in1=b_lo, op=ALU.mult), reads=[Db[d], BcT[d], BcR], writes=[T2[d]])
                    kb.op(eng, lambda e: e.tensor_tensor(out=t2[:, 8:16], in0=v_lo, in1=b_hi, op=ALU.mult), reads=[Db[d], BcT[d], BcR], writes=[T2[d]])
                    kb.op(eng, lambda e: e.tensor_tensor(out=t1, in0=t1, in1=t2, op=ALU.add), reads=[T1[d], T2[d]], writes=[T1[d]])
                    kb.op(eng, lambda e: e.tensor_tensor(out=out_ap, in0=out_ap, in1=t1, op=ALU.add), reads=[Db[d], T1[d]], writes=[Db[d]])
                for d in range(2):
                    eng = "dve" if d == 0 else "pool"
                    q0 = d * 8 + 4 * h
                    for ri in range(2):
                        kb.op(eng, lambda e, d=d, ri=ri, q0=q0: e.tensor_copy(out=AcT[d][:, :, ri, :, :], in_=Z[:, 0, 8:24, q0:q0 + 4].unsqueeze(3).to_broadcast([128, 16, 4, 2])), reads=[Z], writes=[AcT[d]])
                    kb.op(eng, lambda e, d=d, q0=q0: e.tensor_copy(out=BcT[d][:, :, 1, :, :], in_=Z[:, 1, 8:24, q0:q0 + 4].unsqueeze(3).to_broadcast([128, 16, 4, 2])), reads=[Z], writes=[BcT[d]])
                    kb.op(eng, lambda e, d=d: e.tensor_scalar(out=BcT[d][:, :, 0, :, :], in0=BcT[d][:, :, 1, :, :], scalar1=-1.0, scalar2=None, op0=ALU.mult), reads=[BcT[d]], writes=[BcT[d]])
                    if d == 1:
                        for ri in range(2):
                            kb.op(eng, lambda e, ri=ri, q0=q0: e.tensor_copy(out=AcR[:, :, ri, :, :], in_=Z[:, 0, 24:39, q0:q0 + 4].unsqueeze(3).to_broadcast([128, 15, 4, 2])), reads=[Z], writes=[AcR])
                        kb.op(eng, lambda e, q0=q0: e.tensor_copy(out=BcR[:, :, 1, :, :], in_=Z[:, 1, 24:39, q0:q0 + 4].unsqueeze(3).to_broadcast([128, 15, 4, 2])), reads=[Z], writes=[BcR])
                        kb.op(eng, lambda e: e.tensor_scalar(out=BcR[:, :, 0, :, :], in0=BcR[:, :, 1, :, :], scalar1=-1.0, scalar2=None, op0=ALU.mult), reads=[BcR], writes=[BcR])
                if "rec" not in cfg.get("s5_skip", ()):
                    for d in range(2):
                        eng = "dve" if d == 0 else "pool"
                        Dv = Db[d][:].rearrange("p c (B m) -> p c B m", m=16)
                        fl5 = lambda t, m_: t[:, m_, :, :, :].rearrange("p a b c -> p (a b c)")
                        A0, B0 = fl5(AcT[d], 0), fl5(BcT[d], 0)
                        A15, B15 = fl5(AcT[d], 15), fl5(BcT[d], 15)
                        bc18 = lambda a: a.unsqueeze(2).to_broadcast([128, a.shape[1], 18])
                        ms = list(range(1, 16)) if d == 0 else list(range(14, -1, -1))
                        for m_ in ms:
                            mp = m_ - 1 if d == 0 else m_ + 1
                            cplx_acc(eng, d, Dv[:, :, :, m_], Dv[:, 0:8, :, mp], Dv[:, 8:16, :, mp], Dv[:, :, :, mp], bc18(A0), bc18(B0[:, 0:8]), bc18(B0[:, 8:16]), (16, 18))
                        border = list(range(18)) if d == 0 else [1, 0] + list(range(17, 1, -1))
                        me = 15 if d == 0 else 0
                        for i_ in range(1, 18):
                            Bp, Bc = border[i_ - 1], border[i_]
                            cplx_acc(eng, d, Dv[:, :, Bc, me], Dv[:, 0:8, Bp, me], Dv[:, 8:16, Bp, me], Dv[:, :, Bp, me], A15, B15[:, 0:8], B15[:, 8:16], (16,))
                        if d == 0:
                            Am = AcT[0][:, 0:15, :, :, :].rearrange("p m a b c -> p (a b c) m")
                            Bm = BcT[0][:, 0:15, :, :, :].rearrange("p m a b c -> p (a b c) m")
                            msl = slice(0, 15)
                        else:
                            Am = AcR[:].rearrange("p m a b c -> p (a b c) m")
                            Bm = BcR[:].rearrange("p m a b c -> p (a b c) m")
                            msl = slice(1, 16)
                        bc15 = lambda a: a.unsqueeze(2).to_broadcast([128, a.shape[1], 15])
                        for i_ in range(1, 18):
                            Bp, Bc = border[i_ - 1], border[i_]
                            cplx_acc(eng, d, Dv[:, :, Bc, msl], bc15(Dv[:, 0:8, Bp, me]), bc15(Dv[:, 8:16, Bp, me]), bc15(Dv[:, :, Bp, me]), Am, Bm[:, 0:8, :], Bm[:, 8:16, :], (16, 15))
                kb.op("act", lambda e: e.activation(out=Ssh[:, 0, :, 1:NK], in_=Db[0][:, :, 0:NK - 1], func=AF.Copy), reads=[Db[0]], writes=[Ssh])
                kb.op("dve", lambda e: e.memset(Ssh[:, 0, :, 0:1], 0.0), writes=[Ssh])
                kb.op("act", lambda e: e.activation(out=Ssh[:, 1, :, 0:31], in_=Db[1][:, :, 1:32], func=AF.Copy), reads=[Db[1]], writes=[Ssh])
                kb.op("dve", lambda e: e.memset(Ssh[:, 1, :, 31:32], 0.0), writes=[Ssh])
                kb.op("act", lambda e: e.activation(out=Ssh[:, 1, :, 32:NK - 1], in_=Db[1][:, :, 33:NK], func=AF.Copy), reads=[Db[1]], writes=[Ssh])
                kb.op("act", lambda e: e.activation(out=Ssh[:, 1, :, NK - 1:NK], in_=Db[1][:, :, 0:1], func=AF.Copy), reads=[Db[1]], writes=[Ssh])
                gv = g_fm[:, h, :].rearrange("p (b k s) -> p b k s", b=NB, s=8)
                for b in range(NB if "rdo" not in cfg.get("s5_skip", ()) else 0):
                    for t in range(8):
                        py_ = pyr[t % 2]

                        def rdo(e, py_=py_, b=b, t=t):
                            mm = [(dgl[:], uv[:, b, :, t])]
                            for s_ in range(0, t + 1):
                                mm.append((BD[:, 0, t - s_, :], uv[:, b, :, s_]))
                            for s_ in range(t, 8):
                                mm.append((BD[:, 1, s_ - t, :], uv[:, b, :, s_]))
                            for d in range(2):
                                j = t + 1 if d == 0 else 8 - t
                                for j4 in range(4):
                                    for ri in range(2):
                                        mm.append((Cp[:, d * 4 + j4, j, ri, :], Ssh[:, d, ri * 8 + j4 * 2 + b, :]))
                            for i, (a_, b_) in enumerate(mm):
                                ins = e.matmul(py_[:, 0:NK], a_, b_, start=(i == 0), stop=(i == len(mm) - 1))
                            return ins
                        kb.op("pe", rdo, reads=[dgl, BD, Cp, Ssh, u_fm], writes=[py_])
                        x2 = gx2[t % 2]
                        t_ = gt[t % 2]
                        sg_ = gsg[t % 2]
                        yv = py_[:, 0:NK]
                        kb.op("act", lambda e, x2=x2, yv=yv: e.activation(out=x2[:], in_=yv, func=AF.Square), reads=[py_], writes=[x2])
                        kb.op("dve", lambda e, x2=x2: e.tensor_scalar(out=x2[:], in0=x2[:], scalar1=0.044715, scalar2=1.0, op0=ALU.mult, op1=ALU.add), reads=[x2], writes=[x2])
                        kb.op("dve", lambda e, x2=x2, t_=t_, yv=yv: e.tensor_tensor(out=t_[:], in0=yv, in1=x2[:], op=ALU.mult), reads=[py_, x2], writes=[t_])
                        kb.op("act", lambda e, t_=t_, sg_=sg_: e.activation(out=sg_[:], in_=t_[:], func=AF.Sigmoid, scale=2.0 * math.sqrt(2.0 / PI)), reads=[t_], writes=[sg_])
                        kb.op("dve", lambda e, sg_=sg_, yv=yv, b=b, t=t, gv=gv: e.tensor_tensor(out=gv[:, b, :, t], in0=yv, in1=sg_[:], op=ALU.mult), reads=[py_, sg_], writes=[g_fm])
            sgm = [kb.sb(f"sgm{i}", [128, 512], F32) for i in range(2)]
            yso = [kb.sb(f"yso{i}", [128, 512], BF16) for i in range(2)]
            n = 0
            for tb in range(NTOK // 512):
                cs = slice(tb * 512, (tb + 1) * 512)
                for mo in range(2):
                    def glm(e, mo=mo, cs=cs):
                        for kk in range(2):
                            ins = e.matmul(pgl[:], wgl[:, kk, mo * 128:(mo + 1) * 128], g_fm[:, kk, cs], start=(kk == 0), stop=(kk == 1))
                        return ins
                    kb.op("pe", glm, reads=[wgl, g_fm], writes=[pgl])
                    sm_ = sgm[n % 2]
                    yo_ = yso[n % 2]
                    n += 1
                    kb.op("act", lambda e, sm_=sm_, mo=mo: e.activation(out=sm_[:], in_=pgl[:], func=AF.Sigmoid, bias=bgl[:, mo:mo + 1], scale=1.0), reads=[pgl, bgl], writes=[sm_])
                    kb.op("dve", lambda e, sm_=sm_, yo_=yo_, mo=mo, cs=cs: e.tensor_tensor(out=yo_[:], in0=g_fm[:, mo, cs], in1=sm_[:], op=ALU.mult), reads=[g_fm, sm_], writes=[yo_])
                    kb.dma("sp", YS[mo, :, cs], yo_[:], reads=[yo_], writes=[kb.dbuf(("YS", tb, mo))])

    def bcast_row(dst_ap, src_row_ap, reads, wbuf):
        kb.dma("sp", dst_ap.unsqueeze(1), src_row_ap.partition_broadcast(128), reads=reads, writes=[wbuf])

    def out_rows(l, tt):
        b, r, is_ctx, midx = tile_info(tt)
        if l == DEPTH - 1:
            r0 = b * TSEQ + (r - 2) * 128
            return OUT[r0:r0 + 128, :], kb.dbuf(("OUT", tt))
        return XS[tt * 128:(tt + 1) * 128, :], kb.dbuf(("XS", tt))

    def postnorm(upd_aps, upd_bufs, x_ap, x_buf, gate_ap, gate_buf, lng, tmp, res, st, mv, rs, dst_ap, dst_buf, eng2="pool"):
        for nb in range(2):
            kb.op("dve", lambda e, nb=nb: e.tensor_tensor(out=tmp[:, nb * 512:(nb + 1) * 512], in0=upd_aps[nb], in1=gate_ap[:, nb * 512:(nb + 1) * 512], op=ALU.mult),
                  reads=[upd_bufs[nb], gate_buf], writes=[tmp])
        kb.op("dve", lambda e: e.scalar_tensor_tensor(out=res[:], in0=x_ap, scalar=float(ALPHA), in1=tmp[:], op0=ALU.mult, op1=ALU.add), reads=[x_buf, tmp], writes=[res])
        ln_tile(res[:], res, tmp[:], tmp, st, mv, rs)
        kb.op(eng2, lambda e: e.tensor_tensor(out=res[:], in0=tmp[:], in1=lng[:, 0, :], op=ALU.mult), reads=[tmp, lng], writes=[res])
        kb.op(eng2, lambda e: e.tensor_tensor(out=res[:], in0=res[:], in1=lng[:, 1, :], op=ALU.add), reads=[res, lng], writes=[res])
        kb.dma("sp", dst_ap, res[:], reads=[res], writes=[dst_buf])

    def phase6(l, SRC, src_key, need_ctx):
        with kb.phase():
            wout = kb.sb("wout", [128, 8, D], BF16)
            kb.dma("pool", wout[:], WOUT[l].rearrange("(k p) n -> p k n", p=128), writes=[wout])
            gate = kb.sb("gate", [128, 3, D], F32)
            for mi in range(3):
                bcast_row(gate[:, mi, :], MODR[l, mi:mi + 1, 2 * D:3 * D], [kb.dbuf(("MODR", l))], gate)
            lng = kb.sb("lng", [128, 2, D], F32)
            for i in range(2):
                bcast_row(lng[:, i, :], LNG[l, i:i + 1, :], [], lng)
            mixT = [kb.sb(f"mixT{i}", [128, 8, 128], BF16) for i in range(2)]
            xt = [kb.sb(f"xt6{i}", [128, D], F32) for i in range(2)]
            tmp = [kb.sb(f"tmp6{i}", [128, D], F32) for i in range(2)]
            res = [kb.sb(f"res6{i}", [128, D], F32) for i in range(2)]
            st = kb.sb("st6", [128, 2, 6], F32)
            mv = kb.sb("mv6", [128, 2], F32)
            rs = kb.sb("rs6", [128, 2], F32)
            pso = [kb.ps(f"pso{i}", [128, 512]) for i in range(4)]
            YAv = YA.rearrange("h d t -> (h d) t").rearrange("(c p) t -> p c t", p=128)
            tiles6 = [tt for tt in range(NTILES) if need_ctx or not tile_info(tt)[2]]

            def load6(n_):
                tt_ = tiles6[n_]
                b_, r_, _, _ = tile_info(tt_)
                cs_ = slice(tt_ * 128, (tt_ + 1) * 128)
                m_ = mixT[n_ % 2]
                x_ = xt[n_ % 2]
                kb.dma("sp", m_[:, 0:2, :], YS[:, :, cs_].rearrange("c p t -> p c t"), reads=[kb.dbuf(("YS", tt_ // 4, mo)) for mo in range(2)], writes=[m_])
                kb.dma("act", m_[:, 2:6, :], YAv[:, :, cs_], reads=[kb.dbuf(("YA", b_, r_, g)) for g in range(2)], writes=[m_])
                kb.dma("sp", m_[:, 6:8, :], YR[:, :, cs_].rearrange("c p t -> p c t"), reads=[kb.dbuf(("YR", b_, r_))], writes=[m_])
                kb.dma("act", x_[:], SRC[cs_, :], reads=[kb.dbuf((src_key, tt_))], writes=[x_])
            load6(0)
            for n, tt in enumerate(tiles6):
                b, r, is_ctx, midx = tile_info(tt)
                cs = slice(tt * 128, (tt + 1) * 128)
                m_ = mixT[n % 2]
                x_ = xt[n % 2]
                if n + 1 < len(tiles6):
                    load6(n + 1)
                pp = [pso[(n % 2) * 2 + nb] for nb in range(2)]
                for nb in range(2):
                    def mo_(e, nb=nb, m_=m_, p=pp[nb]):
                        for k in range(8):
                            ins = e.matmul(p[:], m_[:, k, :], wout[:, k, nb * 512:(nb + 1) * 512], start=(k == 0), stop=(k == 7))
                        return ins
                    kb.op("pe", mo_, reads=[m_, wout], writes=[pp[nb]])
                dst_ap, dst_buf = XS[cs, :], kb.dbuf(("XS", tt))
                postnorm([pp[0][:], pp[1][:]], pp, x_[:], x_, gate[:, midx, :], gate, lng, tmp[n % 2], res[n % 2], st, mv, rs, dst_ap, dst_buf)

    def phase7(l, need_ctx):
        moe = (l % 2 == 1)
        li = l // 2
        if moe:
            experts = [(MW1[li, e], MW3[li, e], MW2[li, e], e) for e in range(NEXP)]
            nft = DFFE // 128
        else:
            experts = [(FW1[li], FW3[li], FW2[li], None)]
            nft = DFF // 128
        with kb.phase():
            modT = kb.sb("modT7", [128, 48, 3], F32)
            load_modT(l, modT)
            gate = kb.sb("gate7", [128, 3, D], F32)
            for mi in range(3):
                bcast_row(gate[:, mi, :], MODR[l, mi:mi + 1, 5 * D:6 * D], [kb.dbuf(("MODR", l))], gate)
            lng = kb.sb("lng7", [128, 2, D], F32)
            for i in range(2):
                bcast_row(lng[:, i, :], LNG[l, 2 + i:3 + i, :], [], lng)
            rout = kb.sb("rout", [128, 8, NEXP], BF16)
            if moe:
                kb.dma("pool", rout[:], ROUT[li].rearrange("(k p) e -> p k e", p=128), writes=[rout])
            acc = kb.sb("acc", [128, 8, D], F32)
            hT = kb.sb("hT7", [128, 8, 1024], BF16)
            G = kb.sb("G7", [128, 8, NEXP], F32)
            w1c = [kb.sb(f"w1c{i}", [128, 8, 512], BF16) for i in range(2)]
            w3c = [kb.sb(f"w3c{i}", [128, 8, 512], BF16) for i in range(2)]
            w2c = [kb.sb(f"w2c{i}", [128, 4, D], BF16) for i in range(2)]
            act = [kb.sb(f"act{i}", [128, 4, 512], BF16) for i in range(2)]
            sa = [kb.sb(f"sa{i}", [128, 512], F32) for i in range(2)]
            xt = [kb.sb(f"xt7{i}", [128, D], F32) for i in range(2)]
            xn = [kb.sb(f"xn7{i}", [128, D], BF16) for i in range(2)]
            tmpm = kb.sb("tmpm7", [128, 8, 128], F32)
            tmp = kb.sb("tmp7", [128, D], F32)
            res = kb.sb("res7", [128, D], F32)
            st = kb.sb("st7", [128, 2, 6], F32)
            mv = kb.sb("mv7", [128, 2], F32)
            rs = kb.sb("rs7", [128, 2], F32)
            gs_ = kb.sb("gs7", [128, 6, NEXP], F32)
            gm = kb.sb("gm7", [128, 4], F32)
            ptr = kb.ps("ptr7", [128, 8, 128], BF16)
            pup = [kb.ps(f"pup{i}", [128, 512]) for i in range(4)]
            pdn = [kb.ps(f"pdn{i}", [128, 512]) for i in range(2)]
            plg = kb.ps("plg", [128, NEXP])
            tiles = [tt for tt in range(NTILES) if need_ctx or not tile_info(tt)[2]]
            sgs = [tiles[i:i + 8] for i in range(0, len(tiles), 8)]
            nch = 0
            nup = 0
            ndn = 0
            for sg in sgs[:cfg.get("p7_sgs", 99)]:
                ntl = len(sg)
                def load7(i_):
                    tt_ = sg[i_]
                    kb.dma("sp", xt[i_ % 2][:], XS[tt_ * 128:(tt_ + 1) * 128, :], reads=[kb.dbuf(("XS", tt_))], writes=[xt[i_ % 2]])
                load7(0)
                for i, tt in enumerate(sg):
                    b, r, is_ctx, midx = tile_info(tt)
                    x_ = xt[i % 2]
                    xnb = xn[i % 2]
                    if i + 1 < ntl:
                        load7(i + 1)
                    ln_tile(x_[:], x_, xnb[:], xnb, st, mv, rs)

                    def tr(e, xnb=xnb):
                        for k in range(8):
                            ins = e.transpose(ptr[:, k, :], xnb[:, k * 128:(k + 1) * 128], ident[:])
                        return ins
                    kb.op("pe", tr, reads=[xnb, ident], writes=[ptr])
                    kb.op("dve", lambda e, midx=midx: e.tensor_tensor(out=tmpm[:], in0=ptr[:], in1=modT[:, 32:40, midx].unsqueeze(2).to_broadcast([128, 8, 128]), op=ALU.mult),
                          reads=[ptr, modT], writes=[tmpm])
                    kb.op("dve", lambda e, i=i, midx=midx: e.tensor_tensor(out=hT[:, :, i * 128:(i + 1) * 128], in0=tmpm[:], in1=modT[:, 24:32, midx].unsqueeze(2).to_broadcast([128, 8, 128]), op=ALU.add),
                          reads=[tmpm, modT], writes=[hT])
                    if moe:
                        def rl_(e, i=i):
                            for k in range(8):
                                ins = e.matmul(plg[:], hT[:, k, i * 128:(i + 1) * 128], rout[:, k, :], start=(k == 0), stop=(k == 7))
                            return ins
                        kb.op("pe", rl_, reads=[hT, rout], writes=[plg])
                        lg_ = gs_[:, 0, :]
                        kb.op("act", lambda e: e.activation(out=gs_[:, 0, :], in_=plg[:], func=AF.Copy), reads=[plg], writes=[gs_])
                        kb.op("dve", lambda e: e.tensor_reduce(out=gm[:, 0:1], in_=gs_[:, 0, :], axis=AX.X, op=ALU.max), reads=[gs_], writes=[gm])
                        kb.op("dve", lambda e: e.tensor_scalar(out=gs_[:, 1, :], in0=gs_[:, 0, :], scalar1=gm[:, 0:1], scalar2=None, op0=ALU.is_equal), reads=[gs_, gm], writes=[gs_])
                        kb.op("dve", lambda e: e.scalar_tensor_tensor(out=gs_[:, 2, :], in0=gs_[:, 1, :], scalar=-1e30, in1=gs_[:, 0, :], op0=ALU.mult, op1=ALU.add), reads=[gs_], writes=[gs_])
                        kb.op("dve", lambda e: e.tensor_reduce(out=gm[:, 1:2], in_=gs_[:, 2, :], axis=AX.X, op=ALU.max), reads=[gs_], writes=[gm])
                        kb.op("dve", lambda e: e.tensor_scalar(out=gs_[:, 3, :], in0=gs_[:, 0, :], scalar1=gm[:, 1:2], scalar2=None, op0=ALU.is_ge), reads=[gs_, gm], writes=[gs_])
                        kb.op("dve", lambda e: e.tensor_scalar(out=gm[:, 2:3], in0=gm[:, 0:1], scalar1=-1.0, scalar2=None, op0=ALU.mult), reads=[gm], writes=[gm])
                        kb.op("act", lambda e: e.activation(out=gs_[:, 4, :], in_=gs_[:, 0, :], func=AF.Exp, bias=gm[:, 2:3], scale=1.0), reads=[gs_, gm], writes=[gs_])
                        kb.op("dve", lambda e: e.tensor_tensor(out=gs_[:, 5, :], in0=gs_[:, 4, :], in1=gs_[:, 3, :], op=ALU.mult), reads=[gs_], writes=[gs_])
                        kb.op("dve", lambda e: e.tensor_reduce(out=gm[:, 3:4], in_=gs_[:, 5, :], axis=AX.X, op=ALU.add), reads=[gs_], writes=[gm])
                        kb.op("dve", lambda e: e.reciprocal(out=gm[:, 3:4], in_=gm[:, 3:4]), reads=[gm], writes=[gm])
                        kb.op("dve", lambda e, i=i: e.tensor_scalar(out=G[:, i, :], in0=gs_[:, 5, :], scalar1=gm[:, 3:4], scalar2=None, op0=ALU.mult), reads=[gs_, gm], writes=[G])
                first = True
                for (W1, W3, W2, ex) in experts[:cfg.get("p7_exp", 99)]:
                    for ch0 in range(0, nft, 4):
                        nf = min(4, nft - ch0)
                        f0 = ch0 * 128
                        a1 = w1c[nch % 2]
                        a3 = w3c[nch % 2]
                        a2 = w2c[nch % 2]
                        nch += 1
                        if not (cfg.get("p7_nodma") and nch > 2):
                            kb.dma("pool", a1[:, :, 0:nf * 128], W1[:, f0:f0 + nf * 128].rearrange("(k p) f -> p k f", p=128), writes=[a1])
                            kb.dma("pool", a3[:, :, 0:nf * 128], W3[:, f0:f0 + nf * 128].rearrange("(k p) f -> p k f", p=128), writes=[a3])
                            kb.dma("pool", a2[:, 0:nf, :], W2[f0:f0 + nf * 128, :].rearrange("(f p) n -> p f n", p=128), writes=[a2])
                        for tb0 in range(0, ntl, 4):
                            nt = min(4, ntl - tb0)
                            ncol = nt * 128
                            c0 = tb0 * 128
                            ac = act[(nup // 4) % 2]
                            for ft in range(nf):
                                pa = pup[(nup % 2) * 2]
                                pb = pup[(nup % 2) * 2 + 1]
                                s_ = sa[nup % 2]
                                nup += 1

                                def up(e, pa=pa, pb=pb, a1=a1, a3=a3, ft=ft, c0=c0, ncol=ncol):
                                    for (p, w) in ((pa, a1), (pb, a3)):
                                        for k in range(8):
                                            ins = e.matmul(p[:, 0:ncol], w[:, k, ft * 128:(ft + 1) * 128], hT[:, k, c0:c0 + ncol], start=(k == 0), stop=(k == 7))
                                    return ins
                                kb.op("pe", up, reads=[a1, a3, hT], writes=[pa, pb])
                                kb.op("act", lambda e, pa=pa, s_=s_, ncol=ncol: e.activation(out=s_[:, 0:ncol], in_=pa[:, 0:ncol], func=AF.Silu), reads=[pa], writes=[s_])
                                kb.op("dve", lambda e, pb=pb, s_=s_, ac=ac, ft=ft, ncol=ncol: e.tensor_tensor(out=ac[:, ft, 0:ncol], in0=pb[:, 0:ncol], in1=s_[:, 0:ncol], op=ALU.mult), reads=[pb, s_], writes=[ac])
                            for ti in range(nt):
                                i = tb0 + ti
                                for nb in range(2):
                                    po = pdn[ndn % 2]
                                    ndn += 1

                                    def dn(e, po=po, ac=ac, a2=a2, ti=ti, nb=nb, nf=nf):
                                        for ft in range(nf):
                                            ins = e.matmul(po[:], ac[:, ft, ti * 128:(ti + 1) * 128], a2[:, ft, nb * 512:(nb + 1) * 512], start=(ft == 0), stop=(ft == nf - 1))
                                        return ins
                                    kb.op("pe", dn, reads=[ac, a2], writes=[po])
                                    av = acc[:, i, nb * 512:(nb + 1) * 512]
                                    if ex is None:
                                        if first:
                                            kb.op("act", lambda e, po=po, av=av: e.activation(out=av, in_=po[:], func=AF.Copy), reads=[po], writes=[acc])
                                        else:
                                            kb.op("dve", lambda e, po=po, av=av: e.tensor_tensor(out=av, in0=po[:], in1=av, op=ALU.add), reads=[po, acc], writes=[acc])
                                    else:
                                        if first:
                                            kb.op("dve", lambda e, po=po, av=av, i=i, ex=ex: e.tensor_scalar(out=av, in0=po[:], scalar1=G[:, i, ex:ex + 1], scalar2=None, op0=ALU.mult), reads=[po, G], writes=[acc])
                                        else:
                                            kb.op("dve", lambda e, po=po, av=av, i=i, ex=ex: e.scalar_tensor_tensor(out=av, in0=po[:], scalar=G[:, i, ex:ex + 1], in1=av, op0=ALU.mult, op1=ALU.add), reads=[po, G, acc], writes=[acc])
                        first = False
                load7(0)
                for i, tt in enumerate(sg):
                    b, r, is_ctx, midx = tile_info(tt)
                    x_ = xt[i % 2]
                    if i + 1 < ntl:
                        load7(i + 1)
                    dst_ap, dst_buf = out_rows(l, tt)
                    postnorm([acc[:, i, 0:512], acc[:, i, 512:1024]], [acc, acc], x_[:], x_, gate[:, midx, :], gate, lng, tmp, res, st, mv, rs, dst_ap, dst_buf, eng2="dve")

    if cfg.get("only_p7"):
        phase7(1, False)
        layers = []
    for l in layers:
        if stop_after == ("p1",):
            break
        if l == 0:
            phase2(l, XZ, "XZ")
        else:
            phase2(l, XS, "XS")
        if stop_after == ("p2", l):
            break
        need_ctx = l < DEPTH - 1
        if "att" not in cfg.get("skip", ()):
            phase3(l, need_ctx)
        if "ret" not in cfg.get("skip", ()):
            phase4(l, need_ctx)
        if stop_after == ("p4", l):
            break
        if "s5" not in cfg.get("skip", ()):
            phase5(l)
        if stop_after == ("p5", l):
            break
        phase6(l, XZ if l == 0 else XS, "XZ" if l == 0 else "XS", need_ctx)
        if stop_after == ("p6", l):
            break
        phase7(l, need_ctx)
        if stop_after == ("p7", l):
            break

    kb.barrier()
    kb.emit()
    return nc


def prep_shared(inp):
    sh = {}
    sh["w_mod"] = np.ascontiguousarray(inp["w_mod"], dtype=np.float32)
    sh["b_mod"] = np.ascontiguousarray(inp["b_mod"], dtype=np.float32)
    cols = win_columns()
    sh["w_in_p"] = np.ascontiguousarray(inp["w_in"][:, :, cols], dtype=np.float32)
    tab, tm = rope_tables()
    sh["rope_tab"] = tab
    sh["rope_tm"] = tm
    sh["ident"] = np.eye(128, dtype=np.float32)
    kj = np.arange(128)[:, None]
    qi = np.arange(128)[None, :]
    mP = (kj >= qi).astype(np.float32)
    mN = (kj <= qi).astype(np.float32)
    sh["att_mask"] = np.ascontiguousarray(np.stack([np.broadcast_to(mP[:, None, :], (128, 4, 128)), np.broadcast_to(mN[:, None, :], (128, 4, 128))], axis=1))
    sh["attn_sink"] = np.ascontiguousarray(inp["attn_sink"], dtype=np.float32)
    sh["ret_d12"] = np.ascontiguousarray(np.stack([np.maximum(qi - kj, 0), np.maximum(kj - qi, 0)], axis=1).astype(np.float32))
    sh["ret_jc"] = np.ascontiguousarray(np.stack([127 - np.arange(128), np.arange(128)], axis=1).astype(np.float32))
    sh["ret_irow"] = np.ascontiguousarray(np.broadcast_to(np.stack([np.arange(128) + 1, 128 - np.arange(128)], axis=0)[None], (128, 2, 128)).astype(np.float32))
    lg = np.asarray(inp["ret_log_gamma"], dtype=np.float32)
    sh["ret_lgb"] = np.ascontiguousarray(np.broadcast_to(lg.reshape(DEPTH, 1, 8), (DEPTH, 128, 8)))
    lgp = np.zeros((DEPTH, 128, 2, 2), np.float32)
    for p_ in range(128):
        for c_ in range(2):
            lgp[:, p_, c_, :] = lg[:, :, 2 * c_ + p_ // 64]
    sh["ret_lgp"] = lgp
    def col(a):
        a = np.asarray(a, dtype=np.float32)
        sh_ = a.shape
        a = a.reshape(sh_[0], 2, 8, 2, 64, *sh_[4:])
        a = np.moveaxis(a, (3, 4), (1, 2))
        return np.ascontiguousarray(a.reshape(sh_[0], 128, 16, *sh_[4:]))
    lst = np.broadcast_to(np.asarray(inp["s5_log_step"], np.float32)[:, :, :, None], (DEPTH, 2, 16, 64))
    sh["s5_par"] = np.ascontiguousarray(np.stack([col(inp["s5_lam_re"]), col(inp["s5_lam_im"]), col(lst)], axis=2))
    sh["s5_b"] = np.ascontiguousarray(np.stack([col(inp["s5_b_re"]), col(inp["s5_b_im"])], axis=2))
    cre = np.swapaxes(np.asarray(inp["s5_c_re"], np.float32), 3, 4)
    cim = np.swapaxes(np.asarray(inp["s5_c_im"], np.float32), 3, 4)
    sh["s5_c"] = np.ascontiguousarray(np.stack([col(cre), col(cim)], axis=2))
    jv = list(range(9)) + [8 * (m_ + 1) for m_ in range(1, 16)] + [120 - 8 * i_ for i_ in range(15)]
    sh["s5_jt"] = np.ascontiguousarray(np.broadcast_to(np.asarray(jv, dtype=np.float32)[None, :, None], (128, S5NJ, 16)))
    sh["s5_dcol"] = np.ascontiguousarray(np.asarray(inp["s5_d"], np.float32).reshape(DEPTH, 2, 128).transpose(0, 2, 1))
    sh["s5_w_glu"] = np.ascontiguousarray(inp["s5_w_glu"], dtype=np.float32)
    sh["s5_bglu"] = np.ascontiguousarray(np.asarray(inp["s5_b_glu"], np.float32).reshape(DEPTH, 2, 128).transpose(0, 2, 1))
    sh["w_out"] = np.ascontiguousarray(inp["w_out"], dtype=np.float32)
    sh["ln_gb"] = np.ascontiguousarray(np.stack([inp["ln1_g"], inp["ln1_b"], inp["ln2_g"], inp["ln2_b"]], axis=1), dtype=np.float32)
    for k_ in ("ffn_w1", "ffn_w3", "ffn_w2", "moe_router", "moe_w1", "moe_w3", "moe_w2"):
        sh[k_] = np.ascontiguousarray(inp[k_], dtype=np.float32)
    return sh


def prep_core(inp, c):
    b0 = c * NB
    xz = np.concatenate([np.concatenate([inp["ctx"][b0 + i], inp["x"][b0 + i]], axis=0) for i in range(NB)], axis=0)
    cv = np.stack([inp["c"][b0], inp["c"][b0 + 1], inp["c_ctx"]], axis=0)
    cT = np.ascontiguousarray(cv.reshape(3, 8, 128).transpose(2, 1, 0))
    return {"xz": np.ascontiguousarray(xz, dtype=np.float32), "cT": cT.astype(np.float32)}


def kernel(**inputs):
    inp = {k: np.asarray(v) for k, v in inputs.items()}
    nc = build()
    sh = prep_shared(inp)
    in_maps = []
    for c in range(NCORES):
        m = dict(sh)
        m.update(prep_core(inp, c))
        in_maps.append(m)
    res = run_bass_kernel_spmd(nc, in_maps, core_ids=list(range(NCORES)))
    out = np.concatenate([r["out"].reshape(NB, TSEQ, D) for r in res.results], axis=0)
    return out.astype(np.float32)
```

```python
import contextlib
import math
import numpy as np
import concourse.bass as bass
import concourse.mybir as mybir
from concourse.bass_utils import run_bass_kernel_spmd

F32 = mybir.dt.float32
BF16 = mybir.dt.bfloat16
AF = mybir.ActivationFunctionType
ALU = mybir.AluOpType
AX = mybir.AxisListType

D = 1024
NB = 2
LCTX = 256
TSEQ = 2048
TPB = LCTX + TSEQ
NTOK = NB * TPB
TILES_PB = TPB // 128
NTILES = NTOK // 128
DEPTH = 2
DFF = 2816
NEXP = 8
DFFE = 3584
ALPHA = (2 * DEPTH) ** 0.25
LN_EPS = 1e-5
NCORES = 8
WINC = 3456
TWO_PI = 2.0 * math.pi
S5NJ = 39

ENGS = ("pe", "act", "dve", "pool", "sp")
NDMA = {"sp": 24, "pool": 16, "act": 8}


class Buf:
    __slots__ = ("ap", "lastw", "readers", "name")

    def __init__(self, ap, name=""):
        self.ap = ap
        self.lastw = None
        self.readers = []
        self.name = name

    def __getitem__(self, k):
        return self.ap[k]


class _Rec:
    def __init__(self):
        self.calls = []

    def __getattr__(self, name):
        def f(*a, **k):
            self.calls.append((name, a, k))
            return None
        return f


class KB:
    def __init__(self, nc):
        self.nc = nc
        self.es = contextlib.ExitStack()
        self.base_es = self.es
        self.ops = {e: [] for e in ENGS}
        self.cnt = {e: 0 for e in ENGS}
        self.sem = {}
        for e in ("pe", "act", "dve", "pool"):
            self.sem[e] = self.es.enter_context(nc.semaphore("s_" + e))
        self.dsem = {}
        for q, n in NDMA.items():
            self.dsem[q] = [[self.es.enter_context(nc.semaphore(f"d_{q}{i}")), 0] for i in range(n)]
        self.drr = {q: 0 for q in NDMA}
        self.waited = {e: {} for e in ENGS}
        self.dram_bufs = {}
        self.uid = 0

    def sb(self, name, shape, dtype):
        self.uid += 1
        t = self.es.enter_context(self.nc.sbuf_tensor(f"{name}_{self.uid}", list(shape), dtype))
        return Buf(t, name)

    def ps(self, name, shape, dtype=F32):
        self.uid += 1
        t = self.es.enter_context(self.nc.psum_tensor(f"{name}_{self.uid}", list(shape), dtype))
        return Buf(t, name)

    def dbuf(self, key):
        b = self.dram_bufs.get(key)
        if b is None:
            b = Buf(None, str(key))
            self.dram_bufs[key] = b
        return b

    @contextlib.contextmanager
    def phase(self):
        old = self.es
        with contextlib.ExitStack() as es:
            self.es = es
            yield
            self.es = old
        self.barrier()

    def _deps(self, eng, reads, writes):
        toks = []
        for b in reads:
            if b.lastw is not None:
                toks.append(b.lastw)
        for b in writes:
            if b.lastw is not None:
                toks.append(b.lastw)
            toks.extend(b.readers)
        need = {}
        for (kind, s, v, src) in toks:
            if kind == "eng" and src == eng and eng == "pe":
                continue
            k = id(s)
            if k not in need or need[k][1] < v:
                need[k] = (s, v)
        out = []
        w = self.waited[eng]
        for k, (s, v) in need.items():
            if w.get(k, 0) >= v:
                continue
            w[k] = v
            out.append((s, v))
        return out

    def _commit(self, tok, reads, writes):
        for b in reads:
            b.readers.append(tok)
            if len(b.readers) > 96:
                b.readers = b.readers[-64:]
        for b in writes:
            b.lastw = tok
            b.readers = []

    def op(self, eng, fn, reads=(), writes=()):
        waits = self._deps(eng, reads, writes)
        self.cnt[eng] += 1
        tok = ("eng", self.sem[eng], self.cnt[eng], eng)
        rec = _Rec()
        fn(rec)
        assert rec.calls
        self.ops[eng].append((waits, rec.calls, (self.sem[eng], 1)))
        self._commit(tok, reads, writes)
        return tok

    def dma(self, q, out, in_, reads=(), writes=(), **kw):
        pool = self.dsem[q]
        i = self.drr[q]
        self.drr[q] = (i + 1) % len(pool)
        ent = pool[i]
        waits = self._deps(q, reads, writes)
        w = self.waited[q]
        if ent[1] > 0 and w.get(id(ent[0]), 0) < ent[1]:
            w[id(ent[0])] = ent[1]
            waits.append((ent[0], ent[1]))
        ent[1] += 16
        tok = ("dma", ent[0], ent[1], q)
        kw2 = dict(kw)
        kw2["out"] = out
        kw2["in_"] = in_
        self.ops[q].append((waits, [("dma_start", (), kw2)], (ent[0], 16)))
        self._commit(tok, reads, writes)
        return tok

    def barrier(self):
        targets = []
        for e in ("pe", "act", "dve", "pool"):
            if self.cnt[e] > 0:
                targets.append((self.sem[e], self.cnt[e]))
        for q in NDMA:
            for ent in self.dsem[q]:
                if ent[1] > 0:
                    targets.append((ent[0], ent[1]))
        for e in ENGS:
            w = self.waited[e]
            waits = []
            for (s, v) in targets:
                if w.get(id(s), 0) < v:
                    w[id(s)] = v
                    waits.append((s, v))
            if waits:
                self.ops[e].append((waits, None, None))

    def emit(self):
        nc = self.nc
        with nc.Block() as block:
            def run(e, h):
                for (waits, fn, inc) in self.ops[e]:
                    for (s, v) in waits:
                        h.wait_ge(s, v)
                    if fn is not None:
                        for (m_, a_, k_) in fn:
                            ins = getattr(h, m_)(*a_, **k_)
                        ins.then_inc(inc[0], inc[1])

            @block.tensor
            def _(h):
                run("pe", h)

            @block.scalar
            def _(h):
                run("act", h)

            @block.vector
            def _(h):
                run("dve", h)

            @block.gpsimd
            def _(h):
                run("pool", h)

            @block.sync
            def _(h):
                run("sp", h)
        self.base_es.close()


def _att_partner_perm():
    d = np.arange(64)
    return np.where((d % 32) < 16, d + 16, d - 16)


def _ret_partner_perm():
    d = np.arange(64)
    return np.where(d < 32, d + 32, d - 32)


def win_columns():
    pa = _att_partner_perm()
    pr = _ret_partner_perm()
    cols = []
    cols += list(range(0, 256))
    for j in range(4):
        cols += [256 + j * 64 + d for d in range(64)] + [256 + (4 + j) * 64 + d for d in range(64)]
    for j in range(4):
        cols += [256 + j * 64 + pa[d] for d in range(64)] + [256 + (4 + j) * 64 + pa[d] for d in range(64)]
    cols += [768 + g * 64 + d for g in range(2) for d in range(64)]
    cols += [768 + g * 64 + pa[d] for g in range(2) for d in range(64)]
    cols += [1024 + h * 64 + d for h in range(4) for d in range(64)]
    cols += [1024 + h * 64 + pr[d] for h in range(4) for d in range(64)]
    cols += [1280 + h * 64 + d for h in range(4) for d in range(64)]
    cols += [1280 + h * 64 + pr[d] for h in range(4) for d in range(64)]
    cols += list(range(896, 1024))
    cols += list(range(1536, 1792))
    cols += list(range(1792, 2048))
    cols += list(range(1280, 1536))
    assert len(cols) == WINC
    return np.asarray(cols)


def rope_tables():
    t = np.arange(TSEQ)
    rows = (t // 64).astype(np.float32)
    colsp = (t % 64).astype(np.float32)
    pos = t.astype(np.float32)
    f16 = (10000.0 ** (-np.arange(0, 32, 2, dtype=np.float32) / 32)).astype(np.float32)
    f32_ = (10000.0 ** (-np.arange(0, 64, 2, dtype=np.float32) / 64)).astype(np.float32)
    d = np.arange(64)
    ang = np.where((d < 32)[:, None], rows[None, :] * f16[(d % 16)][:, None], colsp[None, :] * f16[(d % 16)][:, None]).astype(np.float32)
    sgn = np.where((d % 32) < 16, -1.0, 1.0).astype(np.float32)[:, None]
    att_cos = np.ones((64, TPB), np.float32)
    att_sin = np.zeros((64, TPB), np.float32)
    att_cos[:, LCTX:] = np.cos(ang)
    att_sin[:, LCTX:] = np.sin(ang) * sgn
    angr = (pos[None, :] * f32_[(d % 32)][:, None]).astype(np.float32)
    sgr = np.where(d < 32, -1.0, 1.0).astype(np.float32)[:, None]
    ret_cos = np.ones((64, TPB), np.float32)
    ret_sin = np.zeros((64, TPB), np.float32)
    ret_cos[:, LCTX:] = np.cos(angr)
    ret_sin[:, LCTX:] = np.sin(angr) * sgr
    tab = np.stack([np.tile(att_cos, (2, 1)), np.tile(att_sin, (2, 1)),
                    np.tile(ret_cos, (2, 1)), np.tile(ret_sin, (2, 1))], axis=1)
    tm = np.zeros((TPB, 2, 32), np.float32)
    tm[:, 0, :] = 1.0
    a2 = pos[:, None] * f32_[None, :]
    tm[LCTX:, 0, :] = np.cos(a2)
    tm[LCTX:, 1, :] = np.sin(a2)
    tm = tm.reshape(TILES_PB, 128, 2, 1, 32).transpose(1, 0, 2, 3, 4)
    tm = np.broadcast_to(tm, (128, TILES_PB, 2, 4, 32))
    return np.ascontiguousarray(tab), np.ascontiguousarray(tm)


def tile_info(tt):
    b = tt // TILES_PB
    r = tt % TILES_PB
    is_ctx = r < 2
    midx = 2 if is_ctx else b
    return b, r, is_ctx, midx


TOKEN_GROUPS = []
for _b in range(NB):
    for _s in (0, 4, 8, 12, 16):
        TOKEN_GROUPS.append(list(range(_b * TILES_PB + _s, _b * TILES_PB + min(_s + 4, TILES_PB))))


def build(cfg=None):
    cfg = cfg or {}
    dump = set(cfg.get("dump", ()))
    layers = cfg.get("layers", list(range(DEPTH)))
    stop_after = cfg.get("stop_after", None)
    nc = bass.Bass("TRN2", target_bir_lowering=False)

    def din(name, shape, dt=F32):
        return nc.dram_tensor(name, list(shape), dt, kind="ExternalInput").ap()

    def dscr(name, shape, dt):
        kind = "ExternalOutput" if name in dump else "Internal"
        return nc.dram_tensor(name, list(shape), dt, kind=kind).ap()

    XZ = din("xz", [NTOK, D])
    CT = din("cT", [128, 8, 3])
    WMOD = din("w_mod", [DEPTH, D, 6 * D])
    BMOD = din("b_mod", [DEPTH, 6 * D])
    WIN = din("w_in_p", [DEPTH, D, WINC])
    ROPE = din("rope_tab", [128, 4, TPB])
    ROPETM = din("rope_tm", [128, TILES_PB, 2, 4, 32])
    IDENT = din("ident", [128, 128])
    AMASK = din("att_mask", [128, 2, 4, 128])
    SINK = din("attn_sink", [DEPTH, 8])
    RD12 = din("ret_d12", [128, 2, 128])
    RJC = din("ret_jc", [128, 2])
    RIROW = din("ret_irow", [128, 2, 128])
    LGB = din("ret_lgb", [DEPTH, 128, 8])
    LGP = din("ret_lgp", [DEPTH, 128, 2, 2])
    S5P = din("s5_par", [DEPTH, 128, 3, 16])
    S5B = din("s5_b", [DEPTH, 128, 2, 16, 16])
    S5C = din("s5_c", [DEPTH, 128, 2, 16, 16])
    S5JT = din("s5_jt", [128, S5NJ, 16])
    S5D = din("s5_dcol", [DEPTH, 128, 2])
    WGLU = din("s5_w_glu", [DEPTH, 256, 256])
    BGLU = din("s5_bglu", [DEPTH, 128, 2])
    WOUT = din("w_out", [DEPTH, D, D])
    LNG = din("ln_gb", [DEPTH, 4, D])
    FW1 = din("ffn_w1", [1, D, DFF])
    FW3 = din("ffn_w3", [1, D, DFF])
    FW2 = din("ffn_w2", [1, DFF, D])
    ROUT = din("moe_router", [1, D, NEXP])
    if cfg.get("small_moe"):
        MW1 = MW3 = MW2 = None
    else:
        MW1 = din("moe_w1", [1, NEXP, D, DFFE])
        MW3 = din("moe_w3", [1, NEXP, D, DFFE])
        MW2 = din("moe_w2", [1, NEXP, DFFE, D])
    OUT = nc.dram_tensor("out", [NB * TSEQ, D], F32, kind="ExternalOutput").ap()

    XS = dscr("XS", [NTOK, D], F32)
    MODR = dscr("MODR", [DEPTH, 3, 6 * D], F32)
    PF = dscr("PF", [11, 128, NTOK], BF16)
    PT = dscr("PT", [NTOK, 896], BF16)
    YA = dscr("YA", [8, 64, NTOK], BF16)
    YR = dscr("YR", [2, 128, NTOK], BF16)
    YS = dscr("YS", [2, 128, NTOK], BF16)

    kb = KB(nc)

    ident = kb.sb("ident", [128, 128], BF16)
    kb.dma("pool", ident[:], IDENT[:, :], writes=[ident])
    identf = kb.sb("identf", [128, 128], F32)
    kb.dma("sp", identf[:], IDENT[:, :], writes=[identf])
    epsc = kb.sb("epsc", [128, 1], F32)
    kb.op("dve", lambda e: e.memset(epsc[:], LN_EPS), writes=[epsc])

    with kb.phase():
        ct = kb.sb("ct", [128, 8, 3], F32)
        kb.dma("sp", ct[:], CT[:, :, :], writes=[ct])
        silT = kb.sb("silT", [128, 8, 3], BF16)
        kb.op("act", lambda e: e.activation(out=silT[:], in_=ct[:], func=AF.Silu), reads=[ct], writes=[silT])
        wm = [kb.sb(f"wm{i}", [128, 8, 512], BF16) for i in range(2)]
        bm = kb.sb("bm", [3, 6 * D], F32)
        modrow = kb.sb("modrow", [3, 6 * D], F32)
        pm = [kb.ps(f"pm{i}", [3, 512]) for i in range(2)]
        for l in range(DEPTH):
            kb.dma("sp", bm[:].unsqueeze(1), BMOD[l:l + 1, :].partition_broadcast(3), writes=[bm])
            for cb in range(12):
                w = wm[cb % 2]
                p = pm[cb % 2]
                kb.dma("pool", w[:], WMOD[l].rearrange("(k p) n -> p k n", p=128)[:, :, cb * 512:(cb + 1) * 512], writes=[w])

                def mm(e, w=w, p=p):
                    for k in range(8):
                        ins = e.matmul(p[:], silT[:, k, :], w[:, k, :], start=(k == 0), stop=(k == 7))
                    return ins
                kb.op("pe", mm, reads=[w, silT], writes=[p])
                kb.op("dve", lambda e, p=p, cb=cb: e.tensor_tensor(out=modrow[:, cb * 512:(cb + 1) * 512], in0=p[:], in1=bm[:, cb * 512:(cb + 1) * 512], op=ALU.add),
                      reads=[p, bm], writes=[modrow])
            kb.dma("sp", MODR[l], modrow[:], reads=[modrow], writes=[kb.dbuf(("MODR", l))])

    def load_modT(l, modT):
        for i in range(3):
            kb.dma("sp", modT[:, :, i], MODR[l, i].rearrange("(c p) -> p c", p=128), reads=[kb.dbuf(("MODR", l))], writes=[modT],
                   allow_slow_non_contiguous=True)
        for c0 in (8, 32):
            kb.op("dve", lambda e, c0=c0: e.tensor_scalar(out=modT[:, c0:c0 + 8, :], in0=modT[:, c0:c0 + 8, :], scalar1=1.0, scalar2=None, op0=ALU.add),
                  reads=[modT], writes=[modT])

    def ln_tile(xt_ap, xt_buf, xn_ap, xn_buf, st, mv, rs):
        for h in range(2):
            kb.op("dve", lambda e, h=h: e.bn_stats(out=st[:, h, :], in_=xt_ap[:, h * 512:(h + 1) * 512]), reads=[xt_buf], writes=[st])
        kb.op("dve", lambda e: e.bn_aggr(out=mv[:], in_=st[:].rearrange("p a b -> p (a b)")), reads=[st], writes=[mv])
        kb.op("act", lambda e: e.activation(out=rs[:, 0:1], in_=mv[:, 1:2], func=AF.Sqrt, bias=epsc[:], scale=1.0), reads=[mv, epsc], writes=[rs])
        kb.op("dve", lambda e: e.reciprocal(out=rs[:, 1:2], in_=rs[:, 0:1]), reads=[rs], writes=[rs])
        kb.op("dve", lambda e: e.tensor_scalar(out=xn_ap, in0=xt_ap, scalar1=mv[:, 0:1], scalar2=rs[:, 1:2], op0=ALU.subtract, op1=ALU.mult),
              reads=[xt_buf, mv, rs], writes=[xn_buf])

    def phase2(l, SRC, src_key):
        with kb.phase():
            modT = kb.sb("modT", [128, 48, 3], F32)
            load_modT(l, modT)
            win = kb.sb("win", [128, 8, WINC], BF16)
            for k in range(8):
                kb.dma("pool", win[:, k, :], WIN[l, k * 128:(k + 1) * 128, :], writes=[win])
            rope = kb.sb("rope", [128, 4, TPB], F32)
            kb.dma("sp", rope[:], ROPE[:, :, :], writes=[rope])
            ropetm = kb.sb("ropetm", [128, TILES_PB, 2, 4, 32], F32)
            kb.dma("sp", ropetm[:], ROPETM[:, :, :, :, :], writes=[ropetm])
            xt = [kb.sb(f"xt{i}", [128, 4, D], F32) for i in range(2)]
            xn = [kb.sb(f"xn{i}", [128, D], BF16) for i in range(2)]
            st = kb.sb("st", [128, 2, 6], F32)
            mv = kb.sb("mv", [128, 2], F32)
            rs = kb.sb("rs", [128, 2], F32)
            tmpm = [kb.sb(f"tmpm{i}", [128, 8, 128], F32) for i in range(2)]
            hT = [kb.sb(f"hT{i}", [128, 8, 512], BF16) for i in range(2)]
            ptr = [kb.ps(f"ptr{i}", [128, 8, 128], BF16) for i in range(2)]
            pf = [kb.ps(f"pf{i}", [128, 512]) for i in range(4)]
            ptk = [kb.ps(f"ptk{i}", [128, 512]) for i in range(2)]
            t1 = [kb.sb(f"t1_{i}", [128, 512], F32) for i in range(2)]
            t2 = [kb.sb(f"t2_{i}", [128, 512], F32) for i in range(2)]
            stf = [kb.sb(f"stf{i}", [128, 512], BF16) for i in range(4)]
            stt = [kb.sb(f"stt{i}", [128, 896], BF16) for i in range(2)]
            kt = [kb.sb(f"kt{i}", [128, 4, 4, 32], F32) for i in range(2)]
            kcp = [kb.sb(f"kcp{i}", [128, 256], F32) for i in range(2)]
            SRCv = SRC.rearrange("(t p) d -> p t d", p=128)
            nfe = 0
            lim = cfg.get("p2_lim", 99)
            groups2 = TOKEN_GROUPS[:cfg.get("p2_groups", 99)]

            def load_x2(gi_):
                grp_ = groups2[gi_]
                kb.dma("sp", xt[gi_ % 2][:, 0:len(grp_), :], SRCv[:, grp_[0]:grp_[0] + len(grp_), :], reads=[kb.dbuf((src_key, g)) for g in grp_], writes=[xt[gi_ % 2]])
            nfe_box = [0]

            def stageA(gi):
                grp = groups2[gi]
                ng = len(grp)
                ncol = ng * 128
                x = xt[gi % 2]
                h = hT[gi % 2]
                if gi + 1 < len(groups2):
                    load_x2(gi + 1)
                b0, r0, _, _ = tile_info(grp[0])
                if lim < 2:
                    return
                for i, tt in enumerate(grp):
                    b, r, is_ctx, midx = tile_info(tt)
                    xnb = xn[tt % 2]
                    ln_tile(x[:, i, :], x, xnb[:], xnb, st, mv, rs)
                    if lim < 3:
                        return
                    p = ptr[tt % 2]

                    def tr(e, xnb=xnb, p=p):
                        for k in range(8):
                            ins = e.transpose(p[:, k, :], xnb[:, k * 128:(k + 1) * 128], ident[:])
                        return ins
                    kb.op("pe", tr, reads=[xnb, ident], writes=[p])
                    tm_ = tmpm[tt % 2]
                    kb.op("dve", lambda e, p=p, tm_=tm_, midx=midx: e.tensor_tensor(out=tm_[:], in0=p[:], in1=modT[:, 8:16, midx].unsqueeze(2).to_broadcast([128, 8, 128]), op=ALU.mult),
                          reads=[p, modT], writes=[tm_])
                    kb.op("pool", lambda e, tm_=tm_, h=h, i=i, midx=midx: e.tensor_tensor(out=h[:, :, i * 128:(i + 1) * 128], in0=tm_[:], in1=modT[:, 0:8, midx].unsqueeze(2).to_broadcast([128, 8, 128]), op=ALU.add),
                          reads=[tm_, modT], writes=[h])

            def stageB(gi):
                grp = groups2[gi]
                ng = len(grp)
                ncol = ng * 128
                h = hT[gi % 2]
                b0, r0, _, _ = tile_info(grp[0])
                nfe = nfe_box[0]
                if lim < 4:
                    return
                c0 = r0 * 128
                col0 = grp[0] * 128

                def proj(e, p, ci, h=h, ncol=ncol):
                    for k in range(8):
                        ins = e.matmul(p[:, 0:ncol], win[:, k, ci * 128:(ci + 1) * 128], h[:, k, 0:ncol], start=(k == 0), stop=(k == 7))
                    return ins
                for ci in (0, 1):
                    p = pf[nfe % 4]
                    s = stf[nfe % 4]
                    nfe += 1
                    kb.op("pe", lambda e, p=p, ci=ci, f=proj: f(e, p, ci), reads=[win, h], writes=[p])
                    kb.op("act", lambda e, p=p, s=s, ncol=ncol: e.activation(out=s[:, 0:ncol], in_=p[:, 0:ncol], func=AF.Copy), reads=[p], writes=[s])
                    kb.dma("sp", PF[ci, :, col0:col0 + ncol], s[:, 0:ncol], reads=[s], writes=[kb.dbuf(("PF", ci, gi))])
                pairs = [(2 + j, 6 + j, 2 + j, 0) for j in range(4)] + [(10, 11, 6, 0)] + \
                        [(12 + j, 14 + j, 7 + j, 2) for j in range(2)] + [(16 + j, 18 + j, 9 + j, 2) for j in range(2)]
                for (ca, cr, oi, tb) in pairs:
                    pa = pf[nfe % 4]
                    pr_ = pf[(nfe + 1) % 4]
                    s = stf[nfe % 4]
                    a1 = t1[(nfe // 2) % 2]
                    a2 = t2[(nfe // 2) % 2]
                    nfe += 2
                    kb.op("pe", lambda e, p=pa, ci=ca, f=proj: f(e, p, ci), reads=[win, h], writes=[pa])
                    kb.op("pe", lambda e, p=pr_, ci=cr, f=proj: f(e, p, ci), reads=[win, h], writes=[pr_])
                    kb.op("dve", lambda e, pa=pa, a1=a1, tb=tb, ncol=ncol, c0=c0: e.tensor_tensor(out=a1[:, 0:ncol], in0=pa[:, 0:ncol], in1=rope[:, tb, c0:c0 + ncol], op=ALU.mult),
                          reads=[pa, rope], writes=[a1])
                    kb.op("dve", lambda e, pr_=pr_, a2=a2, tb=tb, ncol=ncol, c0=c0: e.tensor_tensor(out=a2[:, 0:ncol], in0=pr_[:, 0:ncol], in1=rope[:, tb + 1, c0:c0 + ncol], op=ALU.mult),
                          reads=[pr_, rope], writes=[a2])
                    kb.op("pool", lambda e, a1=a1, a2=a2, s=s, ncol=ncol: e.tensor_tensor(out=s[:, 0:ncol], in0=a1[:, 0:ncol], in1=a2[:, 0:ncol], op=ALU.add),
                          reads=[a1, a2], writes=[s])
                    kb.dma("sp", PF[oi, :, col0:col0 + ncol], s[:, 0:ncol], reads=[s], writes=[kb.dbuf(("PF", oi, gi))])
                if lim < 5:
                    return
                for i, tt in enumerate(grp):
                    b, r, is_ctx, midx = tile_info(tt)
                    p1 = ptk[0]
                    p2 = ptk[1]
                    s = stt[tt % 2]
                    k_ = kt[tt % 2]

                    def tproj(e, p, c_lo, n, i=i, h=h):
                        for k in range(8):
                            ins = e.matmul(p[:, 0:n], h[:, k, i * 128:(i + 1) * 128], win[:, k, c_lo:c_lo + n], start=(k == 0), stop=(k == 7))
                        return ins
                    kb.op("pe", lambda e, p1=p1, f=tproj: f(e, p1, 2560, 512), reads=[win, h], writes=[p1])
                    kb.op("pe", lambda e, p2=p2, f=tproj: f(e, p2, 3072, 384), reads=[win, h], writes=[p2])
                    kb.op("act", lambda e, p1=p1, s=s: e.activation(out=s[:, 0:512], in_=p1[:], func=AF.Copy), reads=[p1], writes=[s])
                    kb.op("act", lambda e, p2=p2, s=s: e.activation(out=s[:, 512:640], in_=p2[:, 0:128], func=AF.Copy), reads=[p2], writes=[s])
                    tml = cfg.get("tm_lim", 99)
                    if tml < 2:
                        return
                    kc_ = kcp[tt % 2]
                    kb.op("act", lambda e, p2=p2, kc_=kc_: e.activation(out=kc_[:], in_=p2[:, 128:384], func=AF.Copy), reads=[p2], writes=[kc_])
                    kv = kc_[:].rearrange("p (h a f) -> p h a f", h=4, a=2)
                    cosb = ropetm[:, r, 0, :, :]
                    if cfg.get('dbgA'):
                        cosb = rope[:, 0, 0:128].rearrange('p (h f) -> p h f', h=4)
                    sinb = ropetm[:, r, 1, :, :]
                    for j, (src_a, tb_) in enumerate(((0, cosb), (1, sinb), (0, sinb), (1, cosb))[:cfg.get('tmj', 4)]):
                        kb.op("dve", lambda e, j=j, src_a=src_a, tb_=tb_, k_=k_, kv=kv: e.tensor_tensor(out=k_[:, :, j, :], in0=kv[:, :, src_a, :], in1=tb_, op=ALU.mult),
                              reads=[kc_, ropetm], writes=[k_])
                    if tml < 3:
                        return
                    so = s[:, 640:896].rearrange("p (h a f) -> p h a f", h=4, a=2)
                    kb.op("pool", lambda e, k_=k_, so=so: e.tensor_tensor(out=so[:, :, 0, :], in0=k_[:, :, 0, :], in1=k_[:, :, 1, :], op=ALU.subtract), reads=[k_], writes=[s])
                    kb.op("pool", lambda e, k_=k_, so=so: e.tensor_tensor(out=so[:, :, 1, :], in0=k_[:, :, 2, :], in1=k_[:, :, 3, :], op=ALU.add), reads=[k_], writes=[s])
                    if tml < 4:
                        return
                    kb.dma("sp", PT[tt * 128:(tt + 1) * 128, :], s[:], reads=[s], writes=[kb.dbuf(("PT", tt))])
                nfe_box[0] = nfe

            load_x2(0)
            stageA(0)
            for gi in range(len(groups2)):
                if gi + 1 < len(groups2):
                    stageA(gi + 1)
                stageB(gi)


    def pf_reads(ci):
        return [kb.dbuf(("PF", ci, gi)) for gi in range(len(TOKEN_GROUPS))]

    def pt_reads(tiles):
        return [kb.dbuf(("PT", tt)) for tt in tiles]

    def phase3(l, need_ctx):
        with kb.phase():
            amask = kb.sb("amask", [128, 2, 4, 128], BF16)
            kb.dma("pool", amask[:], AMASK[:, :, :, :], writes=[amask])
            ones64 = kb.sb("ones64", [128, 64], BF16)
            kb.op("dve", lambda e: e.memset(ones64[:], 1.0), writes=[ones64])
            sk = kb.sb("sk", [1, 8], F32)
            kb.dma("sp", sk[:], SINK[l:l + 1, :], writes=[sk])
            esk = kb.sb("esk", [1, 8], F32)
            kb.op("act", lambda e: e.activation(out=esk[:], in_=sk[:], func=AF.Exp), reads=[sk], writes=[esk])
            esrow = kb.sb("esrow", [1, 8, 128], BF16)
            kb.op("dve", lambda e: e.tensor_copy(out=esrow[:], in_=esk[:].unsqueeze(2).to_broadcast([1, 8, 128])), reads=[esk], writes=[esrow])
            QT = kb.sb("QT", [128, 4, NTOK], BF16)
            KT = kb.sb("KT", [128, NTOK], BF16)
            V = kb.sb("V", [128, NTILES, 128], BF16)
            for j in range(4):
                kb.dma("sp", QT[:, j, :], PF[2 + j, :, :], reads=pf_reads(2 + j), writes=[QT])
            kb.dma("sp", KT[:], PF[6, :, :], reads=pf_reads(6), writes=[KT])
            kb.dma("sp", V[:], PT.rearrange("(t p) c -> p t c", p=128)[:, :, 0:128], reads=pt_reads(range(NTILES)), writes=[V])
            pss = [kb.ps(f"pss{i}", [128, 512]) for i in range(3)]
            po = [kb.ps(f"po{i}", [64, 512]) for i in range(2)]
            pd = [kb.ps(f"pd{i}", [64, 512]) for i in range(2)]
            pT = [kb.sb(f"pT{i}", [128, 512], BF16) for i in range(4)]
            pTm = [kb.sb(f"pTm{i}", [128, 512], BF16) for i in range(3)]
            rden = [kb.sb(f"rden{i}", [64, 512], F32) for i in range(2)]
            ot = [kb.sb(f"ot{i}", [64, 512], BF16) for i in range(2)]
            items = []
            nu = 0
            for b in range(NB):
                for qt in range(TILES_PB):
                    if qt < 2 and not need_ctx:
                        continue
                    keys = [(0, None), (1, None)]
                    if qt >= 2:
                        if qt - 1 >= 2:
                            keys.append((qt - 1, 0))
                        keys.append((qt, None))
                        if qt + 1 < TILES_PB:
                            keys.append((qt + 1, 1))
                    for g in range(2):
                        for idx, (kt_, mk) in enumerate(keys):
                            items.append((b, qt, g, nu, idx, len(keys), kt_, mk))
                        nu += 1
            LA = 2
            nm = 0
            sres = {}
            for it in range(len(items) + LA):
                if it < len(items):
                    (b, qt, g, u, idx, nkeys, kt_, mk) = items[it]
                    gs = slice(g * 64, (g + 1) * 64)
                    qc0 = b * TPB + qt * 128
                    kc0 = b * TPB + kt_ * 128
                    p = pss[it % 3]
                    kb.op("pe", lambda e, p=p, kc0=kc0, gs=gs, qc0=qc0: e.matmul(p[:], KT[gs, kc0:kc0 + 128], QT[gs, :, qc0:qc0 + 128], start=True, stop=True),
                          reads=[KT, QT], writes=[p])
                    t_ = pT[it % 4]
                    kb.op("act", lambda e, p=p, t_=t_: e.activation(out=t_[:], in_=p[:], func=AF.Exp, scale=0.125), reads=[p], writes=[t_])
                    if mk is not None:
                        tm_ = pTm[nm % 3]
                        nm += 1
                        kb.op("pool", lambda e, t_=t_, tm_=tm_, mk=mk: e.tensor_tensor(out=tm_[:], in0=t_[:], in1=amask[:, mk, :, :].rearrange("p a b -> p (a b)"), op=ALU.mult),
                              reads=[t_, amask], writes=[tm_])
                        t_ = tm_
                    sres[it] = t_
                j = it - LA
                if j < 0:
                    continue
                (b, qt, g, u, idx, nkeys, kt_, mk) = items[j]
                gs = slice(g * 64, (g + 1) * 64)
                qc0 = b * TPB + qt * 128
                t_ = sres.pop(j)
                o_ps = po[u % 2]
                d_ps = pd[u % 2]
                kb.op("pe", lambda e, t_=t_, kt_=kt_, idx=idx, b=b, gs=gs, o_ps=o_ps, nkeys=nkeys: e.matmul(o_ps[:], V[:, b * TILES_PB + kt_, gs], t_[:], start=(idx == 0), stop=(idx == nkeys - 1)),
                      reads=[V, t_], writes=[o_ps])
                kb.op("pe", lambda e, t_=t_, idx=idx, d_ps=d_ps: e.matmul(d_ps[:], ones64[:], t_[:], start=(idx == 0), stop=False),
                      reads=[ones64, t_], writes=[d_ps])
                if idx == nkeys - 1:
                    kb.op("pe", lambda e, d_ps=d_ps, g=g: e.matmul(d_ps[:], ones64[0:1, :], esrow[0:1, g * 4:(g + 1) * 4, :], start=False, stop=True),
                          reads=[ones64, esrow], writes=[d_ps])
                    rd = rden[u % 2]
                    o_ = ot[u % 2]
                    kb.op("dve", lambda e, rd=rd, d_ps=d_ps: e.reciprocal(out=rd[:], in_=d_ps[:]), reads=[d_ps], writes=[rd])
                    kb.op("dve", lambda e, rd=rd, o_=o_, o_ps=o_ps: e.tensor_tensor(out=o_[:], in0=o_ps[:], in1=rd[:], op=ALU.mult), reads=[o_ps, rd], writes=[o_])
                    kb.dma("sp", YA[g * 4:(g + 1) * 4, :, qc0:qc0 + 128].rearrange("h d t -> d h t"), o_[:].rearrange("d (h t) -> d h t", h=4),
                           reads=[o_], writes=[kb.dbuf(("YA", b, qt, g))])

    def phase4(l, need_ctx):
        LN8 = math.log(0.125)
        with kb.phase():
            d12 = kb.sb("d12", [128, 2, 128], F32)
            kb.dma("sp", d12[:], RD12[:, :, :], writes=[d12])
            jc = kb.sb("jc", [128, 2], F32)
            kb.dma("sp", jc[:], RJC[:, :], writes=[jc])
            irow = kb.sb("irow", [128, 2, 128], F32)
            kb.dma("sp", irow[:], RIROW[:, :, :], writes=[irow])
            lgb = kb.sb("lgb", [128, 8], F32)
            kb.dma("sp", lgb[:], LGB[l], writes=[lgb])
            lgp = kb.sb("lgp", [128, 2, 2], F32)
            kb.dma("sp", lgp[:], LGP[l], writes=[lgp])
            ln8 = kb.sb("ln8", [128, 1], F32)
            kb.op("dve", lambda e: e.memset(ln8[:], LN8), writes=[ln8])
            marg = kb.sb("marg", [128, 4, 128], F32)
            M = kb.sb("M", [128, 4, 128], F32)
            for hp in range(4):
                h = (hp % 2) * 2 + hp // 2
                kb.op("dve", lambda e, h=h, hp=hp: e.tensor_scalar(out=marg[:, hp, :], in0=d12[:, 0, :], scalar1=lgb[:, h:h + 1], scalar2=None, op0=ALU.mult), reads=[d12, lgb], writes=[marg])
                kb.op("dve", lambda e, h=h, hp=hp: e.scalar_tensor_tensor(out=marg[:, hp, :], in0=d12[:, 1, :], scalar=lgb[:, 4 + h:5 + h], in1=marg[:, hp, :], op0=ALU.mult, op1=ALU.add),
                      reads=[d12, lgb, marg], writes=[marg])
            kb.op("act", lambda e: e.activation(out=M[:], in_=marg[:], func=AF.Exp, bias=ln8[:], scale=1.0), reads=[marg, ln8], writes=[M])
            warg = kb.sb("warg", [128, 2, 4], F32)
            wk = kb.sb("wk", [128, 2, 4], F32)
            for dr in range(2):
                kb.op("dve", lambda e, dr=dr: e.tensor_scalar(out=warg[:, dr, :], in0=lgb[:, dr * 4:(dr + 1) * 4], scalar1=jc[:, dr:dr + 1], scalar2=None, op0=ALU.mult), reads=[lgb, jc], writes=[warg])
            kb.op("act", lambda e: e.activation(out=wk[:], in_=warg[:], func=AF.Exp, bias=ln8[:], scale=1.0), reads=[warg, ln8], writes=[wk])
            qw = kb.sb("qw", [128, 2, 2, 128], BF16)
            for c in range(2):
                for dr in range(2):
                    kb.op("act", lambda e, c=c, dr=dr: e.activation(out=qw[:, c, dr, :], in_=irow[:, dr, :], func=AF.Exp, scale=lgp[:, c, dr:dr + 1]), reads=[irow, lgp], writes=[qw])
            dec = kb.sb("dec", [128, 2, 2], F32)
            kb.op("act", lambda e: e.activation(out=dec[:], in_=lgp[:].rearrange("p c d -> p d c"), func=AF.Exp, scale=128.0), reads=[lgp], writes=[dec])

            QTr = kb.sb("QTr", [128, 2, TPB], BF16)
            KTr = kb.sb("KTr", [128, 2, TPB], BF16)
            Vr = kb.sb("Vr", [128, TILES_PB, 256], BF16)
            Kr = kb.sb("Kr", [128, TILES_PB, 256], BF16)
            Gr = kb.sb("Gr", [128, TILES_PB, 256], BF16)
            CT = kb.sb("CTs", [128, TILES_PB, 2, 2, 64], F32)
            SA = kb.sb("SA", [128, TILES_PB, 2, 2, 64], BF16)
            srun = [kb.sb(f"srun{i}", [128, 2, 64], F32) for i in range(2)]
            stmp = kb.sb("stmp", [128, 2, 64], F32)
            kw = [kb.sb(f"kw{i}", [128, 2, 256], BF16) for i in range(2)]
            pc = [kb.ps(f"pc{i}", [128, 512]) for i in range(1)]
            psr = [kb.ps(f"psr{i}", [128, 512]) for i in range(4)]
            por = [kb.ps(f"por{i}", [128, 256]) for i in range(2)]
            ptr2 = [kb.ps(f"ptr2{i}", [128, 2, 128], BF16) for i in range(1)]
            PTm = [kb.sb(f"PTm{i}", [128, 4, 128], BF16) for i in range(2)]
            qs = [kb.sb(f"qs{i}", [128, 2, 2, 128], BF16) for i in range(2)]
            o32 = [kb.sb(f"o32{i}", [128, 256], F32) for i in range(2)]
            sq = kb.sb("sq", [128, 256], F32)
            sg = kb.sb("sg", [128, 256], F32)
            stt_ = kb.sb("stats", [128, 6, 4], F32)
            yt = [kb.sb(f"yt{i}", [128, 256], BF16) for i in range(2)]
            yT = [kb.sb(f"yT{i}", [128, 2, 128], BF16) for i in range(2)]
            PTv = PT.rearrange("(t p) c -> p t c", p=128)
            rl = cfg.get("r_lim", 99)
            for b in range(NB):
                if rl < 2:
                    break
                t0 = b * TILES_PB
                c0 = b * TPB
                for c in range(2):
                    kb.dma("sp", QTr[:, c, :], PF[7 + c, :, c0:c0 + TPB], reads=pf_reads(7 + c), writes=[QTr])
                    kb.dma("sp", KTr[:, c, :], PF[9 + c, :, c0:c0 + TPB], reads=pf_reads(9 + c), writes=[KTr])
                kb.dma("sp", Vr[:], PTv[:, t0:t0 + TILES_PB, 128:384], reads=pt_reads(range(t0, t0 + TILES_PB)), writes=[Vr])
                kb.dma("sp", Gr[:], PTv[:, t0:t0 + TILES_PB, 384:640], reads=pt_reads(range(t0, t0 + TILES_PB)), writes=[Gr])
                kb.dma("sp", Kr[:], PTv[:, t0:t0 + TILES_PB, 640:896], reads=pt_reads(range(t0, t0 + TILES_PB)), writes=[Kr])
                for t in range(TILES_PB):
                    k_ = kw[t % 2]
                    for dr in range(2):
                        eng = "dve" if dr == 0 else "pool"
                        kb.op(eng, lambda e, k_=k_, dr=dr, t=t: e.tensor_tensor(out=k_[:, dr, :].rearrange("p (h d) -> p h d", h=4), in0=Kr[:, t, :].rearrange("p (h d) -> p h d", h=4),
                                                                                  in1=wk[:, dr, :].unsqueeze(2).to_broadcast([128, 4, 64]), op=ALU.mult),
                              reads=[Kr, wk], writes=[k_])
                    p = pc[0]

                    def cm(e, p=p, k_=k_, t=t):
                        for dr in range(2):
                            for c in range(2):
                                ins = e.matmul(p[:, (dr * 2 + c) * 128:(dr * 2 + c + 1) * 128], k_[:, dr, c * 128:(c + 1) * 128], Vr[:, t, c * 128:(c + 1) * 128], start=True, stop=True)
                        return ins
                    kb.op("pe", cm, reads=[k_, Vr], writes=[p])
                    pv_ = p[:].rearrange("p (a c) -> p a c", c=128)
                    kb.op("act", lambda e, pv_=pv_, t=t: e.activation(out=CT[0:64, t, :, :, :].rearrange("p a b c -> p (a b) c"), in_=pv_[0:64, :, 0:64], func=AF.Copy), reads=[p], writes=[CT])
                    kb.op("act", lambda e, pv_=pv_, t=t: e.activation(out=CT[64:128, t, :, :, :].rearrange("p a b c -> p (a b) c"), in_=pv_[64:128, :, 64:128], func=AF.Copy), reads=[p], writes=[CT])
                for dr in range(2):
                    if rl < 3:
                        break
                    order = list(range(TILES_PB)) if dr == 0 else [1, 0] + list(range(TILES_PB - 1, 1, -1))
                    cur = srun[0]
                    nxt = srun[1]
                    kb.op("dve", lambda e, cur=cur: e.memset(cur[:], 0.0), writes=[cur])
                    for t in order:
                        kb.op("pool", lambda e, cur=cur, t=t, dr=dr: e.tensor_copy(out=SA[:, t, dr, :, :], in_=cur[:]), reads=[cur], writes=[SA])
                        kb.op("dve", lambda e, cur=cur, dr=dr: e.tensor_tensor(out=stmp[:], in0=cur[:], in1=dec[:, dr, :].unsqueeze(2).to_broadcast([128, 2, 64]), op=ALU.mult),
                              reads=[cur, dec], writes=[stmp])
                        kb.op("dve", lambda e, nxt=nxt, t=t, dr=dr: e.tensor_tensor(out=nxt[:], in0=stmp[:], in1=CT[:, t, dr, :, :], op=ALU.add), reads=[stmp, CT], writes=[nxt])
                        cur, nxt = nxt, cur
                for t in range(TILES_PB):
                    if t < 2 and not need_ctx:
                        continue
                    if rl < 4:
                        continue
                    tc0 = t * 128
                    pA = psr[(t % 2) * 2]
                    pB = psr[(t % 2) * 2 + 1]

                    def sc(e, pA=pA, pB=pB, tc0=tc0):
                        for hl, p in ((0, pA), (1, pB)):
                            hs = slice(hl * 64, hl * 64 + 64)
                            for c in range(2):
                                ins = e.matmul(p[:, c * 128:(c + 1) * 128], KTr[hs, c, tc0:tc0 + 128], QTr[hs, c, tc0:tc0 + 128], start=True, stop=True)
                        return ins
                    kb.op("pe", sc, reads=[KTr, QTr], writes=[pA, pB])
                    if cfg.get("r4", 9) < 2:
                        continue
                    pm_ = PTm[t % 2]
                    for hl, p in ((0, pA), (1, pB)):
                        kb.op("dve", lambda e, p=p, pm_=pm_, hl=hl: e.tensor_tensor(out=pm_[:, hl * 2:hl * 2 + 2, :].rearrange("p a b -> p (a b)"), in0=p[:, 0:256], in1=M[:, hl * 2:hl * 2 + 2, :].rearrange("p a b -> p (a b)"), op=ALU.mult), reads=[p, M], writes=[pm_])
                    q_ = qs[t % 2]
                    if cfg.get("r4", 9) < 3:
                        continue
                    for dr in range(2):
                        for c in range(2):
                            kb.op("pool" if c == 0 else "dve", lambda e, q_=q_, dr=dr, c=c, tc0=tc0: e.tensor_tensor(out=q_[:, c, dr, :], in0=QTr[:, c, tc0:tc0 + 128], in1=qw[:, c, dr, :], op=ALU.mult), reads=[QTr, qw], writes=[q_])
                    if rl < 5:
                        continue
                    o_ps = por[t % 2]

                    def om(e, o_ps=o_ps, pm_=pm_, q_=q_, t=t):
                        for h in range(4):
                            hl = h % 2
                            c = h // 2
                            hs = slice(hl * 64, hl * 64 + 64)
                            e.matmul(o_ps[:, h * 64:(h + 1) * 64], pm_[:, hl * 2 + c, :], Vr[:, t, h * 64:(h + 1) * 64], start=True, stop=False)
                            e.matmul(o_ps[:, h * 64:(h + 1) * 64], q_[hs, c, 0, :], SA[hs, t, 0, c, :], start=False, stop=False)
                            ins = e.matmul(o_ps[:, h * 64:(h + 1) * 64], q_[hs, c, 1, :], SA[hs, t, 1, c, :], start=False, stop=True)
                        return ins
                    kb.op("pe", om, reads=[pm_, q_, Vr, SA], writes=[o_ps])
                    o_ = o32[t % 2]
                    kb.op("act", lambda e, o_=o_, o_ps=o_ps: e.activation(out=o_[:], in_=o_ps[:], func=AF.Copy), reads=[o_ps], writes=[o_])
                    ov = o_[:].rearrange("p (h d) -> p h d", h=4)
                    if rl < 6:
                        continue
                    kb.op("dve", lambda e, ov=ov: e.tensor_reduce(out=stt_[:, 0, :], in_=ov, axis=AX.X, op=ALU.add), reads=[o_], writes=[stt_])
                    kb.op("act", lambda e, o_=o_: e.activation(out=sq[:], in_=o_[:], func=AF.Square), reads=[o_], writes=[sq])
                    kb.op("dve", lambda e: e.tensor_reduce(out=stt_[:, 1, :], in_=sq[:].rearrange("p (h d) -> p h d", h=4), axis=AX.X, op=ALU.add), reads=[sq], writes=[stt_])
                    kb.op("dve", lambda e: e.tensor_scalar(out=stt_[:, 2:4, :], in0=stt_[:, 0:2, :], scalar1=1.0 / 64, scalar2=None, op0=ALU.mult), reads=[stt_], writes=[stt_])
                    kb.op("dve", lambda e: e.tensor_tensor(out=stt_[:, 4, :], in0=stt_[:, 2, :], in1=stt_[:, 2, :], op=ALU.mult), reads=[stt_], writes=[stt_])
                    kb.op("dve", lambda e: e.tensor_tensor(out=stt_[:, 5, :], in0=stt_[:, 3, :], in1=stt_[:, 4, :], op=ALU.subtract), reads=[stt_], writes=[stt_])
                    kb.op("act", lambda e: e.activation(out=stt_[:, 4, :], in_=stt_[:, 5, :], func=AF.Sqrt, bias=epsc[:], scale=1.0), reads=[stt_, epsc], writes=[stt_])
                    kb.op("dve", lambda e: e.reciprocal(out=stt_[:, 5, :], in_=stt_[:, 4, :]), reads=[stt_], writes=[stt_])
                    kb.op("dve", lambda e, ov=ov: e.tensor_tensor(out=ov, in0=ov, in1=stt_[:, 2, :].unsqueeze(2).to_broadcast([128, 4, 64]), op=ALU.subtract), reads=[o_, stt_], writes=[o_])
                    kb.op("dve", lambda e, ov=ov: e.tensor_tensor(out=ov, in0=ov, in1=stt_[:, 5, :].unsqueeze(2).to_broadcast([128, 4, 64]), op=ALU.mult), reads=[o_, stt_], writes=[o_])
                    kb.op("act", lambda e, t=t: e.activation(out=sg[:], in_=Gr[:, t, :], func=AF.Silu), reads=[Gr], writes=[sg])
                    y_ = yt[t % 2]
                    kb.op("dve", lambda e, y_=y_, o_=o_: e.tensor_tensor(out=y_[:], in0=o_[:], in1=sg[:], op=ALU.mult), reads=[o_, sg], writes=[y_])
                    if rl < 7:
                        continue
                    pt_ = ptr2[0]

                    def tr2(e, pt_=pt_, y_=y_):
                        for c in range(2):
                            ins = e.transpose(pt_[:, c, :], y_[:, c * 128:(c + 1) * 128], ident[:])
                        return ins
                    kb.op("pe", tr2, reads=[y_, ident], writes=[pt_])
                    yT_ = yT[t % 2]
                    kb.op("act", lambda e, pt_=pt_, yT_=yT_: e.activation(out=yT_[:], in_=pt_[:], func=AF.Copy), reads=[pt_], writes=[yT_])
                    kb.dma("sp", YR[:, :, c0 + tc0:c0 + tc0 + 128].rearrange("c p t -> p c t"), yT_[:], reads=[yT_], writes=[kb.dbuf(("YR", b, t))])

    def phase5(l):
        PI = math.pi
        NK = TPB // 8
        with kb.phase():
            alt = [0]

            def ve():
                alt[0] ^= 1
                return "dve" if alt[0] else "pool"
            sd = kb.sb("sd", [128, 2], F32)
            kb.dma("sp", sd[:], S5D[l], writes=[sd])
            bgl = kb.sb("bgl", [128, 2], F32)
            kb.dma("sp", bgl[:], BGLU[l], writes=[bgl])
            wgl = kb.sb("wgl", [128, 2, 256], BF16)
            kb.dma("pool", wgl[:], WGLU[l].rearrange("(k p) n -> p k n", p=128), writes=[wgl])
            Z = kb.sb("Z", [128, 2, S5NJ, 16], F32)
            bb = kb.sb("bb", [128, 2, 16, 16], F32)
            BL = kb.sb("BL", [128, 16, 8, 2, 16], BF16)
            CL = kb.sb("CL", [128, 16, 9, 2, 16], BF16)
            with kb.phase():
                par = kb.sb("par", [128, 3, 16], F32)
                kb.dma("sp", par[:], S5P[l], writes=[par])
                bri = kb.sb("bri", [128, 2, 16, 16], F32)
                kb.dma("sp", bri[:], S5B[l], writes=[bri])
                cri = kb.sb("cri", [128, 2, 16, 16], F32)
                kb.dma("sp", cri[:], S5C[l], writes=[cri])
                jt = kb.sb("jt", [128, S5NJ, 16], F32)
                kb.dma("sp", jt[:], S5JT[:, :, :], writes=[jt])
                negpi = kb.sb("negpi", [128, 1], F32)
                kb.op("dve", lambda e: e.memset(negpi[:], -PI), writes=[negpi])
                lre = par[:, 0, :]
                lim = par[:, 1, :]
                dt = kb.sb("dt", [128, 16], F32)
                kb.op("act", lambda e: e.activation(out=dt[:], in_=par[:, 2, :], func=AF.Exp), reads=[par], writes=[dt])
                ab = kb.sb("ab", [128, 2, 16], F32)
                for i in range(2):
                    kb.op("dve", lambda e, i=i: e.tensor_tensor(out=ab[:, i, :], in0=par[:, i, :], in1=dt[:], op=ALU.mult), reads=[par, dt], writes=[ab])
                am = kb.sb("am", [128, S5NJ, 16], F32)
                bp = kb.sb("bp", [128, S5NJ, 16], F32)
                kb.op("dve", lambda e: e.tensor_tensor(out=am[:], in0=ab[:, 0, :].unsqueeze(1).to_broadcast([128, S5NJ, 16]), in1=jt[:], op=ALU.mult), reads=[ab, jt], writes=[am])
                kb.op("dve", lambda e: e.tensor_tensor(out=bp[:], in0=ab[:, 1, :].unsqueeze(1).to_broadcast([128, S5NJ, 16]), in1=jt[:], op=ALU.mult), reads=[ab, jt], writes=[bp])
                mag = kb.sb("mag", [128, S5NJ, 16], F32)
                kb.op("act", lambda e: e.activation(out=mag[:].rearrange("p a b -> p (a b)"), in_=am[:].rearrange("p a b -> p (a b)"), func=AF.Exp), reads=[am], writes=[mag])
                rsn = kb.sb("rsn", [128, 2, S5NJ, 16], F32)
                MAGIC = 12582912.0
                xs_ = kb.sb("xs_", [128, 2, S5NJ, 16], F32)
                kk_ = kb.sb("kk_", [128, 2, S5NJ, 16], F32)
                kb.op("dve", lambda e: e.tensor_scalar(out=xs_[:, 0, :, :], in0=bp[:], scalar1=0.5 * PI, scalar2=None, op0=ALU.add), reads=[bp], writes=[xs_])
                kb.op("dve", lambda e: e.tensor_copy(out=xs_[:, 1, :, :], in_=bp[:]), reads=[bp], writes=[xs_])
                fl = lambda a: a[:].rearrange("p a b c -> p (a b c)")
                kb.op("dve", lambda e: e.tensor_scalar(out=fl(kk_), in0=fl(xs_), scalar1=1.0 / TWO_PI, scalar2=MAGIC, op0=ALU.mult, op1=ALU.add), reads=[xs_], writes=[kk_])
                kb.op("dve", lambda e: e.tensor_scalar(out=fl(kk_), in0=fl(kk_), scalar1=-MAGIC, scalar2=None, op0=ALU.add), reads=[kk_], writes=[kk_])
                kb.op("dve", lambda e: e.scalar_tensor_tensor(out=fl(rsn), in0=fl(kk_), scalar=-TWO_PI, in1=fl(xs_), op0=ALU.mult, op1=ALU.add), reads=[kk_, xs_], writes=[rsn])
                csn = kb.sb("csn", [128, 2, S5NJ, 16], F32)
                kb.op("act", lambda e: e.activation(out=csn[:].rearrange("p a b c -> p (a b c)"), in_=rsn[:].rearrange("p a b c -> p (a b c)"), func=AF.Sin), reads=[rsn], writes=[csn])
                for i in range(2):
                    kb.op("dve", lambda e, i=i: e.tensor_tensor(out=Z[:, i, :, :], in0=csn[:, i, :, :], in1=mag[:], op=ALU.mult), reads=[csn, mag], writes=[Z])
                tw = kb.sb("tw", [128, 8, 16], F32)
                W = kb.sb("W", [128, 2, 16], F32)

                def tt(o, a, b, op):
                    kb.op("dve", lambda e: e.tensor_tensor(out=o, in0=a, in1=b, op=op), reads=[par, Z, tw, W], writes=[tw, W])
                tt(tw[:, 0, :], lre, lre, ALU.mult)
                tt(tw[:, 1, :], lim, lim, ALU.mult)
                tt(tw[:, 0, :], tw[:, 0, :], tw[:, 1, :], ALU.add)
                kb.op("dve", lambda e: e.reciprocal(out=tw[:, 1, :], in_=tw[:, 0, :]), reads=[tw], writes=[tw])
                kb.op("dve", lambda e: e.tensor_scalar(out=tw[:, 2, :], in0=Z[:, 0, 1, :], scalar1=-1.0, scalar2=None, op0=ALU.add), reads=[Z], writes=[tw])
                tt(tw[:, 3, :], tw[:, 2, :], lre, ALU.mult)
                tt(tw[:, 4, :], Z[:, 1, 1, :], lim, ALU.mult)
                tt(tw[:, 3, :], tw[:, 3, :], tw[:, 4, :], ALU.add)
                tt(W[:, 0, :], tw[:, 3, :], tw[:, 1, :], ALU.mult)
                tt(tw[:, 5, :], Z[:, 1, 1, :], lre, ALU.mult)
                tt(tw[:, 6, :], tw[:, 2, :], lim, ALU.mult)
                tt(tw[:, 5, :], tw[:, 5, :], tw[:, 6, :], ALU.subtract)
                tt(W[:, 1, :], tw[:, 5, :], tw[:, 1, :], ALU.mult)
                t4 = [kb.sb(f"t4_{i}", [128, 16, 16], F32) for i in range(4)]

                def cmul(out_r, out_i, xr, xi, fr, fi, rd, wr, neg_im=False):
                    frb = fr.unsqueeze(2).to_broadcast([128, 16, 16])
                    fib = fi.unsqueeze(2).to_broadcast([128, 16, 16])
                    e1, e2 = ve(), ve()
                    kb.op(e1, lambda e: e.tensor_tensor(out=t4[0][:], in0=xr, in1=frb, op=ALU.mult), reads=rd, writes=[t4[0]])
                    kb.op(e2, lambda e: e.tensor_tensor(out=t4[1][:], in0=xi, in1=fib, op=ALU.mult), reads=rd, writes=[t4[1]])
                    kb.op(e1, lambda e: e.tensor_tensor(out=t4[2][:], in0=xr, in1=fib, op=ALU.mult), reads=rd, writes=[t4[2]])
                    kb.op(e2, lambda e: e.tensor_tensor(out=t4[3][:], in0=xi, in1=frb, op=ALU.mult), reads=rd, writes=[t4[3]])
                    kb.op(e1, lambda e: e.tensor_tensor(out=out_r, in0=t4[0][:], in1=t4[1][:], op=ALU.subtract), reads=[t4[0], t4[1]], writes=wr)
                    if neg_im:
                        kb.op("dve", lambda e: e.scalar_tensor_tensor(out=out_i, in0=t4[2][:], scalar=-1.0, in1=t4[3][:], op0=ALU.mult, op1=ALU.subtract), reads=[t4[2], t4[3]], writes=wr)
                    else:
                        kb.op(e2, lambda e: e.tensor_tensor(out=out_i, in0=t4[2][:], in1=t4[3][:], op=ALU.add), reads=[t4[2], t4[3]], writes=wr)
                cmul(bb[:, 0, :, :], bb[:, 1, :, :], bri[:, 0, :, :], bri[:, 1, :, :], W[:, 0, :], W[:, 1, :], [bri, W], [bb])
                for sidx in range(8):
                    cmul(BL[:, :, sidx, 0, :], BL[:, :, sidx, 1, :], bb[:, 0, :, :], bb[:, 1, :, :], Z[:, 0, 7 - sidx, :], Z[:, 1, 7 - sidx, :], [bb, Z], [BL])
                for j in range(9):
                    cmul(CL[:, :, j, 0, :], CL[:, :, j, 1, :], cri[:, 0, :, :], cri[:, 1, :, :], Z[:, 0, j, :], Z[:, 1, j, :], [cri, Z], [CL], neg_im=True)

            g_fm = kb.sb("g_fm", [128, 2, NTOK], BF16)
            u_fm = kb.sb("u_fm", [128, NTOK], BF16)
            Cp = kb.sb("Cp", [128, 8, 9, 2, 128], BF16)
            Bpad = kb.sb("Bpad", [128, 8, 2, 128], BF16)
            Xd = kb.sb("Xd", [128, 8, 2, 128], BF16)
            Ld = [kb.sb(f"Ld{i}", [128, 8, 2, 128], BF16) for i in range(2)]
            BD = kb.sb("BD", [128, 2, 8, 128], BF16)
            dgl = kb.sb("dgl", [128, 128], BF16)
            Db = [kb.sb(f"Db{d}", [128, 16, NK], F32) for d in range(2)]
            Ssh = kb.sb("Ssh", [128, 2, 16, NK], BF16)
            AcT = [kb.sb(f"AcT{d}", [128, 16, 2, 4, 2], F32) for d in range(2)]
            BcT = [kb.sb(f"BcT{d}", [128, 16, 2, 4, 2], F32) for d in range(2)]
            AcR = kb.sb("AcR", [128, 15, 2, 4, 2], F32)
            BcR = kb.sb("BcR", [128, 15, 2, 4, 2], F32)
            T1 = [kb.sb(f"T1{d}", [128, 16, 18], F32) for d in range(2)]
            T2 = [kb.sb(f"T2{d}", [128, 16, 18], F32) for d in range(2)]
            ptp = [kb.ps(f"ptp{i}", [128, 512]) for i in range(2)]
            pbd = kb.ps("pbd", [128, 512])
            pdv = [kb.ps(f"pdv{i}", [128, 512]) for i in range(2)]
            pyr = [kb.ps(f"pyr{i}", [128, 512]) for i in range(2)]
            pgl = kb.ps("pgl", [128, 512])
            gx2 = [kb.sb(f"gx2{i}", [128, NK], F32) for i in range(2)]
            gt = [kb.sb(f"gt{i}", [128, NK], F32) for i in range(2)]
            gsg = [kb.sb(f"gsg{i}", [128, NK], F32) for i in range(2)]
            uv = u_fm[:].rearrange("p (b k s) -> p b k s", b=NB, s=8)
            nev = 0
            for h in range(2):
                kb.dma("sp", u_fm[:], PF[h, :, :], reads=pf_reads(h), writes=[u_fm])
                kb.op("pool", lambda e: e.memset(Cp[:].rearrange("p a b c d -> p (a b c d)"), 0.0), writes=[Cp])
                kb.op("pool", lambda e: e.memset(Bpad[:].rearrange("p a b c -> p (a b c)"), 0.0), writes=[Bpad])
                kb.op("dve", lambda e, h=h: e.tensor_scalar(out=dgl[:], in0=identf[:], scalar1=sd[:, h:h + 1], scalar2=None, op0=ALU.mult), reads=[identf, sd], writes=[dgl])
                for d in range(2):
                    for j4 in range(4):
                        q = d * 8 + 4 * h + j4
                        ql = d * 4 + j4
                        for g2 in range(2):
                            hs = slice(g2 * 64, g2 * 64 + 64)
                            c0 = 32 * j4 + 16 * g2
                            kb.op(ve(), lambda e, hs=hs, ql=ql, q=q, c0=c0: e.tensor_copy(out=Cp[hs, ql, :, :, c0:c0 + 16].rearrange("p j r c -> p (j r) c"), in_=CL[hs, q, :, :, :].rearrange("p j r c -> p (j r) c")), reads=[CL], writes=[Cp])
                            kb.op(ve(), lambda e, hs=hs, ql=ql, q=q, c0=c0: e.tensor_copy(out=Bpad[hs, ql, :, c0:c0 + 16], in_=bb[hs, :, q, :]), reads=[bb], writes=[Bpad])
                for d in range(2):
                    for l0 in (0, 4):
                        def bdm(e, d=d, l0=l0):
                            for lg in range(l0, l0 + 4):
                                n = 0
                                for j4 in range(4):
                                    for ri in range(2):
                                        ins = e.matmul(pbd[:, (lg - l0) * 128:(lg - l0 + 1) * 128], Bpad[:, d * 4 + j4, ri, :], Cp[:, d * 4 + j4, lg, ri, :], start=(n == 0), stop=(n == 7))
                                        n += 1
                            return ins
                        kb.op("pe", bdm, reads=[Bpad, Cp], writes=[pbd])
                        kb.op("act", lambda e, d=d, l0=l0: e.activation(out=BD[:, d, l0:l0 + 4, :].rearrange("p a b -> p (a b)"), in_=pbd[:], func=AF.Copy), reads=[pbd], writes=[BD])
                for d in range(2):
                    for j4 in range(4):
                        q = d * 8 + 4 * h + j4
                        ql = d * 4 + j4
                        kb.op("pool", lambda e: e.memset(Xd[:].rearrange("p a b c -> p (a b c)"), 0.0), writes=[Xd])
                        for g2 in range(2):
                            hs = slice(g2 * 64, g2 * 64 + 64)
                            c0 = 32 * j4 + 16 * g2
                            kb.op("pool", lambda e, hs=hs, q=q, c0=c0: e.tensor_copy(out=Xd[hs, :, :, c0:c0 + 16].rearrange("p s r c -> p (s r) c"), in_=BL[hs, q, :, :, :].rearrange("p s r c -> p (s r) c")), reads=[BL], writes=[Xd])
                        L_ = Ld[ql % 2]
                        for s0 in range(0, 8, 2):
                            pt_ = ptp[(s0 // 2) % 2]

                            def trm(e, pt_=pt_, s0=s0):
                                for i in range(4):
                                    ins = e.matmul(pt_[:, i * 128:(i + 1) * 128], Xd[:, s0 + i // 2, i % 2, :], ident[:], start=True, stop=True)
                                return ins
                            kb.op("pe", trm, reads=[Xd, ident], writes=[pt_])
                            kb.op("act", lambda e, pt_=pt_, L_=L_, s0=s0: e.activation(out=L_[:, s0:s0 + 2, :, :].rearrange("p a b c -> p (a b c)"), in_=pt_[:], func=AF.Copy), reads=[pt_], writes=[L_])
                        for ri in range(2):
                            for b in range(NB):
                                pd_ = pdv[nev % 2]
                                nev += 1

                                def drv(e, pd_=pd_, L_=L_, ri=ri, b=b, d=d):
                                    for sp_ in range(8):
                                        st_ = sp_ if d == 0 else 7 - sp_
                                        ins = e.matmul(pd_[:, 0:NK], L_[:, sp_, ri, :], uv[:, b, :, st_], start=(sp_ == 0), stop=(sp_ == 7))
                                    return ins
                                kb.op("pe", drv, reads=[L_, u_fm], writes=[pd_])
                                col = ri * 8 + j4 * 2 + b
                                kb.op("act", lambda e, pd_=pd_, d=d, col=col: e.activation(out=Db[d][:, col, :], in_=pd_[:, 0:NK], func=AF.Copy), reads=[pd_], writes=[Db[d]])
                def cplx_acc(eng, d, out_ap, v_lo, v_hi, v_all, a_ap, b_lo, b_hi, shape):
                    t1 = T1[d][:, :, 0:shape[1]] if len(shape) == 2 else T1[d][:, :, 0]
                    t2 = T2[d][:, :, 0:shape[1]] if len(shape) == 2 else T2[d][:, :, 0]
                    kb.op(eng, lambda e: e.tensor_tensor(out=t1, in0=v_all, in1=a_ap, op=ALU.mult), reads=[Db[d], AcT[d], AcR], writes=[T1[d]])
                    kb.op(eng, lambda e: e.tensor_tensor(out=t2[:, 0:8], in0=v_hi, in1=b_lo, op=ALU.mult), reads=[Db[d], BcT[d], BcR], writes=[T2[d]])
                    kb.op(eng, lambda e: e.tensor_tensor(out=t2[:, 8:16], in0=v_lo, in1=b_hi, op=ALU.mult), reads=[Db[d], BcT[d], BcR], writes=[T2[d]])
                    kb.op(eng, lambda e: e.tensor_tensor(out=t1, in0=t1, in1=t2, op=ALU.add), reads=[T1[d], T2[d]], writes=[T1[d]])
                    kb.op(eng, lambda e: e.tensor_tensor(out=out_ap, in0=out_ap, in1=t1, op=ALU.add), reads=[Db[d], T1[d]], writes=[Db[d]])
                for d in range(2):
                    eng = "dve" if d == 0 else "pool"
                    q0 = d * 8 + 4 * h
                    for ri in range(2):
                        kb.op(eng, lambda e, d=d, ri=ri, q0=q0: e.tensor_copy(out=AcT[d][:, :, ri, :, :], in_=Z[:, 0, 8:24, q0:q0 + 4].unsqueeze(3).to_broadcast([128, 16, 4, 2])), reads=[Z], writes=[AcT[d]])
                    kb.op(eng, lambda e, d=d, q0=q0: e.tensor_copy(out=BcT[d][:, :, 1, :, :], in_=Z[:, 1, 8:24, q0:q0 + 4].unsqueeze(3).to_broadcast([128, 16, 4, 2])), reads=[Z], writes=[BcT[d]])
                    kb.op(eng, lambda e, d=d: e.tensor_scalar(out=BcT[d][:, :, 0, :, :], in0=BcT[d][:, :, 1, :, :], scalar1=-1.0, scalar2=None, op0=ALU.mult), reads=[BcT[d]], writes=[BcT[d]])
                    if d == 1:
                        for ri in range(2):
                            kb.op(eng, lambda e, ri=ri, q0=q0: e.tensor_copy(out=AcR[:, :, ri, :, :], in_=Z[:, 0, 24:39, q0:q0 + 4].unsqueeze(3).to_broadcast([128, 15, 4, 2])), reads=[Z], writes=[AcR])
                        kb.op(eng, lambda e, q0=q0: e.tensor_copy(out=BcR[:, :, 1, :, :], in_=Z[:, 1, 24:39, q0:q0 + 4].unsqueeze(3).to_broadcast([128, 15, 4, 2])), reads=[Z], writes=[BcR])
                        kb.op(eng, lambda e: e.tensor_scalar(out=BcR[:, :, 0, :, :], in0=BcR[:, :, 1, :, :], scalar1=-1.0, scalar2=None, op0=ALU.mult), reads=[BcR], writes=[BcR])
                if "rec" not in cfg.get("s5_skip", ()):
                    for d in range(2):
                        eng = "dve" if d == 0 else "pool"
                        Dv = Db[d][:].rearrange("p c (B m) -> p c B m", m=16)
                        fl5 = lambda t, m_: t[:, m_, :, :, :].rearrange("p a b c -> p (a b c)")
                        A0, B0 = fl5(AcT[d], 0), fl5(BcT[d], 0)
                        A15, B15 = fl5(AcT[d], 15), fl5(BcT[d], 15)
                        bc18 = lambda a: a.unsqueeze(2).to_broadcast([128, a.shape[1], 18])
                        ms = list(range(1, 16)) if d == 0 else list(range(14, -1, -1))
                        for m_ in ms:
                            mp = m_ - 1 if d == 0 else m_ + 1
                            cplx_acc(eng, d, Dv[:, :, :, m_], Dv[:, 0:8, :, mp], Dv[:, 8:16, :, mp], Dv[:, :, :, mp], bc18(A0), bc18(B0[:, 0:8]), bc18(B0[:, 8:16]), (16, 18))
                        border = list(range(18)) if d == 0 else [1, 0] + list(range(17, 1, -1))
                        me = 15 if d == 0 else 0
                        for i_ in range(1, 18):
                            Bp, Bc = border[i_ - 1], border[i_]
                            cplx_acc(eng, d, Dv[:, :, Bc, me], Dv[:, 0:8, Bp, me], Dv[:, 8:16, Bp, me], Dv[:, :, Bp, me], A15, B15[:, 0:8], B15[:, 8:16], (16,))
                        if d == 0:
                            Am = AcT[0][:, 0:15, :, :, :].rearrange("p m a b c -> p (a b c) m")
                            Bm = BcT[0][:, 0:15, :, :, :].rearrange("p m a b c -> p (a b c) m")
                            msl = slice(0, 15)
                        else:
                            Am = AcR[:].rearrange("p m a b c -> p (a b c) m")
                            Bm = BcR[:].rearrange("p m a b c -> p (a b c) m")
                            msl = slice(1, 16)
                        bc15 = lambda a: a.unsqueeze(2).to_broadcast([128, a.shape[1], 15])
                        for i_ in range(1, 18):
                            Bp, Bc = border[i_ - 1], border[i_]
                            cplx_acc(eng, d, Dv[:, :, Bc, msl], bc15(Dv[:, 0:8, Bp, me]), bc15(Dv[:, 8:16, Bp, me]), bc15(Dv[:, :, Bp, me]), Am, Bm[:, 0:8, :], Bm[:, 8:16, :], (16, 15))
                kb.op("act", lambda e: e.activation(out=Ssh[:, 0, :, 1:NK], in_=Db[0][:, :, 0:NK - 1], func=AF.Copy), reads=[Db[0]], writes=[Ssh])
                kb.op("dve", lambda e: e.memset(Ssh[:, 0, :, 0:1], 0.0), writes=[Ssh])
                kb.op("act", lambda e: e.activation(out=Ssh[:, 1, :, 0:31], in_=Db[1][:, :, 1:32], func=AF.Copy), reads=[Db[1]], writes=[Ssh])
                kb.op("dve", lambda e: e.memset(Ssh[:, 1, :, 31:32], 0.0), writes=[Ssh])
                kb.op("act", lambda e: e.activation(out=Ssh[:, 1, :, 32:NK - 1], in_=Db[1][:, :, 33:NK], func=AF.Copy), reads=[Db[1]], writes=[Ssh])
                kb.op("act", lambda e: e.activation(out=Ssh[:, 1, :, NK - 1:NK], in_=Db[1][:, :, 0:1], func=AF.Copy), reads=[Db[1]], writes=[Ssh])
                gv = g_fm[:, h, :].rearrange("p (b k s) -> p b k s", b=NB, s=8)
                for b in range(NB if "rdo" not in cfg.get("s5_skip", ()) else 0):
                    for t in range(8):
                        py_ = pyr[t % 2]

                        def rdo(e, py_=py_, b=b, t=t):
                            mm = [(dgl[:], uv[:, b, :, t])]
                            for s_ in range(0, t + 1):
                                mm.append((BD[:, 0, t - s_, :], uv[:, b, :, s_]))
                            for s_ in range(t, 8):
                                mm.append((BD[:, 1, s_ - t, :], uv[:, b, :, s_]))
                            for d in range(2):
                                j = t + 1 if d == 0 else 8 - t
                                for j4 in range(4):
                                    for ri in range(2):
                                        mm.append((Cp[:, d * 4 + j4, j, ri, :], Ssh[:, d, ri * 8 + j4 * 2 + b, :]))
                            for i, (a_, b_) in enumerate(mm):
                                ins = e.matmul(py_[:, 0:NK], a_, b_, start=(i == 0), stop=(i == len(mm) - 1))
                            return ins
                        kb.op("pe", rdo, reads=[dgl, BD, Cp, Ssh, u_fm], writes=[py_])
                        x2 = gx2[t % 2]
                        t_ = gt[t % 2]
                        sg_ = gsg[t % 2]
                        yv = py_[:, 0:NK]
                        kb.op("act", lambda e, x2=x2, yv=yv: e.activation(out=x2[:], in_=yv, func=AF.Square), reads=[py_], writes=[x2])
                        kb.op("dve", lambda e, x2=x2: e.tensor_scalar(out=x2[:], in0=x2[:], scalar1=0.044715, scalar2=1.0, op0=ALU.mult, op1=ALU.add), reads=[x2], writes=[x2])
                        kb.op("dve", lambda e, x2=x2, t_=t_, yv=yv: e.tensor_tensor(out=t_[:], in0=yv, in1=x2[:], op=ALU.mult), reads=[py_, x2], writes=[t_])
                        kb.op("act", lambda e, t_=t_, sg_=sg_: e.activation(out=sg_[:], in_=t_[:], func=AF.Sigmoid, scale=2.0 * math.sqrt(2.0 / PI)), reads=[t_], writes=[sg_])
                        kb.op("dve", lambda e, sg_=sg_, yv=yv, b=b, t=t, gv=gv: e.tensor_tensor(out=gv[:, b, :, t], in0=yv, in1=sg_[:], op=ALU.mult), reads=[py_, sg_], writes=[g_fm])
            sgm = [kb.sb(f"sgm{i}", [128, 512], F32) for i in range(2)]
            yso = [kb.sb(f"yso{i}", [128, 512], BF16) for i in range(2)]
            n = 0
            for tb in range(NTOK // 512):
                cs = slice(tb * 512, (tb + 1) * 512)
                for mo in range(2):
                    def glm(e, mo=mo, cs=cs):
                        for kk in range(2):
                            ins = e.matmul(pgl[:], wgl[:, kk, mo * 128:(mo + 1) * 128], g_fm[:, kk, cs], start=(kk == 0), stop=(kk == 1))
                        return ins
                    kb.op("pe", glm, reads=[wgl, g_fm], writes=[pgl])
                    sm_ = sgm[n % 2]
                    yo_ = yso[n % 2]
                    n += 1
                    kb.op("act", lambda e, sm_=sm_, mo=mo: e.activation(out=sm_[:], in_=pgl[:], func=AF.Sigmoid, bias=bgl[:, mo:mo + 1], scale=1.0), reads=[pgl, bgl], writes=[sm_])
                    kb.op("dve", lambda e, sm_=sm_, yo_=yo_, mo=mo, cs=cs: e.tensor_tensor(out=yo_[:], in0=g_fm[:, mo, cs], in1=sm_[:], op=ALU.mult), reads=[g_fm, sm_], writes=[yo_])
                    kb.dma("sp", YS[mo, :, cs], yo_[:], reads=[yo_], writes=[kb.dbuf(("YS", tb, mo))])

    def bcast_row(dst_ap, src_row_ap, reads, wbuf):
        kb.dma("sp", dst_ap.unsqueeze(1), src_row_ap.partition_broadcast(128), reads=reads, writes=[wbuf])

    def out_rows(l, tt):
        b, r, is_ctx, midx = tile_info(tt)
        if l == DEPTH - 1:
            r0 = b * TSEQ + (r - 2) * 128
            return OUT[r0:r0 + 128, :], kb.dbuf(("OUT", tt))
        return XS[tt * 128:(tt + 1) * 128, :], kb.dbuf(("XS", tt))

    def postnorm(upd_aps, upd_bufs, x_ap, x_buf, gate_ap, gate_buf, lng, tmp, res, st, mv, rs, dst_ap, dst_buf, eng2="pool", eng1="dve"):
        for nb in range(2):
            kb.op(eng1, lambda e, nb=nb: e.tensor_tensor(out=tmp[:, nb * 512:(nb + 1) * 512], in0=upd_aps[nb], in1=gate_ap[:, nb * 512:(nb + 1) * 512], op=ALU.mult),
                  reads=[upd_bufs[nb], gate_buf], writes=[tmp])
        kb.op("dve", lambda e: e.scalar_tensor_tensor(out=res[:], in0=x_ap, scalar=float(ALPHA), in1=tmp[:], op0=ALU.mult, op1=ALU.add), reads=[x_buf, tmp], writes=[res])
        ln_tile(res[:], res, tmp[:], tmp, st, mv, rs)
        kb.op(eng2, lambda e: e.tensor_tensor(out=res[:], in0=tmp[:], in1=lng[:, 0, :], op=ALU.mult), reads=[tmp, lng], writes=[res])
        kb.op(eng2, lambda e: e.tensor_tensor(out=res[:], in0=res[:], in1=lng[:, 1, :], op=ALU.add), reads=[res, lng], writes=[res])
        kb.dma("sp", dst_ap, res[:], reads=[res], writes=[dst_buf])

    def phase6(l, SRC, src_key, need_ctx):
        with kb.phase():
            wout = kb.sb("wout", [128, 8, D], BF16)
            kb.dma("pool", wout[:], WOUT[l].rearrange("(k p) n -> p k n", p=128), writes=[wout])
            gate = kb.sb("gate", [128, 3, D], F32)
            for mi in range(3):
                bcast_row(gate[:, mi, :], MODR[l, mi:mi + 1, 2 * D:3 * D], [kb.dbuf(("MODR", l))], gate)
            lng = kb.sb("lng", [128, 2, D], F32)
            for i in range(2):
                bcast_row(lng[:, i, :], LNG[l, i:i + 1, :], [], lng)
            mixT = [kb.sb(f"mixT{i}", [128, 8, 128], BF16) for i in range(2)]
            xt = [kb.sb(f"xt6{i}", [128, D], F32) for i in range(2)]
            tmp = [kb.sb(f"tmp6{i}", [128, D], F32) for i in range(2)]
            res = [kb.sb(f"res6{i}", [128, D], F32) for i in range(2)]
            st = kb.sb("st6", [128, 2, 6], F32)
            mv = kb.sb("mv6", [128, 2], F32)
            rs = kb.sb("rs6", [128, 2], F32)
            pso = [kb.ps(f"pso{i}", [128, 512]) for i in range(4)]
            YAv = YA.rearrange("h d t -> (h d) t").rearrange("(c p) t -> p c t", p=128)
            tiles6 = [tt for tt in range(NTILES) if need_ctx or not tile_info(tt)[2]]

            def load6(n_):
                tt_ = tiles6[n_]
                b_, r_, _, _ = tile_info(tt_)
                cs_ = slice(tt_ * 128, (tt_ + 1) * 128)
                m_ = mixT[n_ % 2]
                x_ = xt[n_ % 2]
                kb.dma("sp", m_[:, 0:2, :], YS[:, :, cs_].rearrange("c p t -> p c t"), reads=[kb.dbuf(("YS", tt_ // 4, mo)) for mo in range(2)], writes=[m_])
                kb.dma("act", m_[:, 2:6, :], YAv[:, :, cs_], reads=[kb.dbuf(("YA", b_, r_, g)) for g in range(2)], writes=[m_])
                kb.dma("sp", m_[:, 6:8, :], YR[:, :, cs_].rearrange("c p t -> p c t"), reads=[kb.dbuf(("YR", b_, r_))], writes=[m_])
                kb.dma("act", x_[:], SRC[cs_, :], reads=[kb.dbuf((src_key, tt_))], writes=[x_])
            load6(0)
            for n, tt in enumerate(tiles6):
                b, r, is_ctx, midx = tile_info(tt)
                cs = slice(tt * 128, (tt + 1) * 128)
                m_ = mixT[n % 2]
                x_ = xt[n % 2]
                if n + 1 < len(tiles6):
                    load6(n + 1)
                pp = [pso[(n % 2) * 2 + nb] for nb in range(2)]
                for nb in range(2):
                    def mo_(e, nb=nb, m_=m_, p=pp[nb]):
                        for k in range(8):
                            ins = e.matmul(p[:], m_[:, k, :], wout[:, k, nb * 512:(nb + 1) * 512], start=(k == 0), stop=(k == 7))
                        return ins
                    kb.op("pe", mo_, reads=[m_, wout], writes=[pp[nb]])
                dst_ap, dst_buf = XS[cs, :], kb.dbuf(("XS", tt))
                postnorm([pp[0][:], pp[1][:]], pp, x_[:], x_, gate[:, midx, :], gate, lng, tmp[n % 2], res[n % 2], st, mv, rs, dst_ap, dst_buf)

    def phase7(l, need_ctx):
        moe = (l % 2 == 1)
        li = l // 2
        if moe:
            experts = [(MW1[li, e], MW3[li, e], MW2[li, e], e) for e in range(NEXP)]
            nft = DFFE // 128
        else:
            experts = [(FW1[li], FW3[li], FW2[li], None)]
            nft = DFF // 128
        with kb.phase():
            modT = kb.sb("modT7", [128, 48, 3], F32)
            load_modT(l, modT)
            gate = kb.sb("gate7", [128, 3, D], F32)
            for mi in range(3):
                bcast_row(gate[:, mi, :], MODR[l, mi:mi + 1, 5 * D:6 * D], [kb.dbuf(("MODR", l))], gate)
            lng = kb.sb("lng7", [128, 2, D], F32)
            for i in range(2):
                bcast_row(lng[:, i, :], LNG[l, 2 + i:3 + i, :], [], lng)
            rout = kb.sb("rout", [128, 8, NEXP], BF16)
            if moe:
                kb.dma("pool", rout[:], ROUT[li].rearrange("(k p) e -> p k e", p=128), writes=[rout])
            SGN = 8 if moe else 12
            acc = kb.sb("acc", [128, SGN, D], F32)
            hT = kb.sb("hT7", [128, 8, SGN * 128], BF16)
            G = kb.sb("G7", [128, SGN, NEXP], F32)
            w1c = [kb.sb(f"w1c{i}", [128, 8, 512], BF16) for i in range(2)]
            w3c = [kb.sb(f"w3c{i}", [128, 8, 512], BF16) for i in range(2)]
            w2c = [kb.sb(f"w2c{i}", [128, 4, D], BF16) for i in range(2)]
            act = [kb.sb(f"act{i}", [128, 4, 512], BF16) for i in range(2)]
            sa = [kb.sb(f"sa{i}", [128, 512], F32) for i in range(2)]
            xt = [kb.sb(f"xt7{i}", [128, D], F32) for i in range(2)]
            xn = [kb.sb(f"xn7{i}", [128, D], BF16) for i in range(2)]
            tmpm = [kb.sb(f"tmpm7{i}", [128, 8, 128], F32) for i in range(2)]
            tmp = [kb.sb(f"tmp7{i}", [128, D], F32) for i in range(2)]
            res = [kb.sb(f"res7{i}", [128, D], F32) for i in range(2)]
            st = kb.sb("st7", [128, 2, 6], F32)
            mv = kb.sb("mv7", [128, 2], F32)
            rs = kb.sb("rs7", [128, 2], F32)
            gs_ = kb.sb("gs7", [128, 6, NEXP], F32)
            gm = kb.sb("gm7", [128, 4], F32)
            ptr = kb.ps("ptr7", [128, 8, 128], BF16)
            pup = [kb.ps(f"pup{i}", [128, 512]) for i in range(4)]
            pdn = [kb.ps(f"pdn{i}", [128, 512]) for i in range(2)]
            plg = kb.ps("plg", [128, NEXP])
            tiles = [tt for tt in range(NTILES) if need_ctx or not tile_info(tt)[2]]
            sgs = [tiles[i:i + SGN] for i in range(0, len(tiles), SGN)][:cfg.get("p7_sgs", 99)]
            items = []
            for sgi in range(len(sgs)):
                fst = True
                for (W1, W3, W2, ex) in experts[:cfg.get("p7_exp", 99)]:
                    for ch0 in range(0, nft, 4):
                        items.append((sgi, W1, W3, W2, ex, ch0, min(4, nft - ch0), fst))
                        fst = False
            loaded = set()

            def load_w(k):
                if k >= len(items) or k in loaded:
                    return
                loaded.add(k)
                (sgi_, W1, W3, W2, ex, ch0, nf, fst) = items[k]
                f0 = ch0 * 128
                if cfg.get("p7_nodma") and k > 2:
                    return
                kb.dma("pool", w1c[k % 2][:, :, 0:nf * 128], W1[:, f0:f0 + nf * 128].rearrange("(k p) f -> p k f", p=128), writes=[w1c[k % 2]])
                kb.dma("pool", w3c[k % 2][:, :, 0:nf * 128], W3[:, f0:f0 + nf * 128].rearrange("(k p) f -> p k f", p=128), writes=[w3c[k % 2]])
                kb.dma("pool", w2c[k % 2][:, 0:nf, :], W2[f0:f0 + nf * 128, :].rearrange("(f p) n -> p f n", p=128), writes=[w2c[k % 2]])
            load_w(0)
            kpos = 0
            nun = 0
            cnt = {"up": 0, "dn": 0}
            for sgi, sg in enumerate(sgs):
                ntl = len(sg)

                def load7(i_, sg=sg):
                    tt_ = sg[i_]
                    kb.dma("sp", xt[i_ % 2][:], XS[tt_ * 128:(tt_ + 1) * 128, :], reads=[kb.dbuf(("XS", tt_))], writes=[xt[i_ % 2]])
                load7(0)
                for i, tt in enumerate(sg):
                    b, r, is_ctx, midx = tile_info(tt)
                    x_ = xt[i % 2]
                    xnb = xn[i % 2]
                    if i + 1 < ntl:
                        load7(i + 1)
                    ln_tile(x_[:], x_, xnb[:], xnb, st, mv, rs)

                    def tr(e, xnb=xnb):
                        for k in range(8):
                            ins = e.transpose(ptr[:, k, :], xnb[:, k * 128:(k + 1) * 128], ident[:])
                        return ins
                    kb.op("pe", tr, reads=[xnb, ident], writes=[ptr])
                    tm_ = tmpm[i % 2]
                    kb.op("dve", lambda e, midx=midx, tm_=tm_: e.tensor_tensor(out=tm_[:], in0=ptr[:], in1=modT[:, 32:40, midx].unsqueeze(2).to_broadcast([128, 8, 128]), op=ALU.mult),
                          reads=[ptr, modT], writes=[tm_])
                    kb.op("pool", lambda e, i=i, midx=midx, tm_=tm_: e.tensor_tensor(out=hT[:, :, i * 128:(i + 1) * 128], in0=tm_[:], in1=modT[:, 24:32, midx].unsqueeze(2).to_broadcast([128, 8, 128]), op=ALU.add),
                          reads=[tm_, modT], writes=[hT])
                    if moe:
                        def rl_(e, i=i):
                            for k in range(8):
                                ins = e.matmul(plg[:], hT[:, k, i * 128:(i + 1) * 128], rout[:, k, :], start=(k == 0), stop=(k == 7))
                            return ins
                        kb.op("pe", rl_, reads=[hT, rout], writes=[plg])
                        kb.op("act", lambda e: e.activation(out=gs_[:, 0, :], in_=plg[:], func=AF.Copy), reads=[plg], writes=[gs_])
                        kb.op("dve", lambda e: e.tensor_reduce(out=gm[:, 0:1], in_=gs_[:, 0, :], axis=AX.X, op=ALU.max), reads=[gs_], writes=[gm])
                        kb.op("dve", lambda e: e.tensor_scalar(out=gs_[:, 1, :], in0=gs_[:, 0, :], scalar1=gm[:, 0:1], scalar2=None, op0=ALU.is_equal), reads=[gs_, gm], writes=[gs_])
                        kb.op("dve", lambda e: e.scalar_tensor_tensor(out=gs_[:, 2, :], in0=gs_[:, 1, :], scalar=-1e30, in1=gs_[:, 0, :], op0=ALU.mult, op1=ALU.add), reads=[gs_], writes=[gs_])
                        kb.op("dve", lambda e: e.tensor_reduce(out=gm[:, 1:2], in_=gs_[:, 2, :], axis=AX.X, op=ALU.max), reads=[gs_], writes=[gm])
                        kb.op("dve", lambda e: e.tensor_scalar(out=gs_[:, 3, :], in0=gs_[:, 0, :], scalar1=gm[:, 1:2], scalar2=None, op0=ALU.is_ge), reads=[gs_, gm], writes=[gs_])
                        kb.op("dve", lambda e: e.tensor_scalar(out=gm[:, 2:3], in0=gm[:, 0:1], scalar1=-1.0, scalar2=None, op0=ALU.mult), reads=[gm], writes=[gm])
                        kb.op("act", lambda e: e.activation(out=gs_[:, 4, :], in_=gs_[:, 0, :], func=AF.Exp, bias=gm[:, 2:3], scale=1.0), reads=[gs_, gm], writes=[gs_])
                        kb.op("dve", lambda e: e.tensor_tensor(out=gs_[:, 5, :], in0=gs_[:, 4, :], in1=gs_[:, 3, :], op=ALU.mult), reads=[gs_], writes=[gs_])
                        kb.op("dve", lambda e: e.tensor_reduce(out=gm[:, 3:4], in_=gs_[:, 5, :], axis=AX.X, op=ALU.add), reads=[gs_], writes=[gm])
                        kb.op("dve", lambda e: e.reciprocal(out=gm[:, 3:4], in_=gm[:, 3:4]), reads=[gm], writes=[gm])
                        kb.op("dve", lambda e, i=i: e.tensor_scalar(out=G[:, i, :], in0=gs_[:, 5, :], scalar1=gm[:, 3:4], scalar2=None, op0=ALU.mult), reads=[gs_, gm], writes=[G])
                units = []
                while kpos < len(items) and items[kpos][0] == sgi:
                    for tb0 in range(0, ntl, 4):
                        units.append((kpos, tb0))
                    kpos += 1

                def up_thunks(k, tb0, ac):
                    (sgi_, W1, W3, W2, ex, ch0, nf, fst) = items[k]
                    a1, a3 = w1c[k % 2], w3c[k % 2]
                    nt = min(4, ntl - tb0)
                    ncol = nt * 128
                    c0 = tb0 * 128
                    th = []
                    for ft in range(nf):
                        def one(ft=ft):
                            n_ = cnt["up"]
                            cnt["up"] += 1
                            pa = pup[(n_ % 2) * 2]
                            pb = pup[(n_ % 2) * 2 + 1]
                            s_ = sa[n_ % 2]

                            def up(e):
                                for (p, w) in ((pa, a1), (pb, a3)):
                                    for kx in range(8):
                                        ins = e.matmul(p[:, 0:ncol], w[:, kx, ft * 128:(ft + 1) * 128], hT[:, kx, c0:c0 + ncol], start=(kx == 0), stop=(kx == 7))
                                return ins
                            kb.op("pe", up, reads=[a1, a3, hT], writes=[pa, pb])
                            kb.op("act", lambda e: e.activation(out=s_[:, 0:ncol], in_=pa[:, 0:ncol], func=AF.Silu), reads=[pa], writes=[s_])
                            kb.op("dve", lambda e: e.tensor_tensor(out=ac[:, ft, 0:ncol], in0=pb[:, 0:ncol], in1=s_[:, 0:ncol], op=ALU.mult), reads=[pb, s_], writes=[ac])
                        th.append(one)
                    return th

                def dn_thunks(k, tb0, ac):
                    (sgi_, W1, W3, W2, ex, ch0, nf, fst) = items[k]
                    a2 = w2c[k % 2]
                    nt = min(4, ntl - tb0)
                    th = []
                    for ti in range(nt):
                        for nb in range(2):
                            def one(ti=ti, nb=nb):
                                i = tb0 + ti
                                po = pdn[cnt["dn"] % 2]
                                cnt["dn"] += 1

                                def dn(e):
                                    for ft in range(nf):
                                        ins = e.matmul(po[:], ac[:, ft, ti * 128:(ti + 1) * 128], a2[:, ft, nb * 512:(nb + 1) * 512], start=(ft == 0), stop=(ft == nf - 1))
                                    return ins
                                kb.op("pe", dn, reads=[ac, a2], writes=[po])
                                av = acc[:, i, nb * 512:(nb + 1) * 512]
                                if ex is None:
                                    if fst:
                                        kb.op("act", lambda e: e.activation(out=av, in_=po[:], func=AF.Copy), reads=[po], writes=[acc])
                                    else:
                                        kb.op("dve", lambda e: e.tensor_tensor(out=av, in0=po[:], in1=av, op=ALU.add), reads=[po, acc], writes=[acc])
                                else:
                                    if fst:
                                        kb.op("dve", lambda e: e.tensor_scalar(out=av, in0=po[:], scalar1=G[:, i, ex:ex + 1], scalar2=None, op0=ALU.mult), reads=[po, G], writes=[acc])
                                    else:
                                        kb.op("dve", lambda e: e.scalar_tensor_tensor(out=av, in0=po[:], scalar=G[:, i, ex:ex + 1], in1=av, op0=ALU.mult, op1=ALU.add), reads=[po, G, acc], writes=[acc])
                            th.append(one)
                    return th
                prev = None
                for (k, tb0) in units:
                    ac = act[nun % 2]
                    nun += 1
                    ups = up_thunks(k, tb0, ac)
                    dns = dn_thunks(*prev) if prev is not None else []
                    per = -(-len(dns) // len(ups)) if dns else 0
                    for iu, u in enumerate(ups):
                        u()
                        for dth in dns[iu * per:(iu + 1) * per]:
                            dth()
                    for dth in dns[len(ups) * per:]:
                        dth()
                    load_w(k + 1)
                    prev = (k, tb0, ac)
                for dth in dn_thunks(*prev):
                    dth()
                load7(0)
                for i, tt in enumerate(sg):
                    b, r, is_ctx, midx = tile_info(tt)
                    x_ = xt[i % 2]
                    if i + 1 < ntl:
                        load7(i + 1)
                    dst_ap, dst_buf = out_rows(l, tt)
                    postnorm([acc[:, i, 0:512], acc[:, i, 512:1024]], [acc, acc], x_[:], x_, gate[:, midx, :], gate, lng, tmp[i % 2], res[i % 2], st, mv, rs, dst_ap, dst_buf, eng2="pool", eng1="pool")

    if cfg.get("only_p7"):
        phase7(1, False)
        layers = []
    for l in layers:
        if stop_after == ("p1",):
            break
        if l == 0:
            phase2(l, XZ, "XZ")
        else:
            phase2(l, XS, "XS")
        if stop_after == ("p2", l):
            break
        need_ctx = l < DEPTH - 1
        if "att" not in cfg.get("skip", ()):
            phase3(l, need_ctx)
        if "ret" not in cfg.get("skip", ()):
            phase4(l, need_ctx)
        if stop_after == ("p4", l):
            break
        if "s5" not in cfg.get("skip", ()):
            phase5(l)
        if stop_after == ("p5", l):
            break
        phase6(l, XZ if l == 0 else XS, "XZ" if l == 0 else "XS", need_ctx)
        if stop_after == ("p6", l):
            break
        phase7(l, need_ctx)
        if stop_after == ("p7", l):
            break

    kb.barrier()
    kb.emit()
    return nc


def prep_shared(inp):
    sh = {}
    sh["w_mod"] = np.ascontiguousarray(inp["w_mod"], dtype=np.float32)
    sh["b_mod"] = np.ascontiguousarray(inp["b_mod"], dtype=np.float32)
    cols = win_columns()
    sh["w_in_p"] = np.ascontiguousarray(inp["w_in"][:, :, cols], dtype=np.float32)
    tab, tm = rope_tables()
    sh["rope_tab"] = tab
    sh["rope_tm"] = tm
    sh["ident"] = np.eye(128, dtype=np.float32)
    kj = np.arange(128)[:, None]
    qi = np.arange(128)[None, :]
    mP = (kj >= qi).astype(np.float32)
    mN = (kj <= qi).astype(np.float32)
    sh["att_mask"] = np.ascontiguousarray(np.stack([np.broadcast_to(mP[:, None, :], (128, 4, 128)), np.broadcast_to(mN[:, None, :], (128, 4, 128))], axis=1))
    sh["attn_sink"] = np.ascontiguousarray(inp["attn_sink"], dtype=np.float32)
    sh["ret_d12"] = np.ascontiguousarray(np.stack([np.maximum(qi - kj, 0), np.maximum(kj - qi, 0)], axis=1).astype(np.float32))
    sh["ret_jc"] = np.ascontiguousarray(np.stack([127 - np.arange(128), np.arange(128)], axis=1).astype(np.float32))
    sh["ret_irow"] = np.ascontiguousarray(np.broadcast_to(np.stack([np.arange(128) + 1, 128 - np.arange(128)], axis=0)[None], (128, 2, 128)).astype(np.float32))
    lg = np.asarray(inp["ret_log_gamma"], dtype=np.float32)
    sh["ret_lgb"] = np.ascontiguousarray(np.broadcast_to(lg.reshape(DEPTH, 1, 8), (DEPTH, 128, 8)))
    lgp = np.zeros((DEPTH, 128, 2, 2), np.float32)
    for p_ in range(128):
        for c_ in range(2):
            lgp[:, p_, c_, :] = lg[:, :, 2 * c_ + p_ // 64]
    sh["ret_lgp"] = lgp
    def col(a):
        a = np.asarray(a, dtype=np.float32)
        sh_ = a.shape
        a = a.reshape(sh_[0], 2, 8, 2, 64, *sh_[4:])
        a = np.moveaxis(a, (3, 4), (1, 2))
        return np.ascontiguousarray(a.reshape(sh_[0], 128, 16, *sh_[4:]))
    lst = np.broadcast_to(np.asarray(inp["s5_log_step"], np.float32)[:, :, :, None], (DEPTH, 2, 16, 64))
    sh["s5_par"] = np.ascontiguousarray(np.stack([col(inp["s5_lam_re"]), col(inp["s5_lam_im"]), col(lst)], axis=2))
    sh["s5_b"] = np.ascontiguousarray(np.stack([col(inp["s5_b_re"]), col(inp["s5_b_im"])], axis=2))
    cre = np.swapaxes(np.asarray(inp["s5_c_re"], np.float32), 3, 4)
    cim = np.swapaxes(np.asarray(inp["s5_c_im"], np.float32), 3, 4)
    sh["s5_c"] = np.ascontiguousarray(np.stack([col(cre), col(cim)], axis=2))
    jv = list(range(9)) + [8 * (m_ + 1) for m_ in range(1, 16)] + [120 - 8 * i_ for i_ in range(15)]
    sh["s5_jt"] = np.ascontiguousarray(np.broadcast_to(np.asarray(jv, dtype=np.float32)[None, :, None], (128, S5NJ, 16)))
    sh["s5_dcol"] = np.ascontiguousarray(np.asarray(inp["s5_d"], np.float32).reshape(DEPTH, 2, 128).transpose(0, 2, 1))
    sh["s5_w_glu"] = np.ascontiguousarray(inp["s5_w_glu"], dtype=np.float32)
    sh["s5_bglu"] = np.ascontiguousarray(np.asarray(inp["s5_b_glu"], np.float32).reshape(DEPTH, 2, 128).transpose(0, 2, 1))
    sh["w_out"] = np.ascontiguousarray(inp["w_out"], dtype=np.float32)
    sh["ln_gb"] = np.ascontiguousarray(np.stack([inp["ln1_g"], inp["ln1_b"], inp["ln2_g"], inp["ln2_b"]], axis=1), dtype=np.float32)
    for k_ in ("ffn_w1", "ffn_w3", "ffn_w2", "moe_router", "moe_w1", "moe_w3", "moe_w2"):
        sh[k_] = np.ascontiguousarray(inp[k_], dtype=np.float32)
    return sh


def prep_core(inp, c):
    b0 = c * NB
    xz = np.concatenate([np.concatenate([inp["ctx"][b0 + i], inp["x"][b0 + i]], axis=0) for i in range(NB)], axis=0)
    cv = np.stack([inp["c"][b0], inp["c"][b0 + 1], inp["c_ctx"]], axis=0)
    cT = np.ascontiguousarray(cv.reshape(3, 8, 128).transpose(2, 1, 0))
    return {"xz": np.ascontiguousarray(xz, dtype=np.float32), "cT": cT.astype(np.float32)}


def kernel(**inputs):
    inp = {k: np.asarray(v) for k, v in inputs.items()}
    nc = build()
    sh = prep_shared(inp)
    in_maps = []
    for c in range(NCORES):
        m = dict(sh)
        m.update(prep_core(inp, c))
        in_maps.append(m)
    res = run_bass_kernel_spmd(nc, in_maps, core_ids=list(range(NCORES)))
    out = np.concatenate([r["out"].reshape(NB, TSEQ, D) for r in res.results], axis=0)
    return out.astype(np.float32)
```
